# Optimizing a Trainium2 kernel written in Bass

```python
import jax, jax.numpy as jnp
from jax import lax
import numpy as np

D_MODEL = 1024
BATCH = 8
SEQ = 4096
DEPTH = 1

EPS = 1e-6
ROPE_THETA = 10000.0
ATT_HEADS = 8
ATT_HEAD_DIM = 64
IDX_HEADS = 8
IDX_HEAD_DIM = 64
TOPK_MAX = 256
Q_BLOCK = 64
RET_HEADS = 4
RET_QK_DIM = 128
RET_V_DIM = 256
RET_CHUNK = 128
D_FF = 2816
CONV_WIDTH = 3

ATT_W = ATT_HEADS * ATT_HEAD_DIM
IDX_QW = IDX_HEADS * IDX_HEAD_DIM
RET_QKW = RET_HEADS * RET_QK_DIM
RET_VW = RET_HEADS * RET_V_DIM
IN_SPLITS = (ATT_W, ATT_W, ATT_W,
             IDX_QW, IDX_HEAD_DIM, IDX_HEADS,
             RET_QKW, RET_QKW, RET_VW, RET_VW,
             D_MODEL, D_MODEL)
IN_WIDTH = int(sum(IN_SPLITS))

kernel_name = "hybrid_dsa_retention_convffn_block"


def rms_norm(x, g):
    xf = x.astype(jnp.float32)
    y = xf * lax.rsqrt(jnp.mean(xf * xf, axis=-1, keepdims=True) + EPS)
    return (y * g.astype(jnp.float32)).astype(x.dtype)


def rotary(x, positions):
    d = x.shape[-1]
    half = d // 2
    inv_freq = ROPE_THETA ** (-(jnp.arange(half, dtype=jnp.float32) * 2.0 / d))
    ang = positions.astype(jnp.float32)[..., None] * inv_freq
    cos = jnp.cos(ang)[:, :, None, :]
    sin = jnp.sin(ang)[:, :, None, :]
    xf = x.astype(jnp.float32)
    x1, x2 = xf[..., :half], xf[..., half:]
    out = jnp.concatenate([x1 * cos - x2 * sin, x2 * cos + x1 * sin], axis=-1)
    return out.astype(x.dtype)


def indexer_sparse_attention(q, k, v, q_idx, k_idx, w_idx):
    B, T, H, dh = q.shape
    topk = min(TOPK_MAX, T // 4)
    n_blocks = T // Q_BLOCK
    key_pos = jnp.arange(T)
    scale = dh ** -0.5

    def block(start):
        qi = lax.dynamic_slice_in_dim(q_idx, start, Q_BLOCK, axis=1)
        wi = lax.dynamic_slice_in_dim(w_idx, start, Q_BLOCK, axis=1)
        qb = lax.dynamic_slice_in_dim(q, start, Q_BLOCK, axis=1)
        q_pos = start + jnp.arange(Q_BLOCK)
        rel = jax.nn.relu(jnp.einsum('bqhd,bsd->bqhs', qi, k_idx))
        score = jnp.einsum('bqhs,bqh->bqs', rel, wi).astype(jnp.float32)
        causal = key_pos[None, :] <= q_pos[:, None]
        score = jnp.where(causal[None], score, -jnp.inf)
        _, idx = lax.top_k(score, topk)
        k_sel = jax.vmap(lambda kb, ib: kb[ib])(k, idx)
        v_sel = jax.vmap(lambda vb, ib: vb[ib])(v, idx)
        logits = jnp.einsum('bqhd,bqkhd->bhqk', qb, k_sel).astype(jnp.float32) * scale
        valid = (idx <= q_pos[None, :, None])[:, None]
        p = jax.nn.softmax(jnp.where(valid, logits, -jnp.inf), axis=-1).astype(v.dtype)
        return jnp.einsum('bhqk,bqkhd->bqhd', p, v_sel)

    starts = jnp.arange(n_blocks) * Q_BLOCK
    out = lax.map(block, starts)
    return jnp.transpose(out, (1, 0, 2, 3, 4)).reshape(B, T, H * dh)


def chunkwise_retention(q, k, v):
    B, T, Hr, dk = q.shape
    dv = v.shape[-1]
    C = RET_CHUNK
    N = T // C
    dt = q.dtype
    log_gamma = jnp.log(1.0 - 2.0 ** (-5.0 - jnp.arange(Hr, dtype=jnp.float32)))
    i = jnp.arange(C, dtype=jnp.float32)
    diff = i[:, None] - i[None, :]
    inner_decay = jnp.where(diff[None] >= 0,
                            jnp.exp(jnp.maximum(diff, 0.0)[None] * log_gamma[:, None, None]),
                            0.0).astype(dt)
    xi = jnp.exp((i + 1.0)[:, None] * log_gamma[None, :])[..., None].astype(dt)
    zeta = jnp.exp((C - 1.0 - i)[:, None] * log_gamma[None, :])[..., None].astype(dt)
    chunk_decay = jnp.exp(C * log_gamma).astype(dt)[:, None, None]

    def to_chunks(a):
        return jnp.transpose(a.reshape(B, N, C, Hr, a.shape[-1]), (1, 0, 2, 3, 4))

    def step(R, inp):
        qc, kc, vc = inp
        att = jnp.einsum('bihd,bjhd->bhij', qc, kc) * inner_decay
        inner = jnp.einsum('bhij,bjhe->bihe', att, vc)
        cross = jnp.einsum('bihd,bhde->bihe', qc, R) * xi
        R_new = R * chunk_decay + jnp.einsum('bjhd,bjhe->bhde', kc * zeta, vc)
        return R_new, inner + cross

    R0 = jnp.zeros((B, Hr, dk, dv), dtype=dt)
    _, ys = lax.scan(step, R0, (to_chunks(q), to_chunks(k), to_chunks(v)))
    return jnp.transpose(ys, (1, 0, 2, 3, 4)).reshape(B, T, Hr, dv)


def head_group_norm(o, gain):
    of = o.astype(jnp.float32)
    mu = jnp.mean(of, axis=-1, keepdims=True)
    var = jnp.mean(jnp.square(of - mu), axis=-1, keepdims=True)
    y = ((of - mu) * lax.rsqrt(var + EPS)).reshape(o.shape[0], o.shape[1], -1)
    return (y * gain.astype(jnp.float32)).astype(o.dtype)


def token_mixing(h, positions, w_in, w_br_attn, w_br_ret, ret_gn_gain, w_out):
    B, T, _ = h.shape
    proj = h @ w_in
    cuts = [int(c) for c in np.cumsum(IN_SPLITS)[:-1]]
    (aq, ak, av, iq, ik, iw, rq, rk, rv, rg, ga, gb) = jnp.split(proj, cuts, axis=-1)

    aq = rotary(aq.reshape(B, T, ATT_HEADS, ATT_HEAD_DIM), positions)
    ak = rotary(ak.reshape(B, T, ATT_HEADS, ATT_HEAD_DIM), positions)
    av = av.reshape(B, T, ATT_HEADS, ATT_HEAD_DIM)
    iq = rotary(iq.reshape(B, T, IDX_HEADS, IDX_HEAD_DIM), positions)
    ik = rotary(ik.reshape(B, T, 1, IDX_HEAD_DIM), positions)[:, :, 0]
    iw = iw * (IDX_HEADS ** -0.5 * IDX_HEAD_DIM ** -0.5)
    y_a = indexer_sparse_attention(aq, ak, av, iq, ik, iw) @ w_br_attn

    rq = rotary(rq.reshape(B, T, RET_HEADS, RET_QK_DIM), positions)
    rk = rotary(rk.reshape(B, T, RET_HEADS, RET_QK_DIM), positions) * (RET_QK_DIM ** -0.5)
    rv = rv.reshape(B, T, RET_HEADS, RET_V_DIM)
    ret = head_group_norm(chunkwise_retention(rq, rk, rv), ret_gn_gain)
    y_b = (jax.nn.silu(rg) * ret) @ w_br_ret

    merged = jax.nn.sigmoid(ga) * y_a + jax.nn.sigmoid(gb) * y_b
    return merged @ w_out


def conv_ffn(h, w_ffn_up, conv_w, conv_b, w_ffn_down):
    u = h @ w_ffn_up
    ch = u.shape[-1]
    u = lax.conv_general_dilated(u, conv_w[:, None, :], window_strides=(1,),
                                 padding=[(CONV_WIDTH - 1, 0)],
                                 dimension_numbers=('NWC', 'WIO', 'NWC'),
                                 feature_group_count=ch) + conv_b
    gate, up = jnp.split(u, 2, axis=-1)
    return (jax.nn.silu(gate) * up) @ w_ffn_down


def setup_inputs(seed: int = 0) -> dict:
    key = jax.random.key(seed)
    ks = jax.random.split(key, 16)
    f32 = jnp.float32

    def dense(k, fan_in, fan_out):
        return jax.random.normal(k, (DEPTH, fan_in, fan_out), f32) * fan_in ** -0.5

    def gain(k, n):
        return 1.0 + 0.05 * jax.random.normal(k, (DEPTH, n), f32)

    x = jax.random.normal(ks[0], (BATCH, SEQ, D_MODEL), f32)
    positions = jnp.broadcast_to(jnp.arange(SEQ, dtype=jnp.int32)[None], (BATCH, SEQ))
    return {
        "x": x,
        "positions": positions,
        "norm_pre_mix": gain(ks[1], D_MODEL),
        "w_in": dense(ks[2], D_MODEL, IN_WIDTH),
        "w_br_attn": dense(ks[3], ATT_W, D_MODEL),
        "w_br_ret": dense(ks[4], RET_VW, D_MODEL),
        "ret_gn_gain": gain(ks[5], RET_VW),
        "w_out": dense(ks[6], D_MODEL, D_MODEL),
        "norm_post_mix": gain(ks[7], D_MODEL),
        "norm_pre_ffn": gain(ks[8], D_MODEL),
        "w_ffn_up": dense(ks[9], D_MODEL, 2 * D_FF),
        "conv_w": jax.random.normal(ks[10], (DEPTH, CONV_WIDTH, 2 * D_FF), f32) * CONV_WIDTH ** -0.5,
        "conv_b": 0.02 * jax.random.normal(ks[11], (DEPTH, 2 * D_FF), f32),
        "w_ffn_down": dense(ks[12], D_FF, D_MODEL),
        "norm_post_ffn": gain(ks[13], D_MODEL),
    }


def reference(x, positions, norm_pre_mix, w_in, w_br_attn, w_br_ret, ret_gn_gain, w_out,
              norm_post_mix, norm_pre_ffn, w_ffn_up, conv_w, conv_b, w_ffn_down, norm_post_ffn):
    for l in range(DEPTH):
        h = rms_norm(x, norm_pre_mix[l])
        m = token_mixing(h, positions, w_in[l], w_br_attn[l], w_br_ret[l], ret_gn_gain[l], w_out[l])
        x = x + rms_norm(m, norm_post_mix[l])
        h = rms_norm(x, norm_pre_ffn[l])
        f = conv_ffn(h, w_ffn_up[l], conv_w[l], conv_b[l], w_ffn_down[l])
        x = x + rms_norm(f, norm_post_ffn[l])
    return x
```

```python
import numpy as np
import ml_dtypes
from contextlib import ExitStack
import concourse.bass as bass
import concourse.mybir as mybir
from concourse.bass_utils import run_bass_kernel_spmd

F32 = mybir.dt.float32
BF16 = mybir.dt.bfloat16
I32 = mybir.dt.int32
ALU = mybir.AluOpType
AF = mybir.ActivationFunctionType
AX = mybir.AxisListType

D = 1024
T_FULL = 4096
NTF = 32
INW = 7240
DFF = 2816
EPS = 1e-6
TOPK = 256
NEG = -1.0e30
NBIS = 20
TWO_PI = float(2 * np.pi)


class Buf:
    __slots__ = ("wr", "rd", "excl")

    def __init__(self, excl=False):
        self.wr = None
        self.rd = []
        self.excl = excl


class Sched:
    ENGS = ("pe", "dve", "act", "pool", "sp")

    def __init__(self, nc, stack):
        self.nc = nc
        self.stack = stack
        self.lists = {e: [] for e in self.ENGS}
        self.sems = {}
        self.count = {}
        self.known = {e: {} for e in self.ENGS}
        for e in self.ENGS:
            self._newsem("E_" + e)

    def _newsem(self, key):
        s = self.stack.enter_context(self.nc.semaphore(key))
        self.sems[key] = s
        self.count[key] = 0

    def _deps(self, eng, reads, writes):
        deps = {}
        own = "E_" + eng

        def add(ev):
            if ev is None:
                return
            k, v = ev
            if eng == "pe" and k == own:
                return
            if deps.get(k, 0) < v:
                deps[k] = v
        for b in reads:
            add(b.wr)
        for b in writes:
            add(b.wr)
            for ev in b.rd:
                add(ev)
        waits = []
        kn = self.known[eng]
        for k, v in deps.items():
            if kn.get(k, 0) < v:
                kn[k] = v
                waits.append((k, v))
        return waits

    def _post(self, ev, reads, writes):
        for b in reads:
            b.rd.append(ev)
        for b in writes:
            b.wr = ev
            b.rd = []

    @staticmethod
    def _split(reads, writes):
        ex = [b for b in reads if b.excl]
        if not ex:
            return reads, writes
        return [b for b in reads if not b.excl], list(writes) + ex

    def op(self, eng, fn, reads=(), writes=()):
        reads, writes = self._split(reads, writes)
        waits = self._deps(eng, reads, writes)
        key = "E_" + eng
        self.count[key] += 1
        ev = (key, self.count[key])
        self.lists[eng].append((waits, fn, key, 1, self.count[key]))
        self._post(ev, reads, writes)

    def dma(self, eng, fn, semkey, reads=(), writes=()):
        if semkey not in self.sems:
            self._newsem(semkey)
        waits = self._deps(eng, reads, writes)
        self.count[semkey] += 16
        ev = (semkey, self.count[semkey])
        self.lists[eng].append((waits, fn, semkey, 16, None))
        self._post(ev, reads, writes)

    def wait_all(self, eng, bufs):
        waits = self._deps(eng, bufs, bufs)
        self.lists[eng].append((waits, None, None, 0, None))

    def emit(self):
        sems = self.sems
        lists = self.lists
        needed = {}
        for e in self.ENGS:
            for waits, fn, key, inc, seq in lists[e]:
                for k, v in waits:
                    if k.startswith("E_"):
                        needed.setdefault(k, set()).add(v)
        rank = {}
        for e in self.ENGS:
            key = "E_" + e
            need = needed.get(key, set())
            m = {}
            r = 0
            for waits, fn, k, inc, seq in lists[e]:
                if seq is not None and seq in need:
                    r += 1
                    m[seq] = r
            rank[key] = m

        def replay(e, items):
            for waits, fn, key, inc, seq in items:
                for k, v in waits:
                    if k.startswith("E_"):
                        v = rank[k][v]
                    e.wait_ge(sems[k], v)
                if fn is not None:
                    ins = fn(e)
                    if seq is None:
                        ins.then_inc(sems[key], inc)
                    elif seq in rank[key]:
                        ins.then_inc(sems[key], 1)

        with self.nc.Block() as block:
            @block.tensor
            def _(e):
                replay(e, lists["pe"])

            @block.vector
            def _(e):
                replay(e, lists["dve"])

            @block.scalar
            def _(e):
                replay(e, lists["act"])

            @block.gpsimd
            def _(e):
                replay(e, lists["pool"])

            @block.sync
            def _(e):
                replay(e, lists["sp"])
        self.stats = {k: len(v) for k, v in rank.items()}


class TB:
    def __init__(self, t, n=1, excl=False):
        self.t = t
        self.b = [Buf(excl) for _ in range(n)]


def build_program(NT=NTF, dbg=False, stop_after=None):
    nc = bass.Bass("TRN2", target_bir_lowering=False)
    T = NT * 128

    def din(name, shape, dt=F32):
        return nc.dram_tensor(name, list(shape), dt, kind="ExternalInput").ap()

    def dscr(name, shape, dt):
        return nc.dram_tensor(name, list(shape), dt, kind=("ExternalOutput" if dbg else "Internal")).ap()

    x_d = din("x", [T, D])
    pos_d = din("pos", [128, NT], I32)
    g1_d = din("g_pre_mix", [1, D])
    w_in_d = din("w_in", [D, INW])
    wba_d = din("w_br_attn", [512, D])
    wbr_d = din("w_br_ret", [D, D])
    ggnT_d = din("ggnT", [128, 8])
    wo_d = din("w_out", [D, D])
    g2_d = din("g_post_mix", [1, D])
    g3_d = din("g_pre_ffn", [1, D])
    wup_d = din("w_ffn_up", [D, 2 * DFF])
    cw_d = din("conv_w", [128, 44, 3])
    cb_d = din("conv_b", [128, 44])
    wdn_d = din("w_ffn_down", [DFF, D])
    g4_d = din("g_post_ffn", [1, D])
    invf_d = din("c_invf", [128, 96])
    ident_d = din("c_ident", [128, 128], BF16)
    cmask_d = din("c_cmask", [128, 128])
    decT_d = din("c_decT", [128, 4, 128])
    zc_d = din("c_zc", [128, 4])
    epsg_d = din("c_epsg", [128, 4])
    out_d = nc.dram_tensor("out", [T, D], F32, kind="ExternalOutput").ap()

    fm_d = dscr("s_fm", [NT, 128, 2048], BF16)
    kT_d = dscr("s_kT", [NT, 128, 512], BF16)
    ikT_d = dscr("s_ikT", [64, T], BF16)
    iw_d = dscr("s_iw", [T, 8], F32)
    v_d = dscr("s_v", [T, 520], BF16)
    tm_d = dscr("s_tm", [T, 4608], BF16)
    x1_d = dscr("s_x1", [T, D], F32)

    cdec = [float((1.0 - 2.0 ** (-5.0 - h)) ** 128) for h in range(4)]

    with ExitStack() as st:
        S = Sched(nc, st)

        def sb(name, shape, dt, n=1):
            return TB(st.enter_context(nc.sbuf_tensor(name, list(shape), dt)), n)

        def ps(name, shape, dt, n=1):
            return TB(st.enter_context(nc.psum_tensor(name, list(shape), dt)), n, True)

        def mm(out, lhsT, rhs, start, stop, reads, writes):
            S.op("pe", lambda e: e.matmul(out, lhsT=lhsT, rhs=rhs, start=start, stop=stop), reads, writes)

        def tr(out, in_, ident_ap, reads, writes):
            S.op("pe", lambda e: e.transpose(out=out, in_=in_, identity=ident_ap), reads, writes)

        def act(out, in_, func, reads, writes, bias=None, scale=None, accum=None, saturate=None):
            kw = {}
            if saturate is not None:
                kw["saturate"] = saturate
            if bias is not None:
                kw["bias"] = bias
            if scale is not None:
                kw["scale"] = scale
            if accum is not None:
                kw["accum_out"] = accum
            S.op("act", lambda e: e.activation(out=out, in_=in_, func=func, **kw), reads, writes)

        def tt(eng, out, in0, in1, op, reads, writes):
            S.op(eng, lambda e: e.tensor_tensor(out=out, in0=in0, in1=in1, op=op), reads, writes)

        def ts(eng, out, in0, s1, s2, op0, op1, reads, writes, accum=None):
            kw = {}
            if op1 is not None:
                kw["op1"] = op1
            if accum is not None:
                kw["accum_out"] = accum
            S.op(eng, lambda e: e.tensor_scalar(out=out, in0=in0, scalar1=s1, scalar2=s2, op0=op0, **kw), reads, writes)

        def stt(eng, out, in0, scalar, in1, op0, op1, reads, writes):
            S.op(eng, lambda e: e.scalar_tensor_tensor(out=out, in0=in0, scalar=scalar, in1=in1, op0=op0, op1=op1),
                 reads, writes)

        def cp(eng, out, in_, reads, writes):
            if eng == "act":
                S.op("act", lambda e: e.activation(out=out, in_=in_, func=AF.Copy), reads, writes)
            else:
                S.op(eng, lambda e: e.tensor_copy(out=out, in_=in_), reads, writes)

        def dma(eng, out, in_, key, reads, writes):
            S.dma(eng, lambda e: e.dma_start(out=out, in_=in_), key, reads, writes)

        chain = Buf()

        def cdma(out, in_, writes):
            S.dma("sp", lambda e: e.dma_start(out=out, in_=in_), "ld_misc", [], list(writes) + [chain])

        ident = sb("ident", [128, 128], BF16)
        epsc = sb("epsc", [128, 1], F32)
        cdma(ident.t[:], ident_d, ident.b)
        S.op("dve", lambda e: e.memset(epsc.t[:], EPS), [], epsc.b)

        def rstd_from_ss(ss_ap, ln_ap, out_ap, inv_n, reads, writes):
            act(ln_ap, ss_ap, AF.Ln, list(reads) + epsc.b, writes, bias=epsc.t[:, 0:1], scale=inv_n)
            act(out_ap, ln_ap, AF.Exp, writes, writes, scale=-0.5)

        with ExitStack() as sa:
            def sba(name, shape, dt, n=1):
                return TB(sa.enter_context(nc.sbuf_tensor(name, list(shape), dt)), n)

            def psa(name, shape, dt, n=1):
                return TB(sa.enter_context(nc.psum_tensor(name, list(shape), dt)), n, True)

            win = sba("win", [128, 8, INW], BF16, 8)
            gbc = sba("gbc", [128, D], F32)
            invf = sba("invf", [128, 96], F32)
            posi = sba("posi", [128, NT], I32)
            posf = sba("posf", [128, NT], F32)
            rt = [sba(f"rt{j}", [128, 3, 96], F32) for j in range(2)]
            ang = sba("ang", [128, 96], F32)
            angk = sba("angk", [128, 96], F32)
            angi = sba("angi", [128, 96], I32)
            zc = sba("zc", [128, 4], F32)
            xin = [sba(f"xin{j}", [128, D], F32) for j in range(2)]
            junkb = sba("junkb", [128, D], BF16)
            sm = [sba(f"smA{j}", [128, 8], F32) for j in range(2)]
            hb = sba("hb", [128, D], BF16)
            hT = [sba(f"hT{j}", [128, 8, 128], BF16) for j in range(2)]
            t1 = [sba(f"t1_{j}", [128, 512], F32) for j in range(2)]
            t2 = [sba(f"t2_{j}", [128, 512], F32) for j in range(2)]
            rob = [sba(f"rob{j}", [128, 512], BF16) for j in range(3)]
            tm = [sba(f"tmA{j}", [128, 4608], BF16, 9) for j in range(2)]
            fm = [sba(f"fmA{j}", [128, 2048], BF16, 4) for j in range(2)]
            kTs = [sba(f"kTs{j}", [128, 512], BF16) for j in range(2)]
            vs = [sba(f"vs{j}", [128, 8, 65], BF16) for j in range(2)]
            iks = [sba(f"iks{j}", [64, 128], BF16) for j in range(2)]
            iws = [sba(f"iws{j}", [128, 8], F32) for j in range(2)]
            pj = [psa(f"pj{j}", [128, 512], F32) for j in range(5)]
            ptr = [psa(f"ptrA{j}", [128, 1024], BF16) for j in range(2)]

            for kc in range(8):
                dma("pool", win.t[:, kc, :], w_in_d[kc * 128:(kc + 1) * 128, :], f"ld_win{kc}", [], [win.b[kc]])
            cdma(gbc.t[:], g1_d.broadcast_to([128, D]), gbc.b)
            cdma(invf.t[:], invf_d, invf.b)
            cdma(posi.t[:], pos_d, posi.b)
            cdma(zc.t[:], zc_d, zc.b)
            for j in range(2):
                S.op("pool", lambda e, j=j: e.memset(vs[j].t[:], 1.0), [], vs[j].b)

            cp("dve", posf.t[:], posi.t[:], posi.b, posf.b)

            def range_reduce(shift):
                ts("dve", angk.t[:], ang.t[:], shift, 1.0 / TWO_PI, ALU.add, ALU.mult, ang.b, angk.b)
                cp("dve", angi.t[:], angk.t[:], angk.b, angi.b)
                cp("dve", angk.t[:], angi.t[:], angi.b, angk.b)
                ts("dve", angk.t[:], angk.t[:], -TWO_PI, shift, ALU.mult, ALU.add, angk.b, angk.b)
                tt("dve", angk.t[:], angk.t[:], ang.t[:], ALU.add, angk.b + ang.b, angk.b)
                ts("dve", angk.t[:], angk.t[:], float(np.pi), -float(np.pi), ALU.min, ALU.max, angk.b, angk.b)

            def make_tables(i):
                r_ = rt[i % 2]
                ts("dve", ang.t[:], invf.t[:], posf.t[:, i:i + 1], None, ALU.mult, None, invf.b + posf.b, ang.b)
                range_reduce(float(np.pi / 2))
                act(r_.t[:, 0, :], angk.t[:], AF.Sin, angk.b, r_.b)
                range_reduce(0.0)
                act(r_.t[:, 2, :], angk.t[:], AF.Sin, angk.b, r_.b)
                ts("dve", r_.t[:, 1, :], r_.t[:, 2, :], -1.0, None, ALU.mult, None, r_.b, r_.b)

            def rotary(i, src, nh, half, toff, dst, reads, writes, k):
                a1, a2 = t1[k % 2], t2[k % 2]
                rti = rt[i % 2]
                w = nh * 2 * half
                s4 = src.rearrange("p (h t j) -> p h t j", h=nh, t=2, j=half)
                o1 = a1.t[:, 0:w].rearrange("p (h t j) -> p h t j", h=nh, t=2, j=half)
                o2 = a2.t[:, 0:w].rearrange("p (h t j) -> p h t j", h=nh, t=2, j=half)
                cosb = rti.t[:, 0, toff:toff + half].unsqueeze(1).unsqueeze(1).broadcast_to([128, nh, 2, half])
                nsin = rti.t[:, 1, toff:toff + half].unsqueeze(1).broadcast_to([128, nh, half])
                psin = rti.t[:, 2, toff:toff + half].unsqueeze(1).broadcast_to([128, nh, half])
                tt("dve", o1, s4, cosb, ALU.mult, list(reads) + rti.b, a1.b)
                tt("dve", o2[:, :, 0, :], s4[:, :, 1, :], nsin, ALU.mult, list(reads) + rti.b, a2.b)
                tt("dve", o2[:, :, 1, :], s4[:, :, 0, :], psin, ALU.mult, list(reads) + rti.b, a2.b)
                tt("dve", dst, a1.t[:, 0:w], a2.t[:, 0:w], ALU.add, a1.b + a2.b, writes)

            ncol = [512] * 4 + [72] + [512] * 10
            coff = [0]
            for c in ncol:
                coff.append(coff[-1] + c)
            rotc = {"k": 0}

            def drive(gy, gx):
                cy = cx = 0.0
                while gy is not None or gx is not None:
                    if gy is not None and (gx is None or cy <= cx):
                        try:
                            cy += next(gy)
                        except StopIteration:
                            gy = None
                    else:
                        try:
                            cx += next(gx)
                        except StopIteration:
                            gx = None

            def PA_gen(i):
                a = i % 2
                xt, smt, hTt = xin[a], sm[a], hT[a]
                dma("sp", xt.t[:], x_d[i * 128:(i + 1) * 128, :], f"ld_x{a}", [], xt.b)
                make_tables(i)
                act(junkb.t[:], xt.t[:], AF.Square, xt.b, junkb.b + smt.b, accum=smt.t[:, 0:1])
                rstd_from_ss(smt.t[:, 0:1], smt.t[:, 1:2], smt.t[:, 2:3], 1.0 / D, smt.b, smt.b)
                stt("dve", hb.t[:], xt.t[:], smt.t[:, 2:3], gbc.t[:], ALU.mult, ALU.mult,
                    xt.b + smt.b + gbc.b, hb.b)
                yield 12.0
                pt = ptr[0]
                for kc in range(8):
                    tr(pt.t[:, kc * 128:(kc + 1) * 128], hb.t[:, kc * 128:(kc + 1) * 128], ident.t[:],
                       hb.b + ident.b, pt.b)
                cp("act", hTt.t[:].rearrange("p a b -> p (a b)"), pt.t[:], pt.b, hTt.b)
                yield 1.0

            def MA_gen(i):
                a = i % 2
                hTt, tmt, fmt = hT[a], tm[a], fm[a]
                pend = []

                def flush(item):
                    c, r = item
                    pt = ptr[1]
                    if c == 4:
                        tr(pt.t[0:64, 0:128], r.t[:, 0:64], ident.t[:], r.b + ident.b, pt.b)
                        cp("act", iks[a].t[:], pt.t[0:64, 0:128], pt.b, iks[a].b)
                        dma("sp", ikT_d[:, i * 128:(i + 1) * 128], iks[a].t[:], f"st_ik{a}", iks[a].b, [])
                        return
                    for q in range(4):
                        tr(pt.t[:, q * 128:(q + 1) * 128], r.t[:, q * 128:(q + 1) * 128], ident.t[:],
                           r.b + ident.b, pt.b)
                    if c == 1:
                        cp("act", kTs[a].t[:], pt.t[:, 0:512], pt.b, kTs[a].b)
                        dma("sp", kT_d[i], kTs[a].t[:], f"st_kT{a}", kTs[a].b, [])
                    else:
                        slot = {0: 0, 3: 1, 5: 2, 6: 3}[c]
                        cp("act", fmt.t[:, slot * 512:(slot + 1) * 512], pt.t[:, 0:512], pt.b, [fmt.b[slot]])

                for c in range(15):
                    pb = pj[c % 5]
                    w = ncol[c]
                    for kc in range(8):
                        mm(pb.t[:, 0:w], hTt.t[:, kc, :], win.t[:, kc, coff[c]:coff[c] + w], kc == 0, kc == 7,
                           hTt.b + [win.b[kc]], pb.b)
                    if c in (0, 1, 3, 5, 6):
                        r = rob[rotc["k"] % 3]
                        if c in (5, 6):
                            rotary(i, pb.t[:, :], 4, 64, 0, r.t[:, 0:512], pb.b, r.b, rotc["k"])
                        else:
                            rotary(i, pb.t[:, :], 8, 32, 64, r.t[:, 0:512], pb.b, r.b, rotc["k"])
                        rotc["k"] += 1
                        if c == 6:
                            tt("dve", tmt.t[:, 0:512].rearrange("p (h d) -> p h d", h=4),
                               r.t[:].rearrange("p (h d) -> p h d", h=4),
                               zc.t[:].unsqueeze(2).broadcast_to([128, 4, 128]), ALU.mult,
                               r.b + zc.b, [tmt.b[0]])
                        pend.append((c, r))
                    elif c == 2:
                        cp("act", vs[a].t[:, :, 0:64], pb.t[:, :].rearrange("p (h d) -> p h d", h=8), pb.b, vs[a].b)
                        dma("sp", v_d[i * 128:(i + 1) * 128, :], vs[a].t[:].rearrange("p h d -> p (h d)"),
                            f"st_v{a}", vs[a].b, [])
                    elif c == 4:
                        r = rob[rotc["k"] % 3]
                        rotary(i, pb.t[:, 0:64], 1, 32, 64, r.t[:, 0:64], pb.b, r.b, rotc["k"])
                        rotc["k"] += 1
                        ts("dve", iws[a].t[:], pb.t[:, 64:72], float(8 ** -0.5 * 64 ** -0.5), None, ALU.mult, None,
                           pb.b, iws[a].b)
                        dma("sp", iw_d[i * 128:(i + 1) * 128, :], iws[a].t[:], f"st_iw{a}", iws[a].b, [])
                        pend.append((c, r))
                    elif c in (7, 8):
                        o = 512 + (c - 7) * 512
                        cp("act", tmt.t[:, o:o + 512], pb.t[:, :], pb.b, [tmt.b[1 + c - 7]])
                    elif c in (9, 10):
                        o = 1536 + (c - 9) * 512
                        act(tmt.t[:, o:o + 512], pb.t[:, :], AF.Silu, pb.b, [tmt.b[3 + c - 9]])
                    else:
                        o = 2560 + (c - 11) * 512
                        act(tmt.t[:, o:o + 512], pb.t[:, :], AF.Sigmoid, pb.b, [tmt.b[5 + c - 11]])
                    while pend and pend[0][0] <= c - 2:
                        flush(pend.pop(0))
                    yield 1.7
                while pend:
                    flush(pend.pop(0))
                dma("sp", fm_d[i], fmt.t[:], f"st_fm{a}", fmt.b, [])
                dma("sp", tm_d[i * 128:(i + 1) * 128, :], tmt.t[:], f"st_tm{a}", tmt.b, [])
                yield 0.5

            drive(None, PA_gen(0))
            for i in range(NT):
                drive(MA_gen(i), PA_gen(i + 1) if i + 1 < NT else None)
            fence_a = []
            for a in range(2):
                fence_a += tm[a].b + fm[a].b + kTs[a].b + vs[a].b + iks[a].b + iws[a].b
            allA = fence_a + win.b + gbc.b + invf.b + posi.b + posf.b + rt[0].b + rt[1].b + ang.b + angk.b + angi.b + zc.b + \
                xin[0].b + xin[1].b + junkb.b + sm[0].b + sm[1].b + hb.b + hT[0].b + hT[1].b + \
                t1[0].b + t1[1].b + t2[0].b + t2[1].b + rob[0].b + rob[1].b + rob[2].b + \
                [p.b[0] for p in pj] + [p.b[0] for p in ptr]
        for e in Sched.ENGS:
            S.wait_all(e, allA)

        if stop_after == "A":
            S.wait_all("sp", allA)
            S.emit()
            return nc

        with ExitStack() as sbk:
            def sbb(name, shape, dt, n=1):
                return TB(sbk.enter_context(nc.sbuf_tensor(name, list(shape), dt)), n)

            def psb(name, shape, dt, n=1):
                return TB(sbk.enter_context(nc.psum_tensor(name, list(shape), dt)), n, True)

            NGC = (NT + 7) // 8
            kT = sbb("kTc", [128, NT, 512], BF16, NGC)
            V = sbb("Vc", [128, NT, 520], BF16, NGC)
            ikT = sbb("ikTc", [128, T], BF16, NGC)
            wba = sbb("wba", [128, 4, D], BF16)
            wbr = sbb("wbr", [128, 8, D], BF16)
            wo = sbb("wo", [128, 8, D], BF16)
            ggnT = sbb("ggnT_sb", [128, 8], F32)
            g2 = sbb("g2", [128, D], F32)
            cmask = sbb("cmask", [128, 128], F32)
            decT = sbb("decT", [128, 4, 128], F32)
            epsg = sbb("epsg", [128, 4], F32)
            bigI = sbb("bigI", [128, 128], BF16)
            fmq = [sbb(f"fmq{j}", [128, 2048], BF16) for j in range(2)]
            tmq = sbb("tmq0", [128, 4608], BF16)
            iwq = [sbb(f"iwq{j}", [128, 8], F32) for j in range(2)]
            Th = [sbb(f"Th{j}", [128, 512], BF16) for j in range(3)]
            PT = [sbb(f"PT{j}", [128, 512], BF16) for j in range(3)]
            Sc = sbb("Sc", [128, T], F32)
            maskb = sbb("maskb", [128, T], BF16)
            jnkA = sbb("jnkA", [128, max(128, T - ((T * 9 // 20) // 128) * 128)], mybir.dt.float8e4)
            maskT = [sbb(f"maskT{j}", [128, NT, 128], BF16) for j in range(2)]
            bs = sbb("bs", [128, 8], F32)
            bsd = sbb("bsd", [128, 2], F32)
            bsa = sbb("bsa", [128, 2], F32)
            wk = sbb("wk", [128, NBIS + 1], F32)
            pw2 = sbb("pw2", [128, NBIS + 1], F32)
            rden = sbb("rden", [128, 8], F32)
            On = sbb("On", [128, 512], BF16)
            OnT = sbb("OnT", [128, 4, 128], BF16)
            attb = [sbb(f"attb{j}", [128, 128], BF16) for j in range(2)]
            Rf = sbb("Rf", [128, 4, 256], F32, 4)
            Rb = sbb("Rb", [128, 4, 256], BF16, 4)
            gst = sbb("gst", [128, 4, 6], F32)
            gmv = sbb("gmv", [128, 4, 2], F32)
            gsm = sbb("gsm", [128, 16], F32)
            fA = sbb("fA", [128, D], F32)
            fB = sbb("fB", [128, D], F32)
            ub = sbb("ub", [128, D], BF16)
            uT = sbb("uT", [128, 8, 128], BF16)
            sm2 = sbb("sm2", [128, 8], F32)
            pS = [psb(f"pS{j}", [128, 512], F32) for j in range(3)]
            ptX = psb("ptX", [128, 1024], BF16)
            pL = [psb(f"pL{j}", [128, 512], F32) for j in range(2)]
            pLb = [p.t.bitcast(BF16) for p in pL]
            pY = psb("pY", [128, 1024], F32, 2)

            for kc in range(4):
                dma("pool", wba.t[:, kc, :], wba_d[kc * 128:(kc + 1) * 128, :], "ld_wba", [], wba.b)
            for kc in range(8):
                dma("pool", wbr.t[:, kc, :], wbr_d[kc * 128:(kc + 1) * 128, :], "ld_wbr", [], wbr.b)
            for kc in range(8):
                dma("pool", wo.t[:, kc, :], wo_d[kc * 128:(kc + 1) * 128, :], "ld_wo", [], wo.b)
            cdma(ggnT.t[:], ggnT_d, ggnT.b)
            cdma(g2.t[:], g2_d.broadcast_to([128, D]), g2.b)
            cdma(cmask.t[:], cmask_d, cmask.b)
            cdma(decT.t[:], decT_d, decT.b)
            cdma(epsg.t[:], epsg_d, epsg.b)
            ts("dve", bigI.t[:], ident.t[:], 30000.0, None, ALU.mult, None, ident.b, bigI.b)
            for gc in range(NGC):
                t0, t1_ = gc * 8, min(NT, gc * 8 + 8)
                dma("sp", ikT.t[0:64, t0 * 128:t1_ * 128], ikT_d[:, t0 * 128:t1_ * 128], f"ld_ikA{gc}", fence_a,
                    [ikT.b[gc]])
                dma("sp", ikT.t[64:128, t0 * 128:t1_ * 128], ikT_d[:, t0 * 128:t1_ * 128], f"ld_ikB{gc}", fence_a,
                    [ikT.b[gc]])
                dma("sp", kT.t[:, t0:t1_, :], kT_d[t0:t1_].rearrange("t p c -> p t c"), f"ld_kT{gc}", fence_a,
                    [kT.b[gc]])
                dma("sp", V.t[:, t0:t1_, :], v_d[t0 * 128:t1_ * 128, :].rearrange("(t p) c -> p t c", p=128),
                    f"ld_V{gc}", fence_a, [V.b[gc]])
            for h in range(4):
                S.op("dve", lambda e, h=h: e.memset(Rf.t[:, h, :], 0.0), [], [Rf.b[h]])
                S.op("dve", lambda e, h=h: e.memset(Rb.t[:, h, :], 0.0), [], [Rb.b[h]])
            for k in range(NBIS + 1):
                kk = min(k, NBIS - 1)
                S.op("dve", lambda e, k=k, kk=kk: e.memset(pw2.t[:, k:k + 1], float(2.0 ** -(kk + 1))), [], pw2.b)

            cnt = {"s": 0, "l": 0, "th": 0, "pt": 0}

            def X_gen(i):
                a = i % 2
                fq, wq, mT_ = fmq[a], iwq[a], maskT[a]
                nkt = i + 1
                nk = nkt * 128
                dma("sp", fq.t[:], fm_d[i], f"ld_fmq{a}", fence_a, fq.b)
                dma("sp", wq.t[:], iw_d[i * 128:(i + 1) * 128, :], f"ld_iwq{a}", fence_a, wq.b)
                nch = (nkt + 3) // 4
                for c in range(nch):
                    k0 = c * 512
                    wc = min(512, nk - k0)
                    kts = list(range(c * 4, min(c * 4 + 4, nkt)))
                    for h in range(8):
                        hp, pr = h % 2, h // 2
                        pb = pS[cnt["s"] % 3]
                        cnt["s"] += 1
                        thb = Th[cnt["th"] % 3]
                        cnt["th"] += 1
                        mm(pb.t[:, 0:wc], fq.t[hp * 64:(hp + 1) * 64, 512 + pr * 128:512 + (pr + 1) * 128],
                           ikT.t[hp * 64:(hp + 1) * 64, k0:k0 + wc], True, True,
                           fq.b + [ikT.b[k // 8] for k in kts], pb.b)
                        act(thb.t[:, 0:wc], pb.t[:, 0:wc], AF.Relu, pb.b, thb.b)
                        if h == 0:
                            ts("dve", Sc.t[:, k0:k0 + wc], thb.t[:, 0:wc], wq.t[:, 0:1], None, ALU.mult, None,
                               thb.b + wq.b, Sc.b)
                        else:
                            stt("dve", Sc.t[:, k0:k0 + wc], thb.t[:, 0:wc], wq.t[:, h:h + 1], Sc.t[:, k0:k0 + wc],
                                ALU.mult, ALU.add, thb.b + wq.b + Sc.b, Sc.b)
                        yield 0.25
                    if kts[-1] == i:
                        d0 = k0 + wc - 128
                        tt("dve", Sc.t[:, d0:d0 + 128], Sc.t[:, d0:d0 + 128], cmask.t[:], ALU.add,
                           Sc.b + cmask.b, Sc.b)
                if i >= 2:
                    S.op("dve", lambda e: e.tensor_reduce(out=bs.t[:, 0:1], in_=Sc.t[:, 0:nk], axis=AX.X,
                                                          op=ALU.max), Sc.b, bs.b)
                    S.op("dve", lambda e: e.tensor_reduce(out=bs.t[:, 1:2], in_=Sc.t[:, 0:nk - 128], axis=AX.X,
                                                          op=ALU.min), Sc.b, bs.b)
                    tt("dve", bs.t[:, 2:3], bs.t[:, 0:1], bs.t[:, 1:2], ALU.subtract, bs.b, bs.b)
                    ts("dve", wk.t[:], pw2.t[:], bs.t[:, 2:3], None, ALU.mult, None, pw2.b + bs.b, wk.b)
                    tt("dve", bs.t[:, 3:4], bs.t[:, 1:2], wk.t[:, 0:1], ALU.add, bs.b + wk.b, bs.b)
                    yield 2 * nk / 960.0 + 1.0
                    nd = ((nk * 9 // 20) // 128) * 128
                    nA = nk - nd
                    for k in range(NBIS):
                        ts("dve", maskb.t[:, 0:nd], Sc.t[:, 0:nd], bs.t[:, 3:4], None, ALU.is_ge, ALU.add,
                           Sc.b + bs.b, maskb.b + bsd.b, accum=bsd.t[:, 0:1])
                        act(jnkA.t[:, 0:nA], Sc.t[:, nd:nk], AF.Sign, Sc.b + bs.b, jnkA.b + bsa.b,
                            bias=bs.t[:, 3:4], scale=-1.0, accum=bsa.t[:, 0:1], saturate=False)
                        stt("dve", bs.t[:, 4:5], bsa.t[:, 0:1], -0.5, bsd.t[:, 0:1], ALU.mult, ALU.add,
                            bsa.b + bsd.b, bs.b)
                        ts("dve", bs.t[:, 5:6], bs.t[:, 4:5], TOPK - 0.5 - nA / 2.0, wk.t[:, k:k + 1],
                           ALU.is_ge, ALU.mult, bs.b + wk.b, bs.b)
                        stt("dve", bs.t[:, 3:4], bs.t[:, 5:6], wk.t[:, k + 1:k + 2], bs.t[:, 3:4],
                            ALU.subtract, ALU.add, bs.b + wk.b, bs.b)
                        yield nk / 1900.0 + 1.2
                    ts("dve", maskb.t[:, 0:nk], Sc.t[:, 0:nk], bs.t[:, 3:4], -1.0, ALU.is_ge, ALU.add,
                       Sc.b + bs.b, maskb.b)
                else:
                    ts("dve", maskb.t[:, 0:nk], Sc.t[:, 0:nk], -1.0e29, -1.0, ALU.is_ge, ALU.add, Sc.b, maskb.b)
                yield nk / 1900.0 + 0.3
                for g in range((nkt + 7) // 8):
                    ktl = list(range(g * 8, min(g * 8 + 8, nkt)))
                    for j, kt in enumerate(ktl):
                        tr(ptX.t[:, j * 128:(j + 1) * 128], maskb.t[:, kt * 128:(kt + 1) * 128], ident.t[:],
                           maskb.b + ident.b, ptX.b)
                    n = len(ktl)
                    cp("act", mT_.t[:, g * 8:g * 8 + n, :].rearrange("p a b -> p (a b)"), ptX.t[:, 0:n * 128],
                       ptX.b, mT_.b)
                    yield 1.0

            def nextL():
                j = cnt["l"] % 2
                cnt["l"] += 1
                return pL[j], pLb[j]

            def Y_gen(i):
                a = i % 2
                fq, tq, mT_ = fmq[a], tmq, maskT[a]
                nkt = i + 1
                dma("sp", tq.t[:], tm_d[i * 128:(i + 1) * 128, :], "ld_tmq", fence_a, tq.b)
                ngrp = (nkt + 3) // 4
                groups = [(h, g) for h in range(8) for g in range(ngrp)]

                def qk(h, g):
                    hp, pr = h % 2, h // 2
                    ktl = list(range(g * 4, min(g * 4 + 4, nkt)))
                    n = len(ktl)
                    pb, _ = nextL()
                    ptt = PT[cnt["pt"] % 3]
                    cnt["pt"] += 1
                    mm(pb.t[:, 0:n * 128], bigI.t[:],
                       mT_.t[:, g * 4:g * 4 + n, :].rearrange("p a b -> p (a b)"), True, False,
                       bigI.b + mT_.b, pb.b)
                    for j, kt in enumerate(ktl):
                        mm(pb.t[:, j * 128:(j + 1) * 128],
                           kT.t[hp * 64:(hp + 1) * 64, kt, pr * 128:(pr + 1) * 128],
                           fq.t[hp * 64:(hp + 1) * 64, pr * 128:(pr + 1) * 128], False, j == n - 1,
                           [kT.b[kt // 8]] + fq.b, pb.b)
                    act(ptt.t[:, 0:n * 128], pb.t[:, 0:n * 128], AF.Exp, pb.b, ptt.b, scale=0.125)
                    return ktl, ptt

                def pv(h, ktl, ptt):
                    hb_ = pY.b[0] if h < 4 else pY.b[1]
                    ocol = (h % 4) * 65 + (0 if h < 4 else 512)
                    for j, kt in enumerate(ktl):
                        mm(pY.t[:, ocol:ocol + 65], ptt.t[:, j * 128:(j + 1) * 128],
                           V.t[:, kt, h * 65:(h + 1) * 65], kt == 0, kt == nkt - 1,
                           ptt.b + [V.b[kt // 8]], [hb_])

                nxt = qk(*groups[0])
                for k, (h, g) in enumerate(groups):
                    cur = nxt
                    if k + 1 < len(groups):
                        nxt = qk(*groups[k + 1])
                    pv(h, *cur)
                    yield 1.6
                for half in range(2):
                    o4 = pY.t[:, half * 512:half * 512 + 260].rearrange("p (h d) -> p h d", h=4)
                    S.op("dve", lambda e, o4=o4, half=half: e.reciprocal(
                        out=rden.t[:, half * 4:half * 4 + 4].unsqueeze(2), in_=o4[:, :, 64:65]),
                        [pY.b[half]], rden.b)
                    tt("dve", On.t[:, half * 256:(half + 1) * 256].rearrange("p (h d) -> p h d", h=4),
                       o4[:, :, 0:64], rden.t[:, half * 4:half * 4 + 4].unsqueeze(2).broadcast_to([128, 4, 64]),
                       ALU.mult, [pY.b[half]] + rden.b, On.b)
                pb, pbb = nextL()
                for q in range(4):
                    tr(pbb[:, q * 128:(q + 1) * 128], On.t[:, q * 128:(q + 1) * 128], ident.t[:], On.b + ident.b, pb.b)
                cp("act", OnT.t[:].rearrange("p a b -> p (a b)"), pbb[:, 0:512], pb.b, OnT.b)
                yield 1.5
                for cc in range(2):
                    for kc in range(4):
                        mm(pY.t[:, cc * 512:(cc + 1) * 512], OnT.t[:, kc, :], wba.t[:, kc, cc * 512:(cc + 1) * 512],
                           kc == 0, kc == 3, OnT.b + wba.b, [pY.b[cc]])
                tt("dve", fB.t[:], pY.t[:], tq.t[:, 2560:3584], ALU.mult, pY.b + tq.b, fB.b)
                yield 2.5
                for h in range(4):
                    pb, _ = nextL()
                    ab = attb[h % 2]
                    rqT = fq.t[:, 1024 + h * 128:1024 + (h + 1) * 128]
                    rkT = fq.t[:, 1536 + h * 128:1536 + (h + 1) * 128]
                    rv = tq.t[:, 512 + h * 256:512 + (h + 1) * 256]
                    rkz = tq.t[:, h * 128:(h + 1) * 128]
                    zb = pY.b[h // 2]
                    mm(pb.t[:, 0:128], rkT, rqT, True, True, fq.b, pb.b)
                    tt("dve", ab.t[:], pb.t[:, 0:128], decT.t[:, h, :], ALU.mult, pb.b + decT.b, ab.b)
                    pb2, _ = nextL()
                    mm(pb2.t[:, 0:256], rkz, rv, True, True, tq.b, pb2.b)
                    mm(pY.t[:, h * 256:(h + 1) * 256], rqT, Rb.t[:, h, :], True, False, fq.b + [Rb.b[h]], [zb])
                    mm(pY.t[:, h * 256:(h + 1) * 256], ab.t[:], rv, False, True, ab.b + tq.b, [zb])
                    stt("dve", Rf.t[:, h, :], Rf.t[:, h, :], cdec[h], pb2.t[:, 0:256], ALU.mult, ALU.add,
                        [Rf.b[h]] + pb2.b, [Rf.b[h]])
                    cp("act", Rb.t[:, h, :], Rf.t[:, h, :], [Rf.b[h]], [Rb.b[h]])
                    yield 1.5
                for h in range(4):
                    S.op("dve", lambda e, h=h: e.bn_stats(out=gst.t[:, h, :], in_=pY.t[:, h * 256:(h + 1) * 256]),
                         [pY.b[h // 2]], gst.b)
                    S.op("dve", lambda e, h=h: e.bn_aggr(out=gmv.t[:, h, :], in_=gst.t[:, h, :]), gst.b, gmv.b)
                tt("dve", gsm.t[:, 0:4].unsqueeze(2), gmv.t[:, :, 1:2], epsg.t[:].unsqueeze(2), ALU.add,
                   gmv.b + epsg.b, gsm.b)
                act(gsm.t[:, 4:8], gsm.t[:, 0:4], AF.Ln, gsm.b, gsm.b)
                act(gsm.t[:, 8:12], gsm.t[:, 4:8], AF.Exp, gsm.b, gsm.b, scale=-0.5)
                stt("dve", gsm.t[:, 12:16].unsqueeze(2), gmv.t[:, :, 0:1], -1.0, gsm.t[:, 8:12].unsqueeze(2),
                    ALU.mult, ALU.mult, gmv.b + gsm.b, gsm.b)
                yield 2.0
                for h in range(4):
                    act(fA.t[:, h * 256:(h + 1) * 256], pY.t[:, h * 256:(h + 1) * 256], AF.Identity,
                        [pY.b[h // 2]] + gsm.b, fA.b, bias=gsm.t[:, 12 + h:13 + h], scale=gsm.t[:, 8 + h:9 + h])
                tt("dve", ub.t[:], fA.t[:], tq.t[:, 1536:2560], ALU.mult, fA.b + tq.b, ub.b)
                pb, pbb = nextL()
                for kc in range(8):
                    tr(pbb[:, kc * 128:(kc + 1) * 128], ub.t[:, kc * 128:(kc + 1) * 128], ident.t[:], ub.b + ident.b, pb.b)
                for kc in range(8):
                    act(uT.t[:, kc, :], pbb[:, kc * 128:(kc + 1) * 128], AF.Copy, pb.b + ggnT.b, uT.b,
                        scale=ggnT.t[:, kc:kc + 1])
                yield 3.0
                for cc in range(2):
                    for kc in range(8):
                        mm(pY.t[:, cc * 512:(cc + 1) * 512], uT.t[:, kc, :], wbr.t[:, kc, cc * 512:(cc + 1) * 512],
                           kc == 0, kc == 7, uT.b + wbr.b, [pY.b[cc]])
                tt("dve", fA.t[:], pY.t[:], tq.t[:, 3584:4608], ALU.mult, pY.b + tq.b, fA.b)
                tt("dve", ub.t[:], fB.t[:], fA.t[:], ALU.add, fB.b + fA.b, ub.b)
                yield 4.0
                pb, pbb = nextL()
                for kc in range(8):
                    tr(pbb[:, kc * 128:(kc + 1) * 128], ub.t[:, kc * 128:(kc + 1) * 128], ident.t[:], ub.b + ident.b, pb.b)
                cp("act", uT.t[:].rearrange("p a b -> p (a b)"), pbb[:, :], pb.b, uT.b)
                for cc in range(2):
                    for kc in range(8):
                        mm(pY.t[:, cc * 512:(cc + 1) * 512], uT.t[:, kc, :], wo.t[:, kc, cc * 512:(cc + 1) * 512],
                           kc == 0, kc == 7, uT.b + wo.b, [pY.b[cc]])
                yield 4.0
                act(ub.t[:], pY.t[:], AF.Square, pY.b, ub.b + sm2.b, accum=sm2.t[:, 0:1])
                rstd_from_ss(sm2.t[:, 0:1], sm2.t[:, 1:2], sm2.t[:, 2:3], 1.0 / D, sm2.b, sm2.b)
                stt("dve", fB.t[:], pY.t[:], sm2.t[:, 2:3], g2.t[:], ALU.mult, ALU.mult, pY.b + sm2.b + g2.b, fB.b)
                dma("sp", x1_d[i * 128:(i + 1) * 128, :], fB.t[:], "st_x1", fB.b, [])
                yield 2.5

            def drive(gy, gx):
                cy = cx = 0.0
                while gy is not None or gx is not None:
                    if gy is not None and (gx is None or cy <= cx):
                        try:
                            cy += next(gy)
                        except StopIteration:
                            gy = None
                    else:
                        try:
                            cx += next(gx)
                        except StopIteration:
                            gx = None

            drive(None, X_gen(0))
            for i in range(NT):
                drive(Y_gen(i), X_gen(i + 1) if i + 1 < NT else None)

            fence_b = fB.b
            allB = fence_b + kT.b + V.b + ikT.b + wba.b + wbr.b + wo.b + ggnT.b + g2.b + cmask.b + decT.b + epsg.b \
                + bigI.b + tmq.b
            for lst in (fmq, iwq, Th, PT, attb, maskT, pS, pL):
                for x_ in lst:
                    allB += x_.b
            for x_ in (Sc, maskb, jnkA, bsd, bsa, bs, wk, pw2, rden, On, OnT, Rf, Rb, gst, gmv, gsm, fA, fB, ub, uT,
                       sm2, ptX, pY):
                allB += x_.b
        for e in Sched.ENGS:
            S.wait_all(e, allB)

        if stop_after == "B":
            S.wait_all("sp", allB)
            S.emit()
            return nc

        GT = 2 if NT % 2 == 0 else 1
        NG = NT // GT
        GW = GT * 128
        with ExitStack() as sc:
            def sbc(name, shape, dt, n=1):
                return TB(sc.enter_context(nc.sbuf_tensor(name, list(shape), dt)), n)

            def psc_(name, shape, dt, n=1):
                return TB(sc.enter_context(nc.psum_tensor(name, list(shape), dt)), n, True)

            wup = sbc("wup", [128, 8, 2 * DFF], BF16, 8)
            wdn = sbc("wdn", [128, 22, D], BF16, 22)
            g3 = sbc("g3", [128, D], F32)
            g4 = sbc("g4", [128, D], F32)
            cw = sbc("cw", [128, 44, 3], F32)
            cb = sbc("cb", [128, 44], F32)
            hal = sbc("hal", [128, 44, 2], F32, 44)
            xc = [sbc(f"xc{j}", [128, D], F32) for j in range(2 * GT)]
            r1c = [sbc(f"r1c{j}", [128, D], F32) for j in range(2)]
            outc = [sbc(f"outc{j}", [128, D], F32) for j in range(2)]
            hbc = sbc("hbc", [128, D], BF16)
            junkc = sbc("junkc", [128, D], BF16)
            sm3 = [sbc(f"sm3_{j}", [128, 8], F32) for j in range(2)]
            sm4 = [sbc(f"sm4_{j}", [128, 8], F32) for j in range(2)]
            hTg2 = [sbc(f"hTg{j}", [128, 8, GW], BF16) for j in range(2)]
            hfix = [sbc(f"hfix{j}", [128, 4], F32) for j in range(3)]
            accg = [sbc(f"accg{j}", [128, GW], F32) for j in range(2)]
            accu = [sbc(f"accu{j}", [128, GW], F32) for j in range(2)]
            sg = accg
            aT = sbc("aT", [128, 22, GW], BF16, 22)
            pup = [psc_(f"pup{j}", [128, 512], F32) for j in range(3)]
            ptc = psc_("ptc", [128, 1024], BF16)
            pdn = psc_("pdn", [128, 1024], F32, 2)

            for kc in range(8):
                dma("pool", wup.t[:, kc, :], wup_d[kc * 128:(kc + 1) * 128, :], f"ld_wup{kc}", [], [wup.b[kc]])
            for fc in range(22):
                dma("pool", wdn.t[:, fc, :], wdn_d[fc * 128:(fc + 1) * 128, :], f"ld_wdn{fc % 4}", [], [wdn.b[fc]])
            cdma(g3.t[:], g3_d.broadcast_to([128, D]), g3.b)
            cdma(g4.t[:], g4_d.broadcast_to([128, D]), g4.b)
            cdma(cw.t[:], cw_d, cw.b)
            cdma(cb.t[:], cb_d, cb.b)
            S.op("pool", lambda e: e.memset(hal.t[:], 0.0), [], hal.b)

            ukc = {"k": 0}

            def driveC(gy, gx):
                cy = cx = 0.0
                while gy is not None or gx is not None:
                    if gy is not None and (gx is None or cy <= cx):
                        try:
                            cy += next(gy)
                        except StopIteration:
                            gy = None
                    else:
                        try:
                            cx += next(gx)
                        except StopIteration:
                            gx = None

            def PC_gen(g):
                hTg = hTg2[g % 2]
                for tl in range(GT):
                    i = g * GT + tl
                    a = i % 2
                    xt, smt, r1t = xc[(g % 2) * GT + tl], sm3[a], r1c[a]
                    dma("sp", xt.t[:], x_d[i * 128:(i + 1) * 128, :], f"ld_xc{(g % 2) * GT + tl}", [], xt.b)
                    dma("sp", r1t.t[:], x1_d[i * 128:(i + 1) * 128, :], f"ld_r1c{a}", fence_b, r1t.b)
                    tt("dve", xt.t[:], xt.t[:], r1t.t[:], ALU.add, xt.b + r1t.b, xt.b)
                    act(hbc.t[:], xt.t[:], AF.Square, xt.b, hbc.b + smt.b, accum=smt.t[:, 0:1])
                    rstd_from_ss(smt.t[:, 0:1], smt.t[:, 1:2], smt.t[:, 2:3], 1.0 / D, smt.b, smt.b)
                    stt("dve", hbc.t[:], xt.t[:], smt.t[:, 2:3], g3.t[:], ALU.mult, ALU.mult, xt.b + smt.b + g3.b, hbc.b)
                    yield 14.0
                    for kc in range(8):
                        tr(ptc.t[:, kc * 128:(kc + 1) * 128], hbc.t[:, kc * 128:(kc + 1) * 128], ident.t[:],
                           hbc.b + ident.b, ptc.b)
                    cp("act", hTg.t[:, :, tl * 128:(tl + 1) * 128],
                       ptc.t[:].rearrange("p (a b) -> p a b", a=8), ptc.b, hTg.b)
                    yield 1.0

            def MC_gen(g):
                hTg = hTg2[g % 2]
                for fc in range(22):
                    for which, c in ((0, fc), (1, fc + 22)):
                        pb = pup[ukc["k"] % 3]
                        ukc["k"] += 1
                        for kc in range(8):
                            mm(pb.t[:, 0:GW], wup.t[:, kc, c * 128:(c + 1) * 128], hTg.t[:, kc, :], kc == 0, kc == 7,
                               [wup.b[kc]] + hTg.b, pb.b)
                        accx = (accg if which == 0 else accu)[fc % 2]
                        hf = hfix[ukc["k"] % 3]
                        ts("pool", hf.t[:, 0:2], hal.t[:, c, 0:2], cw.t[:, c, 0:1], None, ALU.mult, None,
                           [hal.b[c]] + cw.b, hf.b)
                        ts("pool", hf.t[:, 2:3], hal.t[:, c, 1:2], cw.t[:, c, 1:2], None, ALU.mult, None,
                           [hal.b[c]] + cw.b, hf.b)
                        cp("act", hal.t[:, c, :], pb.t[:, GW - 2:GW], pb.b, [hal.b[c]])
                        act(accx.t[:], pb.t[:, 0:GW], AF.Identity, pb.b + cw.b + cb.b, accx.b,
                            bias=cb.t[:, c:c + 1], scale=cw.t[:, c, 2:3])
                        stt("dve", accx.t[:, 1:GW], pb.t[:, 0:GW - 1], cw.t[:, c, 1:2], accx.t[:, 1:GW],
                            ALU.mult, ALU.add, pb.b + cw.b + accx.b, accx.b)
                        stt("dve", accx.t[:, 2:GW], pb.t[:, 0:GW - 2], cw.t[:, c, 0:1], accx.t[:, 2:GW],
                            ALU.mult, ALU.add, pb.b + cw.b + accx.b, accx.b)
                        tt("pool", accx.t[:, 0:2], accx.t[:, 0:2], hf.t[:, 0:2], ALU.add, accx.b + hf.b, accx.b)
                        tt("pool", accx.t[:, 0:1], accx.t[:, 0:1], hf.t[:, 2:3], ALU.add, accx.b + hf.b, accx.b)
                        yield 0.9
                    ag, au = accg[fc % 2], accu[fc % 2]
                    act(ag.t[:], ag.t[:], AF.Silu, ag.b, ag.b)
                    tt("dve", aT.t[:, fc, :], ag.t[:], au.t[:], ALU.mult, ag.b + au.b, [aT.b[fc]])
                for tl in range(GT):
                    i = g * GT + tl
                    a = i % 2
                    for cc in range(2):
                        for fc in range(22):
                            mm(pdn.t[:, cc * 512:(cc + 1) * 512], aT.t[:, fc, tl * 128:(tl + 1) * 128],
                               wdn.t[:, fc, cc * 512:(cc + 1) * 512], fc == 0, fc == 21,
                               [aT.b[fc], wdn.b[fc]], [pdn.b[cc]])
                    smt = sm4[a]
                    act(junkc.t[:], pdn.t[:], AF.Square, pdn.b, junkc.b + smt.b, accum=smt.t[:, 4:5])
                    rstd_from_ss(smt.t[:, 4:5], smt.t[:, 5:6], smt.t[:, 6:7], 1.0 / D, smt.b, smt.b)
                    xrt, ot = xc[(g % 2) * GT + tl], outc[a]
                    stt("dve", ot.t[:], pdn.t[:], smt.t[:, 6:7], g4.t[:], ALU.mult, ALU.mult, pdn.b + smt.b + g4.b, ot.b)
                    tt("dve", ot.t[:], ot.t[:], xrt.t[:], ALU.add, ot.b + xrt.b, ot.b)
                    dma("sp", out_d[i * 128:(i + 1) * 128, :], ot.t[:], f"st_out{a}", ot.b, [])
                    yield 9.5

            driveC(None, PC_gen(0))
            for g in range(NG):
                driveC(MC_gen(g), PC_gen(g + 1) if g + 1 < NG else None)
            S.wait_all("sp", outc[0].b + outc[1].b)
        S.emit()
    return nc


def _consts():
    invf128 = 10000.0 ** (-(np.arange(64, dtype=np.float32) * 2.0 / 128)).astype(np.float32)
    invf64 = 10000.0 ** (-(np.arange(32, dtype=np.float32) * 2.0 / 64)).astype(np.float32)
    invf = np.concatenate([invf128, invf64]).astype(np.float32)
    c_invf = np.ascontiguousarray(np.broadcast_to(invf[None, :], (128, 96))).astype(np.float32)
    c_ident = np.eye(128, dtype=np.float32).astype(ml_dtypes.bfloat16)
    jj = np.arange(128)
    c_cmask = np.where(jj[None, :] <= jj[:, None], 0.0, NEG).astype(np.float32)
    gam = 1.0 - 2.0 ** (-5.0 - np.arange(4, dtype=np.float64))
    c = 128.0 ** -0.5
    decT = np.zeros((128, 4, 128), np.float64)
    for h in range(4):
        decT[:, h, :] = (c * gam[h] ** (-(jj[:, None] + 1.0))) * (jj[:, None] <= jj[None, :])
    zc = c * gam[None, :] ** (127.0 - jj[:, None])
    xi = gam[None, :] ** (jj[:, None] + 1.0)
    epsg = EPS / xi ** 2
    return dict(c_invf=c_invf, c_ident=c_ident, c_cmask=c_cmask, c_decT=decT.astype(np.float32),
                c_zc=zc.astype(np.float32), c_epsg=epsg.astype(np.float32))


def make_in_maps(inputs, NT=NTF, cores=8):
    T = NT * 128
    f = lambda a: np.ascontiguousarray(np.asarray(a, dtype=np.float32))
    cw = f(inputs["conv_w"])[0]
    cw_l = np.ascontiguousarray(cw.reshape(3, 44, 128).transpose(2, 1, 0))
    cb_l = np.ascontiguousarray(f(inputs["conv_b"])[0].reshape(44, 128).T)
    shared = dict(
        g_pre_mix=f(inputs["norm_pre_mix"]), w_in=f(inputs["w_in"])[0], w_br_attn=f(inputs["w_br_attn"])[0],
        w_br_ret=f(inputs["w_br_ret"])[0],
        ggnT=np.ascontiguousarray(f(inputs["ret_gn_gain"])[0].reshape(8, 128).T), w_out=f(inputs["w_out"])[0],
        g_post_mix=f(inputs["norm_post_mix"]), g_pre_ffn=f(inputs["norm_pre_ffn"]),
        w_ffn_up=f(inputs["w_ffn_up"])[0], conv_w=cw_l, conv_b=cb_l, w_ffn_down=f(inputs["w_ffn_down"])[0],
        g_post_ffn=f(inputs["norm_post_ffn"]),
    )
    shared.update(_consts())
    x = np.asarray(inputs["x"], dtype=np.float32)
    pos = np.asarray(inputs["positions"]).astype(np.int32)
    maps = []
    for b in range(cores):
        m = dict(shared)
        m["x"] = np.ascontiguousarray(x[b, :T])
        m["pos"] = np.ascontiguousarray(pos[b, :T].reshape(NT, 128).T)
        maps.append(m)
    return maps


_NC_CACHE = {}


def kernel(**inputs):
    if "full" not in _NC_CACHE:
        _NC_CACHE["full"] = build_program(NTF)
    nc = _NC_CACHE["full"]
    maps = make_in_maps(inputs, NTF, 8)
    res = run_bass_kernel_spmd(nc, maps, core_ids=list(range(8)))
    out = np.stack([np.asarray(r["out"], dtype=np.float32) for r in res.results], axis=0)
    return out
```

```python
import numpy as np
import ml_dtypes
from contextlib import ExitStack
import concourse.bass as bass
import concourse.mybir as mybir
from concourse.bass_utils import run_bass_kernel_spmd

F32 = mybir.dt.float32
BF16 = mybir.dt.bfloat16
I32 = mybir.dt.int32
ALU = mybir.AluOpType
AF = mybir.ActivationFunctionType
AX = mybir.AxisListType

D = 1024
T_FULL = 4096
NTF = 32
INW = 7240
DFF = 2816
EPS = 1e-6
TOPK = 256
NEG = -1.0e30
NBIS = 20
TWO_PI = float(2 * np.pi)


class Buf:
    __slots__ = ("wr", "rd", "excl")

    def __init__(self, excl=False):
        self.wr = None
        self.rd = []
        self.excl = excl


class Sched:
    ENGS = ("pe", "dve", "act", "pool", "sp")

    def __init__(self, nc, stack):
        self.nc = nc
        self.stack = stack
        self.lists = {e: [] for e in self.ENGS}
        self.sems = {}
        self.count = {}
        self.known = {e: {} for e in self.ENGS}
        for e in self.ENGS:
            self._newsem("E_" + e)

    def _newsem(self, key):
        s = self.stack.enter_context(self.nc.semaphore(key))
        self.sems[key] = s
        self.count[key] = 0

    def _deps(self, eng, reads, writes):
        deps = {}
        own = "E_" + eng

        def add(ev):
            if ev is None:
                return
            k, v = ev
            if eng == "pe" and k == own:
                return
            if deps.get(k, 0) < v:
                deps[k] = v
        for b in reads:
            add(b.wr)
        for b in writes:
            add(b.wr)
            for ev in b.rd:
                add(ev)
        waits = []
        kn = self.known[eng]
        for k, v in deps.items():
            if kn.get(k, 0) < v:
                kn[k] = v
                waits.append((k, v))
        return waits

    def _post(self, ev, reads, writes):
        for b in reads:
            b.rd.append(ev)
        for b in writes:
            b.wr = ev
            b.rd = []

    @staticmethod
    def _split(reads, writes):
        ex = [b for b in reads if b.excl]
        if not ex:
            return reads, writes
        return [b for b in reads if not b.excl], list(writes) + ex

    def op(self, eng, fn, reads=(), writes=()):
        reads, writes = self._split(reads, writes)
        waits = self._deps(eng, reads, writes)
        key = "E_" + eng
        self.count[key] += 1
        ev = (key, self.count[key])
        self.lists[eng].append((waits, fn, key, 1, self.count[key]))
        self._post(ev, reads, writes)

    def dma(self, eng, fn, semkey, reads=(), writes=()):
        if semkey not in self.sems:
            self._newsem(semkey)
        waits = self._deps(eng, reads, writes)
        self.count[semkey] += 16
        ev = (semkey, self.count[semkey])
        self.lists[eng].append((waits, fn, semkey, 16, None))
        self._post(ev, reads, writes)

    def wait_all(self, eng, bufs):
        waits = self._deps(eng, bufs, bufs)
        self.lists[eng].append((waits, None, None, 0, None))

    def emit(self):
        sems = self.sems
        lists = self.lists
        needed = {}
        for e in self.ENGS:
            for waits, fn, key, inc, seq in lists[e]:
                for k, v in waits:
                    if k.startswith("E_"):
                        needed.setdefault(k, set()).add(v)
        rank = {}
        for e in self.ENGS:
            key = "E_" + e
            need = needed.get(key, set())
            m = {}
            r = 0
            for waits, fn, k, inc, seq in lists[e]:
                if seq is not None and seq in need:
                    r += 1
                    m[seq] = r
            rank[key] = m

        def replay(e, items):
            for waits, fn, key, inc, seq in items:
                for k, v in waits:
                    if k.startswith("E_"):
                        v = rank[k][v]
                    e.wait_ge(sems[k], v)
                if fn is not None:
                    ins = fn(e)
                    if seq is None:
                        ins.then_inc(sems[key], inc)
                    elif seq in rank[key]:
                        ins.then_inc(sems[key], 1)

        with self.nc.Block() as block:
            @block.tensor
            def _(e):
                replay(e, lists["pe"])

            @block.vector
            def _(e):
                replay(e, lists["dve"])

            @block.scalar
            def _(e):
                replay(e, lists["act"])

            @block.gpsimd
            def _(e):
                replay(e, lists["pool"])

            @block.sync
            def _(e):
                replay(e, lists["sp"])
        self.stats = {k: len(v) for k, v in rank.items()}


class TB:
    def __init__(self, t, n=1, excl=False):
        self.t = t
        self.b = [Buf(excl) for _ in range(n)]


def build_program(NT=NTF, dbg=False, stop_after=None):
    nc = bass.Bass("TRN2", target_bir_lowering=False)
    T = NT * 128

    def din(name, shape, dt=F32):
        return nc.dram_tensor(name, list(shape), dt, kind="ExternalInput").ap()

    def dscr(name, shape, dt):
        return nc.dram_tensor(name, list(shape), dt, kind=("ExternalOutput" if dbg else "Internal")).ap()

    x_d = din("x", [T, D])
    pos_d = din("pos", [128, NT], I32)
    g1_d = din("g_pre_mix", [1, D])
    w_in_d = din("w_in", [D, INW])
    wba_d = din("w_br_attn", [512, D])
    wbr_d = din("w_br_ret", [D, D])
    ggnT_d = din("ggnT", [128, 8])
    wo_d = din("w_out", [D, D])
    g2_d = din("g_post_mix", [1, D])
    g3_d = din("g_pre_ffn", [1, D])
    wup_d = din("w_ffn_up", [D, 2 * DFF])
    cw_d = din("conv_w", [128, 44, 3])
    cb_d = din("conv_b", [128, 44])
    wdn_d = din("w_ffn_down", [DFF, D])
    g4_d = din("g_post_ffn", [1, D])
    invf_d = din("c_invf", [128, 96])
    ident_d = din("c_ident", [128, 128], BF16)
    cmask_d = din("c_cmask", [128, 128])
    decT_d = din("c_decT", [128, 4, 128])
    zc_d = din("c_zc", [128, 4])
    epsg_d = din("c_epsg", [128, 4])
    out_d = nc.dram_tensor("out", [T, D], F32, kind="ExternalOutput").ap()

    fm_d = dscr("s_fm", [NT, 128, 2048], BF16)
    kT_d = dscr("s_kT", [NT, 128, 512], BF16)
    ikT_d = dscr("s_ikT", [64, T], BF16)
    iw_d = dscr("s_iw", [T, 8], F32)
    v_d = dscr("s_v", [T, 520], BF16)
    tm_d = dscr("s_tm", [T, 4608], BF16)
    x1_d = dscr("s_x1", [T, D], F32)

    cdec = [float((1.0 - 2.0 ** (-5.0 - h)) ** 128) for h in range(4)]

    with ExitStack() as st:
        S = Sched(nc, st)

        def sb(name, shape, dt, n=1):
            return TB(st.enter_context(nc.sbuf_tensor(name, list(shape), dt)), n)

        def ps(name, shape, dt, n=1):
            return TB(st.enter_context(nc.psum_tensor(name, list(shape), dt)), n, True)

        def mm(out, lhsT, rhs, start, stop, reads, writes):
            S.op("pe", lambda e: e.matmul(out, lhsT=lhsT, rhs=rhs, start=start, stop=stop), reads, writes)

        def tr(out, in_, ident_ap, reads, writes):
            S.op("pe", lambda e: e.transpose(out=out, in_=in_, identity=ident_ap), reads, writes)

        def act(out, in_, func, reads, writes, bias=None, scale=None, accum=None, saturate=None):
            kw = {}
            if saturate is not None:
                kw["saturate"] = saturate
            if bias is not None:
                kw["bias"] = bias
            if scale is not None:
                kw["scale"] = scale
            if accum is not None:
                kw["accum_out"] = accum
            S.op("act", lambda e: e.activation(out=out, in_=in_, func=func, **kw), reads, writes)

        def tt(eng, out, in0, in1, op, reads, writes):
            S.op(eng, lambda e: e.tensor_tensor(out=out, in0=in0, in1=in1, op=op), reads, writes)

        def ts(eng, out, in0, s1, s2, op0, op1, reads, writes, accum=None):
            kw = {}
            if op1 is not None:
                kw["op1"] = op1
            if accum is not None:
                kw["accum_out"] = accum
            S.op(eng, lambda e: e.tensor_scalar(out=out, in0=in0, scalar1=s1, scalar2=s2, op0=op0, **kw), reads, writes)

        def stt(eng, out, in0, scalar, in1, op0, op1, reads, writes):
            S.op(eng, lambda e: e.scalar_tensor_tensor(out=out, in0=in0, scalar=scalar, in1=in1, op0=op0, op1=op1),
                 reads, writes)

        def cp(eng, out, in_, reads, writes):
            if eng == "act":
                S.op("act", lambda e: e.activation(out=out, in_=in_, func=AF.Copy), reads, writes)
            else:
                S.op(eng, lambda e: e.tensor_copy(out=out, in_=in_), reads, writes)

        def dma(eng, out, in_, key, reads, writes):
            S.dma(eng, lambda e: e.dma_start(out=out, in_=in_), key, reads, writes)

        chain = Buf()

        def cdma(out, in_, writes):
            S.dma("sp", lambda e: e.dma_start(out=out, in_=in_), "ld_misc", [], list(writes) + [chain])

        ident = sb("ident", [128, 128], BF16)
        epsc = sb("epsc", [128, 1], F32)
        cdma(ident.t[:], ident_d, ident.b)
        S.op("dve", lambda e: e.memset(epsc.t[:], EPS), [], epsc.b)

        def rstd_from_ss(ss_ap, ln_ap, out_ap, inv_n, reads, writes):
            act(ln_ap, ss_ap, AF.Ln, list(reads) + epsc.b, writes, bias=epsc.t[:, 0:1], scale=inv_n)
            act(out_ap, ln_ap, AF.Exp, writes, writes, scale=-0.5)

        with ExitStack() as sa:
            def sba(name, shape, dt, n=1):
                return TB(sa.enter_context(nc.sbuf_tensor(name, list(shape), dt)), n)

            def psa(name, shape, dt, n=1):
                return TB(sa.enter_context(nc.psum_tensor(name, list(shape), dt)), n, True)

            win = sba("win", [128, 8, INW], BF16, 8)
            gbc = sba("gbc", [128, D], F32)
            invf = sba("invf", [128, 96], F32)
            posi = sba("posi", [128, NT], I32)
            posf = sba("posf", [128, NT], F32)
            rt = [sba(f"rt{j}", [128, 3, 96], F32) for j in range(2)]
            ang = sba("ang", [128, 96], F32)
            angk = sba("angk", [128, 96], F32)
            angi = sba("angi", [128, 96], I32)
            zc = sba("zc", [128, 4], F32)
            xin = [sba(f"xin{j}", [128, D], F32) for j in range(2)]
            junkb = sba("junkb", [128, D], BF16)
            sm = [sba(f"smA{j}", [128, 8], F32) for j in range(2)]
            hb = sba("hb", [128, D], BF16)
            hT = [sba(f"hT{j}", [128, 8, 128], BF16) for j in range(2)]
            t1 = [sba(f"t1_{j}", [128, 512], F32) for j in range(2)]
            t2 = [sba(f"t2_{j}", [128, 512], F32) for j in range(2)]
            rob = [sba(f"rob{j}", [128, 512], BF16) for j in range(3)]
            tm = [sba(f"tmA{j}", [128, 4608], BF16, 9) for j in range(2)]
            fm = [sba(f"fmA{j}", [128, 2048], BF16, 4) for j in range(2)]
            kTs = [sba(f"kTs{j}", [128, 512], BF16) for j in range(2)]
            vs = [sba(f"vs{j}", [128, 8, 65], BF16) for j in range(2)]
            iks = [sba(f"iks{j}", [64, 128], BF16) for j in range(2)]
            iws = [sba(f"iws{j}", [128, 8], F32) for j in range(2)]
            pj = [psa(f"pj{j}", [128, 512], F32) for j in range(5)]
            ptr = [psa(f"ptrA{j}", [128, 1024], BF16) for j in range(2)]

            for kc in range(8):
                dma("pool", win.t[:, kc, :], w_in_d[kc * 128:(kc + 1) * 128, :], f"ld_win{kc}", [], [win.b[kc]])
            cdma(gbc.t[:], g1_d.broadcast_to([128, D]), gbc.b)
            cdma(invf.t[:], invf_d, invf.b)
            cdma(posi.t[:], pos_d, posi.b)
            cdma(zc.t[:], zc_d, zc.b)
            for j in range(2):
                S.op("pool", lambda e, j=j: e.memset(vs[j].t[:], 1.0), [], vs[j].b)

            cp("dve", posf.t[:], posi.t[:], posi.b, posf.b)

            def range_reduce(shift):
                ts("dve", angk.t[:], ang.t[:], shift, 1.0 / TWO_PI, ALU.add, ALU.mult, ang.b, angk.b)
                cp("dve", angi.t[:], angk.t[:], angk.b, angi.b)
                cp("dve", angk.t[:], angi.t[:], angi.b, angk.b)
                ts("dve", angk.t[:], angk.t[:], -TWO_PI, shift, ALU.mult, ALU.add, angk.b, angk.b)
                tt("dve", angk.t[:], angk.t[:], ang.t[:], ALU.add, angk.b + ang.b, angk.b)
                ts("dve", angk.t[:], angk.t[:], float(np.pi), -float(np.pi), ALU.min, ALU.max, angk.b, angk.b)

            def make_tables(i):
                r_ = rt[i % 2]
                ts("dve", ang.t[:], invf.t[:], posf.t[:, i:i + 1], None, ALU.mult, None, invf.b + posf.b, ang.b)
                range_reduce(float(np.pi / 2))
                act(r_.t[:, 0, :], angk.t[:], AF.Sin, angk.b, r_.b)
                range_reduce(0.0)
                act(r_.t[:, 2, :], angk.t[:], AF.Sin, angk.b, r_.b)
                ts("dve", r_.t[:, 1, :], r_.t[:, 2, :], -1.0, None, ALU.mult, None, r_.b, r_.b)

            def rotary(i, src, nh, half, toff, dst, reads, writes, k):
                a1, a2 = t1[k % 2], t2[k % 2]
                rti = rt[i % 2]
                w = nh * 2 * half
                s4 = src.rearrange("p (h t j) -> p h t j", h=nh, t=2, j=half)
                o1 = a1.t[:, 0:w].rearrange("p (h t j) -> p h t j", h=nh, t=2, j=half)
                o2 = a2.t[:, 0:w].rearrange("p (h t j) -> p h t j", h=nh, t=2, j=half)
                cosb = rti.t[:, 0, toff:toff + half].unsqueeze(1).unsqueeze(1).broadcast_to([128, nh, 2, half])
                nsin = rti.t[:, 1, toff:toff + half].unsqueeze(1).broadcast_to([128, nh, half])
                psin = rti.t[:, 2, toff:toff + half].unsqueeze(1).broadcast_to([128, nh, half])
                tt("dve", o1, s4, cosb, ALU.mult, list(reads) + rti.b, a1.b)
                tt("dve", o2[:, :, 0, :], s4[:, :, 1, :], nsin, ALU.mult, list(reads) + rti.b, a2.b)
                tt("dve", o2[:, :, 1, :], s4[:, :, 0, :], psin, ALU.mult, list(reads) + rti.b, a2.b)
                tt("dve", dst, a1.t[:, 0:w], a2.t[:, 0:w], ALU.add, a1.b + a2.b, writes)

            ncol = [512] * 4 + [72] + [512] * 10
            coff = [0]
            for c in ncol:
                coff.append(coff[-1] + c)
            rotc = {"k": 0}

            def drive(gy, gx):
                cy = cx = 0.0
                while gy is not None or gx is not None:
                    if gy is not None and (gx is None or cy <= cx):
                        try:
                            cy += next(gy)
                        except StopIteration:
                            gy = None
                    else:
                        try:
                            cx += next(gx)
                        except StopIteration:
                            gx = None

            def PA_gen(i):
                a = i % 2
                xt, smt, hTt = xin[a], sm[a], hT[a]
                dma("sp", xt.t[:], x_d[i * 128:(i + 1) * 128, :], f"ld_x{a}", [], xt.b)
                make_tables(i)
                act(junkb.t[:], xt.t[:], AF.Square, xt.b, junkb.b + smt.b, accum=smt.t[:, 0:1])
                rstd_from_ss(smt.t[:, 0:1], smt.t[:, 1:2], smt.t[:, 2:3], 1.0 / D, smt.b, smt.b)
                stt("dve", hb.t[:], xt.t[:], smt.t[:, 2:3], gbc.t[:], ALU.mult, ALU.mult,
                    xt.b + smt.b + gbc.b, hb.b)
                yield 12.0
                pt = ptr[0]
                for kc in range(8):
                    tr(pt.t[:, kc * 128:(kc + 1) * 128], hb.t[:, kc * 128:(kc + 1) * 128], ident.t[:],
                       hb.b + ident.b, pt.b)
                cp("act", hTt.t[:].rearrange("p a b -> p (a b)"), pt.t[:], pt.b, hTt.b)
                yield 1.0

            def MA_gen(i):
                a = i % 2
                hTt, tmt, fmt = hT[a], tm[a], fm[a]
                pend = []

                def flush(item):
                    c, r = item
                    pt = ptr[1]
                    if c == 4:
                        tr(pt.t[0:64, 0:128], r.t[:, 0:64], ident.t[:], r.b + ident.b, pt.b)
                        cp("act", iks[a].t[:], pt.t[0:64, 0:128], pt.b, iks[a].b)
                        dma("sp", ikT_d[:, i * 128:(i + 1) * 128], iks[a].t[:], f"st_ik{a}", iks[a].b, [])
                        return
                    for q in range(4):
                        tr(pt.t[:, q * 128:(q + 1) * 128], r.t[:, q * 128:(q + 1) * 128], ident.t[:],
                           r.b + ident.b, pt.b)
                    if c == 1:
                        cp("act", kTs[a].t[:], pt.t[:, 0:512], pt.b, kTs[a].b)
                        dma("sp", kT_d[i], kTs[a].t[:], f"st_kT{a}", kTs[a].b, [])
                    else:
                        slot = {0: 0, 3: 1, 5: 2, 6: 3}[c]
                        cp("act", fmt.t[:, slot * 512:(slot + 1) * 512], pt.t[:, 0:512], pt.b, [fmt.b[slot]])

                for c in range(15):
                    pb = pj[c % 5]
                    w = ncol[c]
                    for kc in range(8):
                        mm(pb.t[:, 0:w], hTt.t[:, kc, :], win.t[:, kc, coff[c]:coff[c] + w], kc == 0, kc == 7,
                           hTt.b + [win.b[kc]], pb.b)
                    if c in (0, 1, 3, 5, 6):
                        r = rob[rotc["k"] % 3]
                        if c in (5, 6):
                            rotary(i, pb.t[:, :], 4, 64, 0, r.t[:, 0:512], pb.b, r.b, rotc["k"])
                        else:
                            rotary(i, pb.t[:, :], 8, 32, 64, r.t[:, 0:512], pb.b, r.b, rotc["k"])
                        rotc["k"] += 1
                        if c == 6:
                            tt("dve", tmt.t[:, 0:512].rearrange("p (h d) -> p h d", h=4),
                               r.t[:].rearrange("p (h d) -> p h d", h=4),
                               zc.t[:].unsqueeze(2).broadcast_to([128, 4, 128]), ALU.mult,
                               r.b + zc.b, [tmt.b[0]])
                        pend.append((c, r))
                    elif c == 2:
                        cp("act", vs[a].t[:, :, 0:64], pb.t[:, :].rearrange("p (h d) -> p h d", h=8), pb.b, vs[a].b)
                        dma("sp", v_d[i * 128:(i + 1) * 128, :], vs[a].t[:].rearrange("p h d -> p (h d)"),
                            f"st_v{a}", vs[a].b, [])
                    elif c == 4:
                        r = rob[rotc["k"] % 3]
                        rotary(i, pb.t[:, 0:64], 1, 32, 64, r.t[:, 0:64], pb.b, r.b, rotc["k"])
                        rotc["k"] += 1
                        ts("dve", iws[a].t[:], pb.t[:, 64:72], float(8 ** -0.5 * 64 ** -0.5), None, ALU.mult, None,
                           pb.b, iws[a].b)
                        dma("sp", iw_d[i * 128:(i + 1) * 128, :], iws[a].t[:], f"st_iw{a}", iws[a].b, [])
                        pend.append((c, r))
                    elif c in (7, 8):
                        o = 512 + (c - 7) * 512
                        cp("act", tmt.t[:, o:o + 512], pb.t[:, :], pb.b, [tmt.b[1 + c - 7]])
                    elif c in (9, 10):
                        o = 1536 + (c - 9) * 512
                        act(tmt.t[:, o:o + 512], pb.t[:, :], AF.Silu, pb.b, [tmt.b[3 + c - 9]])
                    else:
                        o = 2560 + (c - 11) * 512
                        act(tmt.t[:, o:o + 512], pb.t[:, :], AF.Sigmoid, pb.b, [tmt.b[5 + c - 11]])
                    while pend and pend[0][0] <= c - 2:
                        flush(pend.pop(0))
                    yield 1.7
                while pend:
                    flush(pend.pop(0))
                dma("sp", fm_d[i], fmt.t[:], f"st_fm{a}", fmt.b, [])
                dma("sp", tm_d[i * 128:(i + 1) * 128, :], tmt.t[:], f"st_tm{a}", tmt.b, [])
                yield 0.5

            drive(None, PA_gen(0))
            for i in range(NT):
                drive(MA_gen(i), PA_gen(i + 1) if i + 1 < NT else None)
            fence_a = []
            for a in range(2):
                fence_a += tm[a].b + fm[a].b + kTs[a].b + vs[a].b + iks[a].b + iws[a].b
            allA = fence_a + win.b + gbc.b + invf.b + posi.b + posf.b + rt[0].b + rt[1].b + ang.b + angk.b + angi.b + zc.b + \
                xin[0].b + xin[1].b + junkb.b + sm[0].b + sm[1].b + hb.b + hT[0].b + hT[1].b + \
                t1[0].b + t1[1].b + t2[0].b + t2[1].b + rob[0].b + rob[1].b + rob[2].b + \
                [p.b[0] for p in pj] + [p.b[0] for p in ptr]
        for e in Sched.ENGS:
            S.wait_all(e, allA)

        if stop_after == "A":
            S.wait_all("sp", allA)
            S.emit()
            return nc

        with ExitStack() as sbk:
            def sbb(name, shape, dt, n=1):
                return TB(sbk.enter_context(nc.sbuf_tensor(name, list(shape), dt)), n)

            def psb(name, shape, dt, n=1):
                return TB(sbk.enter_context(nc.psum_tensor(name, list(shape), dt)), n, True)

            NGC = (NT + 7) // 8
            kT = sbb("kTc", [128, NT, 512], BF16, NGC)
            V = sbb("Vc", [128, NT, 520], BF16, NGC)
            ikT = sbb("ikTc", [128, T], BF16, NGC)
            wba = sbb("wba", [128, 4, D], BF16)
            wbr = sbb("wbr", [128, 8, D], BF16)
            wo = sbb("wo", [128, 8, D], BF16)
            ggnT = sbb("ggnT_sb", [128, 8], F32)
            g2 = sbb("g2", [128, D], F32)
            cmask = sbb("cmask", [128, 128], F32)
            decT = sbb("decT", [128, 4, 128], F32)
            epsg = sbb("epsg", [128, 4], F32)
            bigI = sbb("bigI", [128, 128], BF16)
            fmq = [sbb(f"fmq{j}", [128, 2048], BF16) for j in range(2)]
            tmq = sbb("tmq0", [128, 4608], BF16)
            iwq = [sbb(f"iwq{j}", [128, 8], F32) for j in range(2)]
            Th = [sbb(f"Th{j}", [128, 512], BF16) for j in range(3)]
            PT = [sbb(f"PT{j}", [128, 512], BF16) for j in range(3)]
            Sc = sbb("Sc", [128, T], F32)
            maskb = sbb("maskb", [128, T], BF16)
            jnkA = sbb("jnkA", [128, max(128, T - ((T * 9 // 20) // 128) * 128)], mybir.dt.float8e4)
            maskT = [sbb(f"maskT{j}", [128, NT, 128], BF16) for j in range(2)]
            bs = sbb("bs", [128, 8], F32)
            bsd = sbb("bsd", [128, 2], F32)
            bsa = sbb("bsa", [128, 2], F32)
            wk = sbb("wk", [128, NBIS + 1], F32)
            pw2 = sbb("pw2", [128, NBIS + 1], F32)
            rden = sbb("rden", [128, 8], F32)
            On = sbb("On", [128, 512], BF16)
            OnT = sbb("OnT", [128, 4, 128], BF16)
            attb = [sbb(f"attb{j}", [128, 128], BF16) for j in range(2)]
            Rf = sbb("Rf", [128, 4, 256], F32, 4)
            Rb = sbb("Rb", [128, 4, 256], BF16, 4)
            gst = sbb("gst", [128, 4, 6], F32)
            gmv = sbb("gmv", [128, 4, 2], F32)
            gsm = sbb("gsm", [128, 16], F32)
            fA = sbb("fA", [128, D], F32)
            fB = sbb("fB", [128, D], F32)
            ub = sbb("ub", [128, D], BF16)
            uT = sbb("uT", [128, 8, 128], BF16)
            sm2 = sbb("sm2", [128, 8], F32)
            pS = [psb(f"pS{j}", [128, 512], F32) for j in range(3)]
            ptX = psb("ptX", [128, 1024], BF16)
            pL = [psb(f"pL{j}", [128, 512], F32) for j in range(2)]
            pLb = [p.t.bitcast(BF16) for p in pL]
            pY = psb("pY", [128, 1024], F32, 2)

            for kc in range(4):
                dma("pool", wba.t[:, kc, :], wba_d[kc * 128:(kc + 1) * 128, :], "ld_wba", [], wba.b)
            for kc in range(8):
                dma("pool", wbr.t[:, kc, :], wbr_d[kc * 128:(kc + 1) * 128, :], "ld_wbr", [], wbr.b)
            for kc in range(8):
                dma("pool", wo.t[:, kc, :], wo_d[kc * 128:(kc + 1) * 128, :], "ld_wo", [], wo.b)
            cdma(ggnT.t[:], ggnT_d, ggnT.b)
            cdma(g2.t[:], g2_d.broadcast_to([128, D]), g2.b)
            cdma(cmask.t[:], cmask_d, cmask.b)
            cdma(decT.t[:], decT_d, decT.b)
            cdma(epsg.t[:], epsg_d, epsg.b)
            ts("dve", bigI.t[:], ident.t[:], 30000.0, None, ALU.mult, None, ident.b, bigI.b)
            for gc in range(NGC):
                t0, t1_ = gc * 8, min(NT, gc * 8 + 8)
                dma("sp", ikT.t[0:64, t0 * 128:t1_ * 128], ikT_d[:, t0 * 128:t1_ * 128], f"ld_ikA{gc}", fence_a,
                    [ikT.b[gc]])
                dma("sp", ikT.t[64:128, t0 * 128:t1_ * 128], ikT_d[:, t0 * 128:t1_ * 128], f"ld_ikB{gc}", fence_a,
                    [ikT.b[gc]])
                dma("sp", kT.t[:, t0:t1_, :], kT_d[t0:t1_].rearrange("t p c -> p t c"), f"ld_kT{gc}", fence_a,
                    [kT.b[gc]])
                dma("sp", V.t[:, t0:t1_, :], v_d[t0 * 128:t1_ * 128, :].rearrange("(t p) c -> p t c", p=128),
                    f"ld_V{gc}", fence_a, [V.b[gc]])
            for h in range(4):
                S.op("dve", lambda e, h=h: e.memset(Rf.t[:, h, :], 0.0), [], [Rf.b[h]])
                S.op("dve", lambda e, h=h: e.memset(Rb.t[:, h, :], 0.0), [], [Rb.b[h]])
            for k in range(NBIS + 1):
                kk = min(k, NBIS - 1)
                S.op("dve", lambda e, k=k, kk=kk: e.memset(pw2.t[:, k:k + 1], float(2.0 ** -(kk + 1))), [], pw2.b)

            cnt = {"s": 0, "l": 0, "th": 0, "pt": 0}

            def X_gen(i):
                a = i % 2
                fq, wq, mT_ = fmq[a], iwq[a], maskT[a]
                nkt = i + 1
                nk = nkt * 128
                dma("sp", fq.t[:], fm_d[i], f"ld_fmq{a}", fence_a, fq.b)
                dma("sp", wq.t[:], iw_d[i * 128:(i + 1) * 128, :], f"ld_iwq{a}", fence_a, wq.b)
                nch = (nkt + 3) // 4
                for c in range(nch):
                    k0 = c * 512
                    wc = min(512, nk - k0)
                    kts = list(range(c * 4, min(c * 4 + 4, nkt)))
                    for h in range(8):
                        hp, pr = h % 2, h // 2
                        pb = pS[cnt["s"] % 3]
                        cnt["s"] += 1
                        thb = Th[cnt["th"] % 3]
                        cnt["th"] += 1
                        mm(pb.t[:, 0:wc], fq.t[hp * 64:(hp + 1) * 64, 512 + pr * 128:512 + (pr + 1) * 128],
                           ikT.t[hp * 64:(hp + 1) * 64, k0:k0 + wc], True, True,
                           fq.b + [ikT.b[k // 8] for k in kts], pb.b)
                        act(thb.t[:, 0:wc], pb.t[:, 0:wc], AF.Relu, pb.b, thb.b)
                        if h == 0:
                            ts("dve", Sc.t[:, k0:k0 + wc], thb.t[:, 0:wc], wq.t[:, 0:1], None, ALU.mult, None,
                               thb.b + wq.b, Sc.b)
                        else:
                            stt("dve", Sc.t[:, k0:k0 + wc], thb.t[:, 0:wc], wq.t[:, h:h + 1], Sc.t[:, k0:k0 + wc],
                                ALU.mult, ALU.add, thb.b + wq.b + Sc.b, Sc.b)
                        yield 0.25
                    if kts[-1] == i:
                        d0 = k0 + wc - 128
                        tt("dve", Sc.t[:, d0:d0 + 128], Sc.t[:, d0:d0 + 128], cmask.t[:], ALU.add,
                           Sc.b + cmask.b, Sc.b)
                if i >= 2:
                    S.op("dve", lambda e: e.tensor_reduce(out=bs.t[:, 0:1], in_=Sc.t[:, 0:nk], axis=AX.X,
                                                          op=ALU.max), Sc.b, bs.b)
                    S.op("dve", lambda e: e.tensor_reduce(out=bs.t[:, 1:2], in_=Sc.t[:, 0:nk - 128], axis=AX.X,
                                                          op=ALU.min), Sc.b, bs.b)
                    tt("dve", bs.t[:, 2:3], bs.t[:, 0:1], bs.t[:, 1:2], ALU.subtract, bs.b, bs.b)
                    ts("dve", wk.t[:], pw2.t[:], bs.t[:, 2:3], None, ALU.mult, None, pw2.b + bs.b, wk.b)
                    tt("dve", bs.t[:, 3:4], bs.t[:, 1:2], wk.t[:, 0:1], ALU.add, bs.b + wk.b, bs.b)
                    yield 2 * nk / 960.0 + 1.0
                    nd = ((nk * 9 // 20) // 128) * 128
                    nA = nk - nd
                    for k in range(NBIS):
                        ts("dve", maskb.t[:, 0:nd], Sc.t[:, 0:nd], bs.t[:, 3:4], None, ALU.is_ge, ALU.add,
                           Sc.b + bs.b, maskb.b + bsd.b, accum=bsd.t[:, 0:1])
                        act(jnkA.t[:, 0:nA], Sc.t[:, nd:nk], AF.Sign, Sc.b + bs.b, jnkA.b + bsa.b,
                            bias=bs.t[:, 3:4], scale=-1.0, accum=bsa.t[:, 0:1], saturate=False)
                        stt("dve", bs.t[:, 4:5], bsa.t[:, 0:1], -0.5, bsd.t[:, 0:1], ALU.mult, ALU.add,
                            bsa.b + bsd.b, bs.b)
                        ts("dve", bs.t[:, 5:6], bs.t[:, 4:5], TOPK - 0.5 - nA / 2.0, wk.t[:, k:k + 1],
                           ALU.is_ge, ALU.mult, bs.b + wk.b, bs.b)
                        stt("dve", bs.t[:, 3:4], bs.t[:, 5:6], wk.t[:, k + 1:k + 2], bs.t[:, 3:4],
                            ALU.subtract, ALU.add, bs.b + wk.b, bs.b)
                        yield nk / 1900.0 + 1.2
                    ts("dve", maskb.t[:, 0:nk], Sc.t[:, 0:nk], bs.t[:, 3:4], -1.0, ALU.is_ge, ALU.add,
                       Sc.b + bs.b, maskb.b)
                else:
                    ts("dve", maskb.t[:, 0:nk], Sc.t[:, 0:nk], -1.0e29, -1.0, ALU.is_ge, ALU.add, Sc.b, maskb.b)
                yield nk / 1900.0 + 0.3
                for g in range((nkt + 7) // 8):
                    ktl = list(range(g * 8, min(g * 8 + 8, nkt)))
                    for j, kt in enumerate(ktl):
                        tr(ptX.t[:, j * 128:(j + 1) * 128], maskb.t[:, kt * 128:(kt + 1) * 128], ident.t[:],
                           maskb.b + ident.b, ptX.b)
                    n = len(ktl)
                    cp("act", mT_.t[:, g * 8:g * 8 + n, :].rearrange("p a b -> p (a b)"), ptX.t[:, 0:n * 128],
                       ptX.b, mT_.b)
                    yield 1.0

            def nextL():
                j = cnt["l"] % 2
                cnt["l"] += 1
                return pL[j], pLb[j]

            def Y_gen(i):
                a = i % 2
                fq, tq, mT_ = fmq[a], tmq, maskT[a]
                nkt = i + 1
                dma("sp", tq.t[:], tm_d[i * 128:(i + 1) * 128, :], "ld_tmq", fence_a, tq.b)
                ngrp = (nkt + 3) // 4
                groups = [(h, g) for h in range(8) for g in range(ngrp)]

                def qk(h, g):
                    hp, pr = h % 2, h // 2
                    ktl = list(range(g * 4, min(g * 4 + 4, nkt)))
                    n = len(ktl)
                    pb, _ = nextL()
                    ptt = PT[cnt["pt"] % 3]
                    cnt["pt"] += 1
                    mm(pb.t[:, 0:n * 128], bigI.t[:],
                       mT_.t[:, g * 4:g * 4 + n, :].rearrange("p a b -> p (a b)"), True, False,
                       bigI.b + mT_.b, pb.b)
                    for j, kt in enumerate(ktl):
                        mm(pb.t[:, j * 128:(j + 1) * 128],
                           kT.t[hp * 64:(hp + 1) * 64, kt, pr * 128:(pr + 1) * 128],
                           fq.t[hp * 64:(hp + 1) * 64, pr * 128:(pr + 1) * 128], False, j == n - 1,
                           [kT.b[kt // 8]] + fq.b, pb.b)
                    act(ptt.t[:, 0:n * 128], pb.t[:, 0:n * 128], AF.Exp, pb.b, ptt.b, scale=0.125)
                    return ktl, ptt

                def pv(h, ktl, ptt):
                    hb_ = pY.b[0] if h < 4 else pY.b[1]
                    ocol = (h % 4) * 65 + (0 if h < 4 else 512)
                    for j, kt in enumerate(ktl):
                        mm(pY.t[:, ocol:ocol + 65], ptt.t[:, j * 128:(j + 1) * 128],
                           V.t[:, kt, h * 65:(h + 1) * 65], kt == 0, kt == nkt - 1,
                           ptt.b + [V.b[kt // 8]], [hb_])

                nxt = qk(*groups[0])
                for k, (h, g) in enumerate(groups):
                    cur = nxt
                    if k + 1 < len(groups):
                        nxt = qk(*groups[k + 1])
                    pv(h, *cur)
                    yield 1.6
                for half in range(2):
                    o4 = pY.t[:, half * 512:half * 512 + 260].rearrange("p (h d) -> p h d", h=4)
                    S.op("dve", lambda e, o4=o4, half=half: e.reciprocal(
                        out=rden.t[:, half * 4:half * 4 + 4].unsqueeze(2), in_=o4[:, :, 64:65]),
                        [pY.b[half]], rden.b)
                    tt("dve", On.t[:, half * 256:(half + 1) * 256].rearrange("p (h d) -> p h d", h=4),
                       o4[:, :, 0:64], rden.t[:, half * 4:half * 4 + 4].unsqueeze(2).broadcast_to([128, 4, 64]),
                       ALU.mult, [pY.b[half]] + rden.b, On.b)
                pb, pbb = nextL()
                for q in range(4):
                    tr(pbb[:, q * 128:(q + 1) * 128], On.t[:, q * 128:(q + 1) * 128], ident.t[:], On.b + ident.b, pb.b)
                cp("act", OnT.t[:].rearrange("p a b -> p (a b)"), pbb[:, 0:512], pb.b, OnT.b)
                yield 1.5
                for cc in range(2):
                    for kc in range(4):
                        mm(pY.t[:, cc * 512:(cc + 1) * 512], OnT.t[:, kc, :], wba.t[:, kc, cc * 512:(cc + 1) * 512],
                           kc == 0, kc == 3, OnT.b + wba.b, [pY.b[cc]])
                tt("dve", fB.t[:], pY.t[:], tq.t[:, 2560:3584], ALU.mult, pY.b + tq.b, fB.b)
                yield 2.5
                for h in range(4):
                    pb, _ = nextL()
                    ab = attb[h % 2]
                    rqT = fq.t[:, 1024 + h * 128:1024 + (h + 1) * 128]
                    rkT = fq.t[:, 1536 + h * 128:1536 + (h + 1) * 128]
                    rv = tq.t[:, 512 + h * 256:512 + (h + 1) * 256]
                    rkz = tq.t[:, h * 128:(h + 1) * 128]
                    zb = pY.b[h // 2]
                    mm(pb.t[:, 0:128], rkT, rqT, True, True, fq.b, pb.b)
                    tt("dve", ab.t[:], pb.t[:, 0:128], decT.t[:, h, :], ALU.mult, pb.b + decT.b, ab.b)
                    pb2, _ = nextL()
                    mm(pb2.t[:, 0:256], rkz, rv, True, True, tq.b, pb2.b)
                    mm(pY.t[:, h * 256:(h + 1) * 256], rqT, Rb.t[:, h, :], True, False, fq.b + [Rb.b[h]], [zb])
                    mm(pY.t[:, h * 256:(h + 1) * 256], ab.t[:], rv, False, True, ab.b + tq.b, [zb])
                    stt("dve", Rf.t[:, h, :], Rf.t[:, h, :], cdec[h], pb2.t[:, 0:256], ALU.mult, ALU.add,
                        [Rf.b[h]] + pb2.b, [Rf.b[h]])
                    cp("act", Rb.t[:, h, :], Rf.t[:, h, :], [Rf.b[h]], [Rb.b[h]])
                    yield 1.5
                for h in range(4):
                    S.op("dve", lambda e, h=h: e.bn_stats(out=gst.t[:, h, :], in_=pY.t[:, h * 256:(h + 1) * 256]),
                         [pY.b[h // 2]], gst.b)
                    S.op("dve", lambda e, h=h: e.bn_aggr(out=gmv.t[:, h, :], in_=gst.t[:, h, :]), gst.b, gmv.b)
                tt("dve", gsm.t[:, 0:4].unsqueeze(2), gmv.t[:, :, 1:2], epsg.t[:].unsqueeze(2), ALU.add,
                   gmv.b + epsg.b, gsm.b)
                act(gsm.t[:, 4:8], gsm.t[:, 0:4], AF.Ln, gsm.b, gsm.b)
                act(gsm.t[:, 8:12], gsm.t[:, 4:8], AF.Exp, gsm.b, gsm.b, scale=-0.5)
                stt("dve", gsm.t[:, 12:16].unsqueeze(2), gmv.t[:, :, 0:1], -1.0, gsm.t[:, 8:12].unsqueeze(2),
                    ALU.mult, ALU.mult, gmv.b + gsm.b, gsm.b)
                yield 2.0
                for h in range(4):
                    act(fA.t[:, h * 256:(h + 1) * 256], pY.t[:, h * 256:(h + 1) * 256], AF.Identity,
                        [pY.b[h // 2]] + gsm.b, fA.b, bias=gsm.t[:, 12 + h:13 + h], scale=gsm.t[:, 8 + h:9 + h])
                tt("dve", ub.t[:], fA.t[:], tq.t[:, 1536:2560], ALU.mult, fA.b + tq.b, ub.b)
                pb, pbb = nextL()
                for kc in range(8):
                    tr(pbb[:, kc * 128:(kc + 1) * 128], ub.t[:, kc * 128:(kc + 1) * 128], ident.t[:], ub.b + ident.b, pb.b)
                for kc in range(8):
                    act(uT.t[:, kc, :], pbb[:, kc * 128:(kc + 1) * 128], AF.Copy, pb.b + ggnT.b, uT.b,
                        scale=ggnT.t[:, kc:kc + 1])
                yield 3.0
                for cc in range(2):
                    for kc in range(8):
                        mm(pY.t[:, cc * 512:(cc + 1) * 512], uT.t[:, kc, :], wbr.t[:, kc, cc * 512:(cc + 1) * 512],
                           kc == 0, kc == 7, uT.b + wbr.b, [pY.b[cc]])
                tt("dve", fA.t[:], pY.t[:], tq.t[:, 3584:4608], ALU.mult, pY.b + tq.b, fA.b)
                tt("dve", ub.t[:], fB.t[:], fA.t[:], ALU.add, fB.b + fA.b, ub.b)
                yield 4.0
                pb, pbb = nextL()
                for kc in range(8):
                    tr(pbb[:, kc * 128:(kc + 1) * 128], ub.t[:, kc * 128:(kc + 1) * 128], ident.t[:], ub.b + ident.b, pb.b)
                cp("act", uT.t[:].rearrange("p a b -> p (a b)"), pbb[:, :], pb.b, uT.b)
                for cc in range(2):
                    for kc in range(8):
                        mm(pY.t[:, cc * 512:(cc + 1) * 512], uT.t[:, kc, :], wo.t[:, kc, cc * 512:(cc + 1) * 512],
                           kc == 0, kc == 7, uT.b + wo.b, [pY.b[cc]])
                yield 4.0
                act(ub.t[:], pY.t[:], AF.Square, pY.b, ub.b + sm2.b, accum=sm2.t[:, 0:1])
                rstd_from_ss(sm2.t[:, 0:1], sm2.t[:, 1:2], sm2.t[:, 2:3], 1.0 / D, sm2.b, sm2.b)
                stt("dve", fB.t[:], pY.t[:], sm2.t[:, 2:3], g2.t[:], ALU.mult, ALU.mult, pY.b + sm2.b + g2.b, fB.b)
                dma("sp", x1_d[i * 128:(i + 1) * 128, :], fB.t[:], "st_x1", fB.b, [])
                yield 2.5

            def drive(gy, gx):
                cy = cx = 0.0
                while gy is not None or gx is not None:
                    if gy is not None and (gx is None or cy <= cx):
                        try:
                            cy += next(gy)
                        except StopIteration:
                            gy = None
                    else:
                        try:
                            cx += next(gx)
                        except StopIteration:
                            gx = None

            drive(None, X_gen(0))
            for i in range(NT):
                drive(Y_gen(i), X_gen(i + 1) if i + 1 < NT else None)

            fence_b = fB.b
            allB = fence_b + kT.b + V.b + ikT.b + wba.b + wbr.b + wo.b + ggnT.b + g2.b + cmask.b + decT.b + epsg.b \
                + bigI.b + tmq.b
            for lst in (fmq, iwq, Th, PT, attb, maskT, pS, pL):
                for x_ in lst:
                    allB += x_.b
            for x_ in (Sc, maskb, jnkA, bsd, bsa, bs, wk, pw2, rden, On, OnT, Rf, Rb, gst, gmv, gsm, fA, fB, ub, uT,
                       sm2, ptX, pY):
                allB += x_.b
        for e in Sched.ENGS:
            S.wait_all(e, allB)

        if stop_after == "B":
            S.wait_all("sp", allB)
            S.emit()
            return nc

        GT = 2 if NT % 2 == 0 else 1
        NG = NT // GT
        GW = GT * 128
        with ExitStack() as sc:
            def sbc(name, shape, dt, n=1):
                return TB(sc.enter_context(nc.sbuf_tensor(name, list(shape), dt)), n)

            def psc_(name, shape, dt, n=1):
                return TB(sc.enter_context(nc.psum_tensor(name, list(shape), dt)), n, True)

            wup = sbc("wup", [128, 8, 2 * DFF], BF16, 8)
            wdn = sbc("wdn", [128, 22, D], BF16, 22)
            g3 = sbc("g3", [128, D], F32)
            g4 = sbc("g4", [128, D], F32)
            cw = sbc("cw", [128, 44, 3], F32)
            cb = sbc("cb", [128, 44], F32)
            hal = sbc("hal", [128, 44, 2], F32, 44)
            xc = [sbc(f"xc{j}", [128, D], F32) for j in range(2 * GT)]
            r1c = [sbc(f"r1c{j}", [128, D], F32) for j in range(2)]
            outc = [sbc(f"outc{j}", [128, D], F32) for j in range(2)]
            hbc = sbc("hbc", [128, D], BF16)
            junkc = sbc("junkc", [128, D], BF16)
            sm3 = [sbc(f"sm3_{j}", [128, 8], F32) for j in range(2)]
            sm4 = [sbc(f"sm4_{j}", [128, 8], F32) for j in range(2)]
            hTg2 = [sbc(f"hTg{j}", [128, 8, GW], BF16) for j in range(2)]
            hfa = sbc("hfa", [128, 44, 2], F32)
            hfb = sbc("hfb", [128, 44, 1], F32)
            accg = [sbc(f"accg{j}", [128, GW], F32) for j in range(2)]
            accu = [sbc(f"accu{j}", [128, GW], F32) for j in range(2)]
            sg = accg
            aT = sbc("aT", [128, 22, GW], BF16, 22)
            pup = [psc_(f"pup{j}", [128, 512], F32) for j in range(3)]
            ptc = psc_("ptc", [128, 1024], BF16)
            pdn = psc_("pdn", [128, 1024], F32, 2)

            for kc in range(8):
                dma("pool", wup.t[:, kc, :], wup_d[kc * 128:(kc + 1) * 128, :], f"ld_wup{kc}", [], [wup.b[kc]])
            for fc in range(22):
                dma("pool", wdn.t[:, fc, :], wdn_d[fc * 128:(fc + 1) * 128, :], f"ld_wdn{fc % 4}", [], [wdn.b[fc]])
            cdma(g3.t[:], g3_d.broadcast_to([128, D]), g3.b)
            cdma(g4.t[:], g4_d.broadcast_to([128, D]), g4.b)
            cdma(cw.t[:], cw_d, cw.b)
            cdma(cb.t[:], cb_d, cb.b)
            S.op("pool", lambda e: e.memset(hal.t[:], 0.0), [], hal.b)

            ukc = {"k": 0}

            def driveC(gy, gx):
                cy = cx = 0.0
                while gy is not None or gx is not None:
                    if gy is not None and (gx is None or cy <= cx):
                        try:
                            cy += next(gy)
                        except StopIteration:
                            gy = None
                    else:
                        try:
                            cx += next(gx)
                        except StopIteration:
                            gx = None

            def PC_gen(g):
                hTg = hTg2[g % 2]
                for tl in range(GT):
                    i = g * GT + tl
                    a = i % 2
                    xt, smt, r1t = xc[(g % 2) * GT + tl], sm3[a], r1c[a]
                    dma("sp", xt.t[:], x_d[i * 128:(i + 1) * 128, :], f"ld_xc{(g % 2) * GT + tl}", [], xt.b)
                    dma("sp", r1t.t[:], x1_d[i * 128:(i + 1) * 128, :], f"ld_r1c{a}", fence_b, r1t.b)
                    tt("dve", xt.t[:], xt.t[:], r1t.t[:], ALU.add, xt.b + r1t.b, xt.b)
                    act(hbc.t[:], xt.t[:], AF.Square, xt.b, hbc.b + smt.b, accum=smt.t[:, 0:1])
                    rstd_from_ss(smt.t[:, 0:1], smt.t[:, 1:2], smt.t[:, 2:3], 1.0 / D, smt.b, smt.b)
                    stt("dve", hbc.t[:], xt.t[:], smt.t[:, 2:3], g3.t[:], ALU.mult, ALU.mult, xt.b + smt.b + g3.b, hbc.b)
                    yield 14.0
                    for kc in range(8):
                        tr(ptc.t[:, kc * 128:(kc + 1) * 128], hbc.t[:, kc * 128:(kc + 1) * 128], ident.t[:],
                           hbc.b + ident.b, ptc.b)
                    cp("act", hTg.t[:, :, tl * 128:(tl + 1) * 128],
                       ptc.t[:].rearrange("p (a b) -> p a b", a=8), ptc.b, hTg.b)
                    yield 1.0

            def MC_gen(g):
                hTg = hTg2[g % 2]
                tt("pool", hfa.t[:], hal.t[:], cw.t[:, :, 0:1].broadcast_to([128, 44, 2]), ALU.mult,
                   hal.b + cw.b, hfa.b)
                tt("pool", hfb.t[:], hal.t[:, :, 1:2], cw.t[:, :, 1:2], ALU.mult, hal.b + cw.b, hfb.b)
                for fc in range(22):
                    for which, c in ((0, fc), (1, fc + 22)):
                        pb = pup[ukc["k"] % 3]
                        ukc["k"] += 1
                        for kc in range(8):
                            mm(pb.t[:, 0:GW], wup.t[:, kc, c * 128:(c + 1) * 128], hTg.t[:, kc, :], kc == 0, kc == 7,
                               [wup.b[kc]] + hTg.b, pb.b)
                        accx = (accg if which == 0 else accu)[fc % 2]
                        cp("act", hal.t[:, c, :], pb.t[:, GW - 2:GW], pb.b, [hal.b[c]])
                        act(accx.t[:], pb.t[:, 0:GW], AF.Identity, pb.b + cw.b + cb.b, accx.b,
                            bias=cb.t[:, c:c + 1], scale=cw.t[:, c, 2:3])
                        stt("dve", accx.t[:, 1:GW], pb.t[:, 0:GW - 1], cw.t[:, c, 1:2], accx.t[:, 1:GW],
                            ALU.mult, ALU.add, pb.b + cw.b + accx.b, accx.b)
                        stt("dve", accx.t[:, 2:GW], pb.t[:, 0:GW - 2], cw.t[:, c, 0:1], accx.t[:, 2:GW],
                            ALU.mult, ALU.add, pb.b + cw.b + accx.b, accx.b)
                        tt("pool", accx.t[:, 0:2], accx.t[:, 0:2], hfa.t[:, c, :], ALU.add, accx.b + hfa.b, accx.b)
                        tt("pool", accx.t[:, 0:1], accx.t[:, 0:1], hfb.t[:, c, :], ALU.add, accx.b + hfb.b, accx.b)
                        yield 0.9
                    ag, au = accg[fc % 2], accu[fc % 2]
                    act(ag.t[:], ag.t[:], AF.Silu, ag.b, ag.b)
                    tt("dve", aT.t[:, fc, :], ag.t[:], au.t[:], ALU.mult, ag.b + au.b, [aT.b[fc]])
                for tl in range(GT):
                    i = g * GT + tl
                    a = i % 2
                    for cc in range(2):
                        for fc in range(22):
                            mm(pdn.t[:, cc * 512:(cc + 1) * 512], aT.t[:, fc, tl * 128:(tl + 1) * 128],
                               wdn.t[:, fc, cc * 512:(cc + 1) * 512], fc == 0, fc == 21,
                               [aT.b[fc], wdn.b[fc]], [pdn.b[cc]])
                    smt = sm4[a]
                    act(junkc.t[:], pdn.t[:], AF.Square, pdn.b, junkc.b + smt.b, accum=smt.t[:, 4:5])
                    rstd_from_ss(smt.t[:, 4:5], smt.t[:, 5:6], smt.t[:, 6:7], 1.0 / D, smt.b, smt.b)
                    xrt, ot = xc[(g % 2) * GT + tl], outc[a]
                    stt("dve", ot.t[:], pdn.t[:], smt.t[:, 6:7], g4.t[:], ALU.mult, ALU.mult, pdn.b + smt.b + g4.b, ot.b)
                    tt("dve", ot.t[:], ot.t[:], xrt.t[:], ALU.add, ot.b + xrt.b, ot.b)
                    dma("sp", out_d[i * 128:(i + 1) * 128, :], ot.t[:], f"st_out{a}", ot.b, [])
                    yield 9.5

            driveC(None, PC_gen(0))
            for g in range(NG):
                driveC(MC_gen(g), PC_gen(g + 1) if g + 1 < NG else None)
            S.wait_all("sp", outc[0].b + outc[1].b)
        S.emit()
    return nc


def _consts():
    invf128 = 10000.0 ** (-(np.arange(64, dtype=np.float32) * 2.0 / 128)).astype(np.float32)
    invf64 = 10000.0 ** (-(np.arange(32, dtype=np.float32) * 2.0 / 64)).astype(np.float32)
    invf = np.concatenate([invf128, invf64]).astype(np.float32)
    c_invf = np.ascontiguousarray(np.broadcast_to(invf[None, :], (128, 96))).astype(np.float32)
    c_ident = np.eye(128, dtype=np.float32).astype(ml_dtypes.bfloat16)
    jj = np.arange(128)
    c_cmask = np.where(jj[None, :] <= jj[:, None], 0.0, NEG).astype(np.float32)
    gam = 1.0 - 2.0 ** (-5.0 - np.arange(4, dtype=np.float64))
    c = 128.0 ** -0.5
    decT = np.zeros((128, 4, 128), np.float64)
    for h in range(4):
        decT[:, h, :] = (c * gam[h] ** (-(jj[:, None] + 1.0))) * (jj[:, None] <= jj[None, :])
    zc = c * gam[None, :] ** (127.0 - jj[:, None])
    xi = gam[None, :] ** (jj[:, None] + 1.0)
    epsg = EPS / xi ** 2
    return dict(c_invf=c_invf, c_ident=c_ident, c_cmask=c_cmask, c_decT=decT.astype(np.float32),
                c_zc=zc.astype(np.float32), c_epsg=epsg.astype(np.float32))


def make_in_maps(inputs, NT=NTF, cores=8):
    T = NT * 128
    f = lambda a: np.ascontiguousarray(np.asarray(a, dtype=np.float32))
    cw = f(inputs["conv_w"])[0]
    cw_l = np.ascontiguousarray(cw.reshape(3, 44, 128).transpose(2, 1, 0))
    cb_l = np.ascontiguousarray(f(inputs["conv_b"])[0].reshape(44, 128).T)
    shared = dict(
        g_pre_mix=f(inputs["norm_pre_mix"]), w_in=f(inputs["w_in"])[0], w_br_attn=f(inputs["w_br_attn"])[0],
        w_br_ret=f(inputs["w_br_ret"])[0],
        ggnT=np.ascontiguousarray(f(inputs["ret_gn_gain"])[0].reshape(8, 128).T), w_out=f(inputs["w_out"])[0],
        g_post_mix=f(inputs["norm_post_mix"]), g_pre_ffn=f(inputs["norm_pre_ffn"]),
        w_ffn_up=f(inputs["w_ffn_up"])[0], conv_w=cw_l, conv_b=cb_l, w_ffn_down=f(inputs["w_ffn_down"])[0],
        g_post_ffn=f(inputs["norm_post_ffn"]),
    )
    shared.update(_consts())
    x = np.asarray(inputs["x"], dtype=np.float32)
    pos = np.asarray(inputs["positions"]).astype(np.int32)
    maps = []
    for b in range(cores):
        m = dict(shared)
        m["x"] = np.ascontiguousarray(x[b, :T])
        m["pos"] = np.ascontiguousarray(pos[b, :T].reshape(NT, 128).T)
        maps.append(m)
    return maps


_NC_CACHE = {}


def kernel(**inputs):
    if "full" not in _NC_CACHE:
        _NC_CACHE["full"] = build_program(NTF)
    nc = _NC_CACHE["full"]
    maps = make_in_maps(inputs, NTF, 8)
    res = run_bass_kernel_spmd(nc, maps, core_ids=list(range(8)))
    out = np.stack([np.asarray(r["out"], dtype=np.float32) for r in res.results], axis=0)
    return out
```

```python
import numpy as np
import ml_dtypes
from contextlib import ExitStack
import concourse.bass as bass
import concourse.mybir as mybir
from concourse.bass_utils import run_bass_kernel_spmd

F32 = mybir.dt.float32
BF16 = mybir.dt.bfloat16
I32 = mybir.dt.int32
ALU = mybir.AluOpType
AF = mybir.ActivationFunctionType
AX = mybir.AxisListType

D = 1024
T_FULL = 4096
NTF = 32
INW = 7240
DFF = 2816
EPS = 1e-6
TOPK = 256
NEG = -1.0e30
NBIS = 20
TWO_PI = float(2 * np.pi)


class Buf:
    __slots__ = ("wr", "rd", "excl")

    def __init__(self, excl=False):
        self.wr = None
        self.rd = []
        self.excl = excl


class Sched:
    ENGS = ("pe", "dve", "act", "pool", "sp")

    def __init__(self, nc, stack):
        self.nc = nc
        self.stack = stack
        self.lists = {e: [] for e in self.ENGS}
        self.sems = {}
        self.count = {}
        self.known = {e: {} for e in self.ENGS}
        for e in self.ENGS:
            self._newsem("E_" + e)

    def _newsem(self, key):
        s = self.stack.enter_context(self.nc.semaphore(key))
        self.sems[key] = s
        self.count[key] = 0

    def _deps(self, eng, reads, writes):
        deps = {}
        own = "E_" + eng

        def add(ev):
            if ev is None:
                return
            k, v = ev
            if eng == "pe" and k == own:
                return
            if deps.get(k, 0) < v:
                deps[k] = v
        for b in reads:
            add(b.wr)
        for b in writes:
            add(b.wr)
            for ev in b.rd:
                add(ev)
        waits = []
        kn = self.known[eng]
        for k, v in deps.items():
            if kn.get(k, 0) < v:
                kn[k] = v
                waits.append((k, v))
        return waits

    def _post(self, ev, reads, writes):
        for b in reads:
            b.rd.append(ev)
        for b in writes:
            b.wr = ev
            b.rd = []

    @staticmethod
    def _split(reads, writes):
        ex = [b for b in reads if b.excl]
        if not ex:
            return reads, writes
        return [b for b in reads if not b.excl], list(writes) + ex

    def op(self, eng, fn, reads=(), writes=()):
        reads, writes = self._split(reads, writes)
        waits = self._deps(eng, reads, writes)
        key = "E_" + eng
        self.count[key] += 1
        ev = (key, self.count[key])
        self.lists[eng].append((waits, fn, key, 1, self.count[key]))
        self._post(ev, reads, writes)

    def dma(self, eng, fn, semkey, reads=(), writes=()):
        if semkey not in self.sems:
            self._newsem(semkey)
        waits = self._deps(eng, reads, writes)
        self.count[semkey] += 16
        ev = (semkey, self.count[semkey])
        self.lists[eng].append((waits, fn, semkey, 16, None))
        self._post(ev, reads, writes)

    def wait_all(self, eng, bufs):
        waits = self._deps(eng, bufs, bufs)
        self.lists[eng].append((waits, None, None, 0, None))

    def emit(self):
        sems = self.sems
        lists = self.lists
        needed = {}
        for e in self.ENGS:
            for waits, fn, key, inc, seq in lists[e]:
                for k, v in waits:
                    if k.startswith("E_"):
                        needed.setdefault(k, set()).add(v)
        rank = {}
        for e in self.ENGS:
            key = "E_" + e
            need = needed.get(key, set())
            m = {}
            r = 0
            for waits, fn, k, inc, seq in lists[e]:
                if seq is not None and seq in need:
                    r += 1
                    m[seq] = r
            rank[key] = m

        def replay(e, items):
            for waits, fn, key, inc, seq in items:
                for k, v in waits:
                    if k.startswith("E_"):
                        v = rank[k][v]
                    e.wait_ge(sems[k], v)
                if fn is not None:
                    ins = fn(e)
                    if seq is None:
                        ins.then_inc(sems[key], inc)
                    elif seq in rank[key]:
                        ins.then_inc(sems[key], 1)

        with self.nc.Block() as block:
            @block.tensor
            def _(e):
                replay(e, lists["pe"])

            @block.vector
            def _(e):
                replay(e, lists["dve"])

            @block.scalar
            def _(e):
                replay(e, lists["act"])

            @block.gpsimd
            def _(e):
                replay(e, lists["pool"])

            @block.sync
            def _(e):
                replay(e, lists["sp"])
        self.stats = {k: len(v) for k, v in rank.items()}


class TB:
    def __init__(self, t, n=1, excl=False):
        self.t = t
        self.b = [Buf(excl) for _ in range(n)]


def build_program(NT=NTF, dbg=False, stop_after=None):
    nc = bass.Bass("TRN2", target_bir_lowering=False)
    T = NT * 128

    def din(name, shape, dt=F32):
        return nc.dram_tensor(name, list(shape), dt, kind="ExternalInput").ap()

    def dscr(name, shape, dt):
        return nc.dram_tensor(name, list(shape), dt, kind=("ExternalOutput" if dbg else "Internal")).ap()

    x_d = din("x", [T, D])
    pos_d = din("pos", [128, NT], I32)
    g1_d = din("g_pre_mix", [1, D])
    w_in_d = din("w_in", [D, INW])
    wba_d = din("w_br_attn", [512, D])
    wbr_d = din("w_br_ret", [D, D])
    ggnT_d = din("ggnT", [128, 8])
    wo_d = din("w_out", [D, D])
    g2_d = din("g_post_mix", [1, D])
    g3_d = din("g_pre_ffn", [1, D])
    wup_d = din("w_ffn_up", [D, 2 * DFF])
    cw_d = din("conv_w", [128, 44, 3])
    cb_d = din("conv_b", [128, 44])
    wdn_d = din("w_ffn_down", [DFF, D])
    g4_d = din("g_post_ffn", [1, D])
    invf_d = din("c_invf", [128, 96])
    ident_d = din("c_ident", [128, 128], BF16)
    cmask_d = din("c_cmask", [128, 128])
    decT_d = din("c_decT", [128, 4, 128])
    zc_d = din("c_zc", [128, 4])
    epsg_d = din("c_epsg", [128, 4])
    out_d = nc.dram_tensor("out", [T, D], F32, kind="ExternalOutput").ap()

    fm_d = dscr("s_fm", [NT, 128, 2048], BF16)
    kT_d = dscr("s_kT", [NT, 128, 512], BF16)
    ikT_d = dscr("s_ikT", [64, T], BF16)
    iw_d = dscr("s_iw", [T, 8], F32)
    v_d = dscr("s_v", [T, 520], BF16)
    tm_d = dscr("s_tm", [T, 4608], BF16)
    x1_d = dscr("s_x1", [T, D], F32)

    cdec = [float((1.0 - 2.0 ** (-5.0 - h)) ** 128) for h in range(4)]

    with ExitStack() as st:
        S = Sched(nc, st)

        def sb(name, shape, dt, n=1):
            return TB(st.enter_context(nc.sbuf_tensor(name, list(shape), dt)), n)

        def ps(name, shape, dt, n=1):
            return TB(st.enter_context(nc.psum_tensor(name, list(shape), dt)), n, True)

        def mm(out, lhsT, rhs, start, stop, reads, writes):
            S.op("pe", lambda e: e.matmul(out, lhsT=lhsT, rhs=rhs, start=start, stop=stop), reads, writes)

        def tr(out, in_, ident_ap, reads, writes):
            S.op("pe", lambda e: e.transpose(out=out, in_=in_, identity=ident_ap), reads, writes)

        def act(out, in_, func, reads, writes, bias=None, scale=None, accum=None, saturate=None):
            kw = {}
            if saturate is not None:
                kw["saturate"] = saturate
            if bias is not None:
                kw["bias"] = bias
            if scale is not None:
                kw["scale"] = scale
            if accum is not None:
                kw["accum_out"] = accum
            S.op("act", lambda e: e.activation(out=out, in_=in_, func=func, **kw), reads, writes)

        def tt(eng, out, in0, in1, op, reads, writes):
            S.op(eng, lambda e: e.tensor_tensor(out=out, in0=in0, in1=in1, op=op), reads, writes)

        def ts(eng, out, in0, s1, s2, op0, op1, reads, writes, accum=None):
            kw = {}
            if op1 is not None:
                kw["op1"] = op1
            if accum is not None:
                kw["accum_out"] = accum
            S.op(eng, lambda e: e.tensor_scalar(out=out, in0=in0, scalar1=s1, scalar2=s2, op0=op0, **kw), reads, writes)

        def stt(eng, out, in0, scalar, in1, op0, op1, reads, writes):
            S.op(eng, lambda e: e.scalar_tensor_tensor(out=out, in0=in0, scalar=scalar, in1=in1, op0=op0, op1=op1),
                 reads, writes)

        def cp(eng, out, in_, reads, writes):
            if eng == "act":
                S.op("act", lambda e: e.activation(out=out, in_=in_, func=AF.Copy), reads, writes)
            else:
                S.op(eng, lambda e: e.tensor_copy(out=out, in_=in_), reads, writes)

        def dma(eng, out, in_, key, reads, writes):
            S.dma(eng, lambda e: e.dma_start(out=out, in_=in_), key, reads, writes)

        chain = Buf()

        def cdma(out, in_, writes):
            S.dma("sp", lambda e: e.dma_start(out=out, in_=in_), "ld_misc", [], list(writes) + [chain])

        ident = sb("ident", [128, 128], BF16)
        epsc = sb("epsc", [128, 1], F32)
        cdma(ident.t[:], ident_d, ident.b)
        S.op("dve", lambda e: e.memset(epsc.t[:], EPS), [], epsc.b)

        def rstd_from_ss(ss_ap, ln_ap, out_ap, inv_n, reads, writes):
            act(ln_ap, ss_ap, AF.Ln, list(reads) + epsc.b, writes, bias=epsc.t[:, 0:1], scale=inv_n)
            act(out_ap, ln_ap, AF.Exp, writes, writes, scale=-0.5)

        with ExitStack() as sa:
            def sba(name, shape, dt, n=1):
                return TB(sa.enter_context(nc.sbuf_tensor(name, list(shape), dt)), n)

            def psa(name, shape, dt, n=1):
                return TB(sa.enter_context(nc.psum_tensor(name, list(shape), dt)), n, True)

            win = sba("win", [128, 8, INW], BF16, 8)
            gbc = sba("gbc", [128, D], F32)
            invf = sba("invf", [128, 96], F32)
            posi = sba("posi", [128, NT], I32)
            posf = sba("posf", [128, NT], F32)
            rt = [sba(f"rt{j}", [128, 3, 96], F32) for j in range(2)]
            ang = sba("ang", [128, 96], F32)
            angk = sba("angk", [128, 96], F32)
            angi = sba("angi", [128, 96], I32)
            zc = sba("zc", [128, 4], F32)
            xin = [sba(f"xin{j}", [128, D], F32) for j in range(2)]
            junkb = sba("junkb", [128, D], BF16)
            sm = [sba(f"smA{j}", [128, 8], F32) for j in range(2)]
            hb = sba("hb", [128, D], BF16)
            hT = [sba(f"hT{j}", [128, 8, 128], BF16) for j in range(2)]
            t1 = [sba(f"t1_{j}", [128, 512], F32) for j in range(2)]
            t2 = [sba(f"t2_{j}", [128, 512], F32) for j in range(2)]
            rob = [sba(f"rob{j}", [128, 512], BF16) for j in range(3)]
            tm = [sba(f"tmA{j}", [128, 4608], BF16, 9) for j in range(2)]
            fm = [sba(f"fmA{j}", [128, 2048], BF16, 4) for j in range(2)]
            kTs = [sba(f"kTs{j}", [128, 512], BF16) for j in range(2)]
            vs = [sba(f"vs{j}", [128, 8, 65], BF16) for j in range(2)]
            iks = [sba(f"iks{j}", [64, 128], BF16) for j in range(2)]
            iws = [sba(f"iws{j}", [128, 8], F32) for j in range(2)]
            pj = [psa(f"pj{j}", [128, 512], F32) for j in range(5)]
            ptr = [psa(f"ptrA{j}", [128, 1024], BF16) for j in range(2)]

            for kc in range(8):
                dma("pool", win.t[:, kc, :], w_in_d[kc * 128:(kc + 1) * 128, :], f"ld_win{kc}", [], [win.b[kc]])
            cdma(gbc.t[:], g1_d.broadcast_to([128, D]), gbc.b)
            cdma(invf.t[:], invf_d, invf.b)
            cdma(posi.t[:], pos_d, posi.b)
            cdma(zc.t[:], zc_d, zc.b)
            for j in range(2):
                S.op("pool", lambda e, j=j: e.memset(vs[j].t[:], 1.0), [], vs[j].b)

            cp("dve", posf.t[:], posi.t[:], posi.b, posf.b)

            def range_reduce(shift):
                ts("dve", angk.t[:], ang.t[:], shift, 1.0 / TWO_PI, ALU.add, ALU.mult, ang.b, angk.b)
                cp("dve", angi.t[:], angk.t[:], angk.b, angi.b)
                cp("dve", angk.t[:], angi.t[:], angi.b, angk.b)
                ts("dve", angk.t[:], angk.t[:], -TWO_PI, shift, ALU.mult, ALU.add, angk.b, angk.b)
                tt("dve", angk.t[:], angk.t[:], ang.t[:], ALU.add, angk.b + ang.b, angk.b)
                ts("dve", angk.t[:], angk.t[:], float(np.pi), -float(np.pi), ALU.min, ALU.max, angk.b, angk.b)

            def make_tables(i):
                r_ = rt[i % 2]
                ts("dve", ang.t[:], invf.t[:], posf.t[:, i:i + 1], None, ALU.mult, None, invf.b + posf.b, ang.b)
                range_reduce(float(np.pi / 2))
                act(r_.t[:, 0, :], angk.t[:], AF.Sin, angk.b, r_.b)
                range_reduce(0.0)
                act(r_.t[:, 2, :], angk.t[:], AF.Sin, angk.b, r_.b)
                ts("dve", r_.t[:, 1, :], r_.t[:, 2, :], -1.0, None, ALU.mult, None, r_.b, r_.b)

            def rotary(i, src, nh, half, toff, dst, reads, writes, k):
                a1, a2 = t1[k % 2], t2[k % 2]
                rti = rt[i % 2]
                w = nh * 2 * half
                s4 = src.rearrange("p (h t j) -> p h t j", h=nh, t=2, j=half)
                o1 = a1.t[:, 0:w].rearrange("p (h t j) -> p h t j", h=nh, t=2, j=half)
                o2 = a2.t[:, 0:w].rearrange("p (h t j) -> p h t j", h=nh, t=2, j=half)
                cosb = rti.t[:, 0, toff:toff + half].unsqueeze(1).unsqueeze(1).broadcast_to([128, nh, 2, half])
                nsin = rti.t[:, 1, toff:toff + half].unsqueeze(1).broadcast_to([128, nh, half])
                psin = rti.t[:, 2, toff:toff + half].unsqueeze(1).broadcast_to([128, nh, half])
                tt("dve", o1, s4, cosb, ALU.mult, list(reads) + rti.b, a1.b)
                tt("dve", o2[:, :, 0, :], s4[:, :, 1, :], nsin, ALU.mult, list(reads) + rti.b, a2.b)
                tt("dve", o2[:, :, 1, :], s4[:, :, 0, :], psin, ALU.mult, list(reads) + rti.b, a2.b)
                tt("dve", dst, a1.t[:, 0:w], a2.t[:, 0:w], ALU.add, a1.b + a2.b, writes)

            ncol = [512] * 4 + [72] + [512] * 10
            coff = [0]
            for c in ncol:
                coff.append(coff[-1] + c)
            rotc = {"k": 0}

            def drive(gy, gx):
                cy = cx = 0.0
                while gy is not None or gx is not None:
                    if gy is not None and (gx is None or cy <= cx):
                        try:
                            cy += next(gy)
                        except StopIteration:
                            gy = None
                    else:
                        try:
                            cx += next(gx)
                        except StopIteration:
                            gx = None

            def PA_gen(i):
                a = i % 2
                xt, smt, hTt = xin[a], sm[a], hT[a]
                dma("sp", xt.t[:], x_d[i * 128:(i + 1) * 128, :], f"ld_x{a}", [], xt.b)
                make_tables(i)
                act(junkb.t[:], xt.t[:], AF.Square, xt.b, junkb.b + smt.b, accum=smt.t[:, 0:1])
                rstd_from_ss(smt.t[:, 0:1], smt.t[:, 1:2], smt.t[:, 2:3], 1.0 / D, smt.b, smt.b)
                stt("dve", hb.t[:], xt.t[:], smt.t[:, 2:3], gbc.t[:], ALU.mult, ALU.mult,
                    xt.b + smt.b + gbc.b, hb.b)
                yield 12.0
                pt = ptr[0]
                for kc in range(8):
                    tr(pt.t[:, kc * 128:(kc + 1) * 128], hb.t[:, kc * 128:(kc + 1) * 128], ident.t[:],
                       hb.b + ident.b, pt.b)
                cp("act", hTt.t[:].rearrange("p a b -> p (a b)"), pt.t[:], pt.b, hTt.b)
                yield 1.0

            def MA_gen(i):
                a = i % 2
                hTt, tmt, fmt = hT[a], tm[a], fm[a]
                pend = []

                def flush(item):
                    c, r = item
                    pt = ptr[1]
                    if c == 4:
                        tr(pt.t[0:64, 0:128], r.t[:, 0:64], ident.t[:], r.b + ident.b, pt.b)
                        cp("act", iks[a].t[:], pt.t[0:64, 0:128], pt.b, iks[a].b)
                        dma("sp", ikT_d[:, i * 128:(i + 1) * 128], iks[a].t[:], f"st_ik{a}", iks[a].b, [])
                        return
                    for q in range(4):
                        tr(pt.t[:, q * 128:(q + 1) * 128], r.t[:, q * 128:(q + 1) * 128], ident.t[:],
                           r.b + ident.b, pt.b)
                    if c == 1:
                        cp("act", kTs[a].t[:], pt.t[:, 0:512], pt.b, kTs[a].b)
                        dma("sp", kT_d[i], kTs[a].t[:], f"st_kT{a}", kTs[a].b, [])
                    else:
                        slot = {0: 0, 3: 1, 5: 2, 6: 3}[c]
                        cp("act", fmt.t[:, slot * 512:(slot + 1) * 512], pt.t[:, 0:512], pt.b, [fmt.b[slot]])

                for c in range(15):
                    pb = pj[c % 5]
                    w = ncol[c]
                    for kc in range(8):
                        mm(pb.t[:, 0:w], hTt.t[:, kc, :], win.t[:, kc, coff[c]:coff[c] + w], kc == 0, kc == 7,
                           hTt.b + [win.b[kc]], pb.b)
                    if c in (0, 1, 3, 5, 6):
                        r = rob[rotc["k"] % 3]
                        if c in (5, 6):
                            rotary(i, pb.t[:, :], 4, 64, 0, r.t[:, 0:512], pb.b, r.b, rotc["k"])
                        else:
                            rotary(i, pb.t[:, :], 8, 32, 64, r.t[:, 0:512], pb.b, r.b, rotc["k"])
                        rotc["k"] += 1
                        if c == 6:
                            tt("dve", tmt.t[:, 0:512].rearrange("p (h d) -> p h d", h=4),
                               r.t[:].rearrange("p (h d) -> p h d", h=4),
                               zc.t[:].unsqueeze(2).broadcast_to([128, 4, 128]), ALU.mult,
                               r.b + zc.b, [tmt.b[0]])
                        pend.append((c, r))
                    elif c == 2:
                        cp("act", vs[a].t[:, :, 0:64], pb.t[:, :].rearrange("p (h d) -> p h d", h=8), pb.b, vs[a].b)
                        dma("sp", v_d[i * 128:(i + 1) * 128, :], vs[a].t[:].rearrange("p h d -> p (h d)"),
                            f"st_v{a}", vs[a].b, [])
                    elif c == 4:
                        r = rob[rotc["k"] % 3]
                        rotary(i, pb.t[:, 0:64], 1, 32, 64, r.t[:, 0:64], pb.b, r.b, rotc["k"])
                        rotc["k"] += 1
                        ts("dve", iws[a].t[:], pb.t[:, 64:72], float(8 ** -0.5 * 64 ** -0.5), None, ALU.mult, None,
                           pb.b, iws[a].b)
                        dma("sp", iw_d[i * 128:(i + 1) * 128, :], iws[a].t[:], f"st_iw{a}", iws[a].b, [])
                        pend.append((c, r))
                    elif c in (7, 8):
                        o = 512 + (c - 7) * 512
                        cp("act", tmt.t[:, o:o + 512], pb.t[:, :], pb.b, [tmt.b[1 + c - 7]])
                    elif c in (9, 10):
                        o = 1536 + (c - 9) * 512
                        act(tmt.t[:, o:o + 512], pb.t[:, :], AF.Silu, pb.b, [tmt.b[3 + c - 9]])
                    else:
                        o = 2560 + (c - 11) * 512
                        act(tmt.t[:, o:o + 512], pb.t[:, :], AF.Sigmoid, pb.b, [tmt.b[5 + c - 11]])
                    while pend and pend[0][0] <= c - 2:
                        flush(pend.pop(0))
                    yield 1.7
                while pend:
                    flush(pend.pop(0))
                dma("sp", fm_d[i], fmt.t[:], f"st_fm{a}", fmt.b, [])
                dma("sp", tm_d[i * 128:(i + 1) * 128, :], tmt.t[:], f"st_tm{a}", tmt.b, [])
                yield 0.5

            drive(None, PA_gen(0))
            for i in range(NT):
                drive(MA_gen(i), PA_gen(i + 1) if i + 1 < NT else None)
            fence_a = []
            for a in range(2):
                fence_a += tm[a].b + fm[a].b + kTs[a].b + vs[a].b + iks[a].b + iws[a].b
            allA = fence_a + win.b + gbc.b + invf.b + posi.b + posf.b + rt[0].b + rt[1].b + ang.b + angk.b + angi.b + zc.b + \
                xin[0].b + xin[1].b + junkb.b + sm[0].b + sm[1].b + hb.b + hT[0].b + hT[1].b + \
                t1[0].b + t1[1].b + t2[0].b + t2[1].b + rob[0].b + rob[1].b + rob[2].b + \
                [p.b[0] for p in pj] + [p.b[0] for p in ptr]
        for e in Sched.ENGS:
            S.wait_all(e, allA)

        if stop_after == "A":
            S.wait_all("sp", allA)
            S.emit()
            return nc

        with ExitStack() as sbk:
            def sbb(name, shape, dt, n=1):
                return TB(sbk.enter_context(nc.sbuf_tensor(name, list(shape), dt)), n)

            def psb(name, shape, dt, n=1):
                return TB(sbk.enter_context(nc.psum_tensor(name, list(shape), dt)), n, True)

            NGC = (NT + 7) // 8
            kT = sbb("kTc", [128, NT, 512], BF16, NGC)
            V = sbb("Vc", [128, NT, 520], BF16, NGC)
            ikT = sbb("ikTc", [128, T], BF16, NGC)
            wba = sbb("wba", [128, 4, D], BF16)
            wbr = sbb("wbr", [128, 8, D], BF16)
            wo = sbb("wo", [128, 8, D], BF16)
            ggnT = sbb("ggnT_sb", [128, 8], F32)
            g2 = sbb("g2", [128, D], F32)
            cmask = sbb("cmask", [128, 128], F32)
            decT = sbb("decT", [128, 4, 128], F32)
            epsg = sbb("epsg", [128, 4], F32)
            bigI = sbb("bigI", [128, 128], BF16)
            fmq = [sbb(f"fmq{j}", [128, 2048], BF16) for j in range(2)]
            tmq = sbb("tmq0", [128, 4608], BF16)
            iwq = [sbb(f"iwq{j}", [128, 8], F32) for j in range(2)]
            Th = [sbb(f"Th{j}", [128, 512], BF16) for j in range(3)]
            PT = [sbb(f"PT{j}", [128, 512], BF16) for j in range(3)]
            Sc = sbb("Sc", [128, T], F32)
            maskb = sbb("maskb", [128, T], BF16)
            jnkA = sbb("jnkA", [128, max(128, T - ((T * 9 // 20) // 128) * 128)], mybir.dt.float8e4)
            maskT = [sbb(f"maskT{j}", [128, NT, 128], BF16) for j in range(2)]
            bs = sbb("bs", [128, 8], F32)
            bsd = sbb("bsd", [128, 2], F32)
            bsa = sbb("bsa", [128, 2], F32)
            wk = sbb("wk", [128, NBIS + 1], F32)
            pw2 = sbb("pw2", [128, NBIS + 1], F32)
            rden = sbb("rden", [128, 8], F32)
            On = sbb("On", [128, 512], BF16)
            OnT = sbb("OnT", [128, 4, 128], BF16)
            attb = [sbb(f"attb{j}", [128, 128], BF16) for j in range(2)]
            Rf = sbb("Rf", [128, 4, 256], F32, 4)
            Rb = sbb("Rb", [128, 4, 256], BF16, 4)
            gst = sbb("gst", [128, 4, 6], F32)
            gmv = sbb("gmv", [128, 4, 2], F32)
            gsm = sbb("gsm", [128, 16], F32)
            fA = sbb("fA", [128, D], F32)
            fB = sbb("fB", [128, D], F32)
            ub = sbb("ub", [128, D], BF16)
            uT = sbb("uT", [128, 8, 128], BF16)
            sm2 = sbb("sm2", [128, 8], F32)
            pS = [psb(f"pS{j}", [128, 512], F32) for j in range(3)]
            ptX = psb("ptX", [128, 1024], BF16)
            pL = [psb(f"pL{j}", [128, 512], F32) for j in range(2)]
            pLb = [p.t.bitcast(BF16) for p in pL]
            pY = psb("pY", [128, 1024], F32, 2)

            for kc in range(4):
                dma("pool", wba.t[:, kc, :], wba_d[kc * 128:(kc + 1) * 128, :], "ld_wba", [], wba.b)
            for kc in range(8):
                dma("pool", wbr.t[:, kc, :], wbr_d[kc * 128:(kc + 1) * 128, :], "ld_wbr", [], wbr.b)
            for kc in range(8):
                dma("pool", wo.t[:, kc, :], wo_d[kc * 128:(kc + 1) * 128, :], "ld_wo", [], wo.b)
            cdma(ggnT.t[:], ggnT_d, ggnT.b)
            cdma(g2.t[:], g2_d.broadcast_to([128, D]), g2.b)
            cdma(cmask.t[:], cmask_d, cmask.b)
            cdma(decT.t[:], decT_d, decT.b)
            cdma(epsg.t[:], epsg_d, epsg.b)
            ts("dve", bigI.t[:], ident.t[:], 30000.0, None, ALU.mult, None, ident.b, bigI.b)
            for gc in range(NGC):
                t0, t1_ = gc * 8, min(NT, gc * 8 + 8)
                dma("sp", ikT.t[0:64, t0 * 128:t1_ * 128], ikT_d[:, t0 * 128:t1_ * 128], f"ld_ikA{gc}", fence_a,
                    [ikT.b[gc]])
                dma("sp", ikT.t[64:128, t0 * 128:t1_ * 128], ikT_d[:, t0 * 128:t1_ * 128], f"ld_ikB{gc}", fence_a,
                    [ikT.b[gc]])
                dma("sp", kT.t[:, t0:t1_, :], kT_d[t0:t1_].rearrange("t p c -> p t c"), f"ld_kT{gc}", fence_a,
                    [kT.b[gc]])
                dma("sp", V.t[:, t0:t1_, :], v_d[t0 * 128:t1_ * 128, :].rearrange("(t p) c -> p t c", p=128),
                    f"ld_V{gc}", fence_a, [V.b[gc]])
            for h in range(4):
                S.op("dve", lambda e, h=h: e.memset(Rf.t[:, h, :], 0.0), [], [Rf.b[h]])
                S.op("dve", lambda e, h=h: e.memset(Rb.t[:, h, :], 0.0), [], [Rb.b[h]])
            for k in range(NBIS + 1):
                kk = min(k, NBIS - 1)
                S.op("dve", lambda e, k=k, kk=kk: e.memset(pw2.t[:, k:k + 1], float(2.0 ** -(kk + 1))), [], pw2.b)

            cnt = {"s": 0, "l": 0, "th": 0, "pt": 0}

            def X_gen(i):
                a = i % 2
                fq, wq, mT_ = fmq[a], iwq[a], maskT[a]
                nkt = i + 1
                nk = nkt * 128
                dma("sp", fq.t[:], fm_d[i], f"ld_fmq{a}", fence_a, fq.b)
                dma("sp", wq.t[:], iw_d[i * 128:(i + 1) * 128, :], f"ld_iwq{a}", fence_a, wq.b)
                nch = (nkt + 3) // 4
                for c in range(nch):
                    k0 = c * 512
                    wc = min(512, nk - k0)
                    kts = list(range(c * 4, min(c * 4 + 4, nkt)))
                    for h in range(8):
                        hp, pr = h % 2, h // 2
                        pb = pS[cnt["s"] % 3]
                        cnt["s"] += 1
                        thb = Th[cnt["th"] % 3]
                        cnt["th"] += 1
                        mm(pb.t[:, 0:wc], fq.t[hp * 64:(hp + 1) * 64, 512 + pr * 128:512 + (pr + 1) * 128],
                           ikT.t[hp * 64:(hp + 1) * 64, k0:k0 + wc], True, True,
                           fq.b + [ikT.b[k // 8] for k in kts], pb.b)
                        act(thb.t[:, 0:wc], pb.t[:, 0:wc], AF.Relu, pb.b, thb.b)
                        if h == 0:
                            ts("dve", Sc.t[:, k0:k0 + wc], thb.t[:, 0:wc], wq.t[:, 0:1], None, ALU.mult, None,
                               thb.b + wq.b, Sc.b)
                        else:
                            stt("dve", Sc.t[:, k0:k0 + wc], thb.t[:, 0:wc], wq.t[:, h:h + 1], Sc.t[:, k0:k0 + wc],
                                ALU.mult, ALU.add, thb.b + wq.b + Sc.b, Sc.b)
                        yield 0.25
                    if kts[-1] == i:
                        d0 = k0 + wc - 128
                        tt("dve", Sc.t[:, d0:d0 + 128], Sc.t[:, d0:d0 + 128], cmask.t[:], ALU.add,
                           Sc.b + cmask.b, Sc.b)
                if i >= 2:
                    S.op("dve", lambda e: e.tensor_reduce(out=bs.t[:, 0:1], in_=Sc.t[:, 0:nk], axis=AX.X,
                                                          op=ALU.max), Sc.b, bs.b)
                    S.op("dve", lambda e: e.tensor_reduce(out=bs.t[:, 1:2], in_=Sc.t[:, 0:nk - 128], axis=AX.X,
                                                          op=ALU.min), Sc.b, bs.b)
                    tt("dve", bs.t[:, 2:3], bs.t[:, 0:1], bs.t[:, 1:2], ALU.subtract, bs.b, bs.b)
                    ts("dve", wk.t[:], pw2.t[:], bs.t[:, 2:3], None, ALU.mult, None, pw2.b + bs.b, wk.b)
                    tt("dve", bs.t[:, 3:4], bs.t[:, 1:2], wk.t[:, 0:1], ALU.add, bs.b + wk.b, bs.b)
                    yield 2 * nk / 960.0 + 1.0
                    nd = ((nk * 9 // 20) // 128) * 128
                    nA = nk - nd
                    for k in range(NBIS):
                        ts("dve", maskb.t[:, 0:nd], Sc.t[:, 0:nd], bs.t[:, 3:4], None, ALU.is_ge, ALU.add,
                           Sc.b + bs.b, maskb.b + bsd.b, accum=bsd.t[:, 0:1])
                        act(jnkA.t[:, 0:nA], Sc.t[:, nd:nk], AF.Sign, Sc.b + bs.b, jnkA.b + bsa.b,
                            bias=bs.t[:, 3:4], scale=-1.0, accum=bsa.t[:, 0:1], saturate=False)
                        stt("dve", bs.t[:, 4:5], bsa.t[:, 0:1], -0.5, bsd.t[:, 0:1], ALU.mult, ALU.add,
                            bsa.b + bsd.b, bs.b)
                        ts("dve", bs.t[:, 5:6], bs.t[:, 4:5], TOPK - 0.5 - nA / 2.0, wk.t[:, k:k + 1],
                           ALU.is_ge, ALU.mult, bs.b + wk.b, bs.b)
                        stt("dve", bs.t[:, 3:4], bs.t[:, 5:6], wk.t[:, k + 1:k + 2], bs.t[:, 3:4],
                            ALU.subtract, ALU.add, bs.b + wk.b, bs.b)
                        yield nk / 1900.0 + 1.2
                    ts("dve", maskb.t[:, 0:nk], Sc.t[:, 0:nk], bs.t[:, 3:4], -1.0, ALU.is_ge, ALU.add,
                       Sc.b + bs.b, maskb.b)
                else:
                    ts("dve", maskb.t[:, 0:nk], Sc.t[:, 0:nk], -1.0e29, -1.0, ALU.is_ge, ALU.add, Sc.b, maskb.b)
                yield nk / 1900.0 + 0.3
                for g in range((nkt + 7) // 8):
                    ktl = list(range(g * 8, min(g * 8 + 8, nkt)))
                    for j, kt in enumerate(ktl):
                        tr(ptX.t[:, j * 128:(j + 1) * 128], maskb.t[:, kt * 128:(kt + 1) * 128], ident.t[:],
                           maskb.b + ident.b, ptX.b)
                    n = len(ktl)
                    cp("act", mT_.t[:, g * 8:g * 8 + n, :].rearrange("p a b -> p (a b)"), ptX.t[:, 0:n * 128],
                       ptX.b, mT_.b)
                    yield 1.0

            def nextL():
                j = cnt["l"] % 2
                cnt["l"] += 1
                return pL[j], pLb[j]

            def Y_gen(i):
                a = i % 2
                fq, tq, mT_ = fmq[a], tmq, maskT[a]
                nkt = i + 1
                dma("sp", tq.t[:], tm_d[i * 128:(i + 1) * 128, :], "ld_tmq", fence_a, tq.b)
                for h in range(4):
                    pb, _ = nextL()
                    ab = attb[h % 2]
                    rqT = fq.t[:, 1024 + h * 128:1024 + (h + 1) * 128]
                    rkT = fq.t[:, 1536 + h * 128:1536 + (h + 1) * 128]
                    rv = tq.t[:, 512 + h * 256:512 + (h + 1) * 256]
                    rkz = tq.t[:, h * 128:(h + 1) * 128]
                    zb = pY.b[h // 2]
                    mm(pb.t[:, 0:128], rkT, rqT, True, True, fq.b, pb.b)
                    tt("dve", ab.t[:], pb.t[:, 0:128], decT.t[:, h, :], ALU.mult, pb.b + decT.b, ab.b)
                    pb2, _ = nextL()
                    mm(pb2.t[:, 0:256], rkz, rv, True, True, tq.b, pb2.b)
                    mm(pY.t[:, h * 256:(h + 1) * 256], rqT, Rb.t[:, h, :], True, False, fq.b + [Rb.b[h]], [zb])
                    mm(pY.t[:, h * 256:(h + 1) * 256], ab.t[:], rv, False, True, ab.b + tq.b, [zb])
                    stt("dve", Rf.t[:, h, :], Rf.t[:, h, :], cdec[h], pb2.t[:, 0:256], ALU.mult, ALU.add,
                        [Rf.b[h]] + pb2.b, [Rf.b[h]])
                    cp("act", Rb.t[:, h, :], Rf.t[:, h, :], [Rf.b[h]], [Rb.b[h]])
                    yield 1.5
                for h in range(4):
                    S.op("dve", lambda e, h=h: e.bn_stats(out=gst.t[:, h, :], in_=pY.t[:, h * 256:(h + 1) * 256]),
                         [pY.b[h // 2]], gst.b)
                    S.op("dve", lambda e, h=h: e.bn_aggr(out=gmv.t[:, h, :], in_=gst.t[:, h, :]), gst.b, gmv.b)
                tt("dve", gsm.t[:, 0:4].unsqueeze(2), gmv.t[:, :, 1:2], epsg.t[:].unsqueeze(2), ALU.add,
                   gmv.b + epsg.b, gsm.b)
                act(gsm.t[:, 4:8], gsm.t[:, 0:4], AF.Ln, gsm.b, gsm.b)
                act(gsm.t[:, 8:12], gsm.t[:, 4:8], AF.Exp, gsm.b, gsm.b, scale=-0.5)
                stt("dve", gsm.t[:, 12:16].unsqueeze(2), gmv.t[:, :, 0:1], -1.0, gsm.t[:, 8:12].unsqueeze(2),
                    ALU.mult, ALU.mult, gmv.b + gsm.b, gsm.b)
                yield 2.0
                for h in range(4):
                    act(fA.t[:, h * 256:(h + 1) * 256], pY.t[:, h * 256:(h + 1) * 256], AF.Identity,
                        [pY.b[h // 2]] + gsm.b, fA.b, bias=gsm.t[:, 12 + h:13 + h], scale=gsm.t[:, 8 + h:9 + h])
                tt("dve", ub.t[:], fA.t[:], tq.t[:, 1536:2560], ALU.mult, fA.b + tq.b, ub.b)
                pb, pbb = nextL()
                for kc in range(8):
                    tr(pbb[:, kc * 128:(kc + 1) * 128], ub.t[:, kc * 128:(kc + 1) * 128], ident.t[:], ub.b + ident.b, pb.b)
                for kc in range(8):
                    act(uT.t[:, kc, :], pbb[:, kc * 128:(kc + 1) * 128], AF.Copy, pb.b + ggnT.b, uT.b,
                        scale=ggnT.t[:, kc:kc + 1])
                yield 3.0
                for cc in range(2):
                    for kc in range(8):
                        mm(pY.t[:, cc * 512:(cc + 1) * 512], uT.t[:, kc, :], wbr.t[:, kc, cc * 512:(cc + 1) * 512],
                           kc == 0, kc == 7, uT.b + wbr.b, [pY.b[cc]])
                tt("dve", fA.t[:], pY.t[:], tq.t[:, 3584:4608], ALU.mult, pY.b + tq.b, fA.b)
                yield 2.0
                ngrp = (nkt + 3) // 4
                groups = [(h, g) for h in range(8) for g in range(ngrp)]

                def qk(h, g):
                    hp, pr = h % 2, h // 2
                    ktl = list(range(g * 4, min(g * 4 + 4, nkt)))
                    n = len(ktl)
                    pb, _ = nextL()
                    ptt = PT[cnt["pt"] % 3]
                    cnt["pt"] += 1
                    mm(pb.t[:, 0:n * 128], bigI.t[:],
                       mT_.t[:, g * 4:g * 4 + n, :].rearrange("p a b -> p (a b)"), True, False,
                       bigI.b + mT_.b, pb.b)
                    for j, kt in enumerate(ktl):
                        mm(pb.t[:, j * 128:(j + 1) * 128],
                           kT.t[hp * 64:(hp + 1) * 64, kt, pr * 128:(pr + 1) * 128],
                           fq.t[hp * 64:(hp + 1) * 64, pr * 128:(pr + 1) * 128], False, j == n - 1,
                           [kT.b[kt // 8]] + fq.b, pb.b)
                    act(ptt.t[:, 0:n * 128], pb.t[:, 0:n * 128], AF.Exp, pb.b, ptt.b, scale=0.125)
                    return ktl, ptt

                def pv(h, ktl, ptt):
                    hb_ = pY.b[0] if h < 4 else pY.b[1]
                    ocol = (h % 4) * 65 + (0 if h < 4 else 512)
                    for j, kt in enumerate(ktl):
                        mm(pY.t[:, ocol:ocol + 65], ptt.t[:, j * 128:(j + 1) * 128],
                           V.t[:, kt, h * 65:(h + 1) * 65], kt == 0, kt == nkt - 1,
                           ptt.b + [V.b[kt // 8]], [hb_])

                nxt = qk(*groups[0])
                for k, (h, g) in enumerate(groups):
                    cur = nxt
                    if k + 1 < len(groups):
                        nxt = qk(*groups[k + 1])
                    pv(h, *cur)
                    yield 1.6
                for half in range(2):
                    o4 = pY.t[:, half * 512:half * 512 + 260].rearrange("p (h d) -> p h d", h=4)
                    S.op("dve", lambda e, o4=o4, half=half: e.reciprocal(
                        out=rden.t[:, half * 4:half * 4 + 4].unsqueeze(2), in_=o4[:, :, 64:65]),
                        [pY.b[half]], rden.b)
                    tt("dve", On.t[:, half * 256:(half + 1) * 256].rearrange("p (h d) -> p h d", h=4),
                       o4[:, :, 0:64], rden.t[:, half * 4:half * 4 + 4].unsqueeze(2).broadcast_to([128, 4, 64]),
                       ALU.mult, [pY.b[half]] + rden.b, On.b)
                pb, pbb = nextL()
                for q in range(4):
                    tr(pbb[:, q * 128:(q + 1) * 128], On.t[:, q * 128:(q + 1) * 128], ident.t[:], On.b + ident.b, pb.b)
                cp("act", OnT.t[:].rearrange("p a b -> p (a b)"), pbb[:, 0:512], pb.b, OnT.b)
                yield 1.5
                for cc in range(2):
                    for kc in range(4):
                        mm(pY.t[:, cc * 512:(cc + 1) * 512], OnT.t[:, kc, :], wba.t[:, kc, cc * 512:(cc + 1) * 512],
                           kc == 0, kc == 3, OnT.b + wba.b, [pY.b[cc]])
                tt("dve", fB.t[:], pY.t[:], tq.t[:, 2560:3584], ALU.mult, pY.b + tq.b, fB.b)
                yield 2.5
                tt("dve", ub.t[:], fB.t[:], fA.t[:], ALU.add, fB.b + fA.b, ub.b)
                yield 4.0
                pb, pbb = nextL()
                for kc in range(8):
                    tr(pbb[:, kc * 128:(kc + 1) * 128], ub.t[:, kc * 128:(kc + 1) * 128], ident.t[:], ub.b + ident.b, pb.b)
                cp("act", uT.t[:].rearrange("p a b -> p (a b)"), pbb[:, :], pb.b, uT.b)
                for cc in range(2):
                    for kc in range(8):
                        mm(pY.t[:, cc * 512:(cc + 1) * 512], uT.t[:, kc, :], wo.t[:, kc, cc * 512:(cc + 1) * 512],
                           kc == 0, kc == 7, uT.b + wo.b, [pY.b[cc]])
                yield 4.0
                act(ub.t[:], pY.t[:], AF.Square, pY.b, ub.b + sm2.b, accum=sm2.t[:, 0:1])
                rstd_from_ss(sm2.t[:, 0:1], sm2.t[:, 1:2], sm2.t[:, 2:3], 1.0 / D, sm2.b, sm2.b)
                stt("dve", fB.t[:], pY.t[:], sm2.t[:, 2:3], g2.t[:], ALU.mult, ALU.mult, pY.b + sm2.b + g2.b, fB.b)
                dma("sp", x1_d[i * 128:(i + 1) * 128, :], fB.t[:], "st_x1", fB.b, [])
                yield 2.5

            def drive(gy, gx):
                cy = cx = 0.0
                while gy is not None or gx is not None:
                    if gy is not None and (gx is None or cy <= cx):
                        try:
                            cy += next(gy)
                        except StopIteration:
                            gy = None
                    else:
                        try:
                            cx += next(gx)
                        except StopIteration:
                            gx = None

            drive(None, X_gen(0))
            for i in range(NT):
                drive(Y_gen(i), X_gen(i + 1) if i + 1 < NT else None)

            fence_b = fB.b
            allB = fence_b + kT.b + V.b + ikT.b + wba.b + wbr.b + wo.b + ggnT.b + g2.b + cmask.b + decT.b + epsg.b \
                + bigI.b + tmq.b
            for lst in (fmq, iwq, Th, PT, attb, maskT, pS, pL):
                for x_ in lst:
                    allB += x_.b
            for x_ in (Sc, maskb, jnkA, bsd, bsa, bs, wk, pw2, rden, On, OnT, Rf, Rb, gst, gmv, gsm, fA, fB, ub, uT,
                       sm2, ptX, pY):
                allB += x_.b
        for e in Sched.ENGS:
            S.wait_all(e, allB)

        if stop_after == "B":
            S.wait_all("sp", allB)
            S.emit()
            return nc

        GT = 2 if NT % 2 == 0 else 1
        NG = NT // GT
        GW = GT * 128
        with ExitStack() as sc:
            def sbc(name, shape, dt, n=1):
                return TB(sc.enter_context(nc.sbuf_tensor(name, list(shape), dt)), n)

            def psc_(name, shape, dt, n=1):
                return TB(sc.enter_context(nc.psum_tensor(name, list(shape), dt)), n, True)

            wup = sbc("wup", [128, 8, 2 * DFF], BF16, 8)
            wdn = sbc("wdn", [128, 22, D], BF16, 22)
            g3 = sbc("g3", [128, D], F32)
            g4 = sbc("g4", [128, D], F32)
            cw = sbc("cw", [128, 44, 3], F32)
            cb = sbc("cb", [128, 44], F32)
            hal = sbc("hal", [128, 44, 2], F32, 44)
            xc = [sbc(f"xc{j}", [128, D], F32) for j in range(2 * GT)]
            r1c = [sbc(f"r1c{j}", [128, D], F32) for j in range(2)]
            outc = [sbc(f"outc{j}", [128, D], F32) for j in range(2)]
            hbc = sbc("hbc", [128, D], BF16)
            junkc = sbc("junkc", [128, D], BF16)
            sm3 = [sbc(f"sm3_{j}", [128, 8], F32) for j in range(2)]
            sm4 = [sbc(f"sm4_{j}", [128, 8], F32) for j in range(2)]
            hTg2 = [sbc(f"hTg{j}", [128, 8, GW], BF16) for j in range(2)]
            ubuf = [sbc(f"ubuf{j}", [128, GW + 2], F32) for j in range(3)]
            accg = [sbc(f"accg{j}", [128, GW], F32) for j in range(2)]
            accu = [sbc(f"accu{j}", [128, GW], F32) for j in range(2)]
            sg = accg
            aT = sbc("aT", [128, 22, GW], BF16, 22)
            pup = [psc_(f"pup{j}", [128, 512], F32) for j in range(3)]
            ptc = psc_("ptc", [128, 1024], BF16)
            pdn = psc_("pdn", [128, 1024], F32, 2)

            for kc in range(8):
                dma("pool", wup.t[:, kc, :], wup_d[kc * 128:(kc + 1) * 128, :], f"ld_wup{kc}", [], [wup.b[kc]])
            for fc in range(22):
                dma("pool", wdn.t[:, fc, :], wdn_d[fc * 128:(fc + 1) * 128, :], f"ld_wdn{fc % 4}", [], [wdn.b[fc]])
            cdma(g3.t[:], g3_d.broadcast_to([128, D]), g3.b)
            cdma(g4.t[:], g4_d.broadcast_to([128, D]), g4.b)
            cdma(cw.t[:], cw_d, cw.b)
            cdma(cb.t[:], cb_d, cb.b)
            S.op("pool", lambda e: e.memset(hal.t[:], 0.0), [], hal.b)

            ukc = {"k": 0}

            def driveC(gy, gx):
                cy = cx = 0.0
                while gy is not None or gx is not None:
                    if gy is not None and (gx is None or cy <= cx):
                        try:
                            cy += next(gy)
                        except StopIteration:
                            gy = None
                    else:
                        try:
                            cx += next(gx)
                        except StopIteration:
                            gx = None

            def PC_gen(g):
                hTg = hTg2[g % 2]
                for tl in range(GT):
                    i = g * GT + tl
                    a = i % 2
                    xt, smt, r1t = xc[(g % 2) * GT + tl], sm3[a], r1c[a]
                    dma("sp", xt.t[:], x_d[i * 128:(i + 1) * 128, :], f"ld_xc{(g % 2) * GT + tl}", [], xt.b)
                    dma("sp", r1t.t[:], x1_d[i * 128:(i + 1) * 128, :], f"ld_r1c{a}", fence_b, r1t.b)
                    tt("dve", xt.t[:], xt.t[:], r1t.t[:], ALU.add, xt.b + r1t.b, xt.b)
                    act(hbc.t[:], xt.t[:], AF.Square, xt.b, hbc.b + smt.b, accum=smt.t[:, 0:1])
                    rstd_from_ss(smt.t[:, 0:1], smt.t[:, 1:2], smt.t[:, 2:3], 1.0 / D, smt.b, smt.b)
                    stt("dve", hbc.t[:], xt.t[:], smt.t[:, 2:3], g3.t[:], ALU.mult, ALU.mult, xt.b + smt.b + g3.b, hbc.b)
                    yield 14.0
                    for kc in range(8):
                        tr(ptc.t[:, kc * 128:(kc + 1) * 128], hbc.t[:, kc * 128:(kc + 1) * 128], ident.t[:],
                           hbc.b + ident.b, ptc.b)
                    cp("act", hTg.t[:, :, tl * 128:(tl + 1) * 128],
                       ptc.t[:].rearrange("p (a b) -> p a b", a=8), ptc.b, hTg.b)
                    yield 1.0

            def MC_gen(g):
                hTg = hTg2[g % 2]
                for fc in range(22):
                    for which, c in ((0, fc), (1, fc + 22)):
                        pb = pup[ukc["k"] % 3]
                        ukc["k"] += 1
                        for kc in range(8):
                            mm(pb.t[:, 0:GW], wup.t[:, kc, c * 128:(c + 1) * 128], hTg.t[:, kc, :], kc == 0, kc == 7,
                               [wup.b[kc]] + hTg.b, pb.b)
                        accx = (accg if which == 0 else accu)[fc % 2]
                        ubf = ubuf[ukc["k"] % 3]
                        cp("pool", ubf.t[:, 0:2], hal.t[:, c, :], [hal.b[c]], ubf.b)
                        cp("act", ubf.t[:, 2:GW + 2], pb.t[:, 0:GW], pb.b, ubf.b)
                        cp("pool", hal.t[:, c, :], ubf.t[:, GW:GW + 2], ubf.b, [hal.b[c]])
                        act(accx.t[:], pb.t[:, 0:GW], AF.Identity, pb.b + cw.b + cb.b, accx.b,
                            bias=cb.t[:, c:c + 1], scale=cw.t[:, c, 2:3])
                        stt("dve", accx.t[:], ubf.t[:, 1:GW + 1], cw.t[:, c, 1:2], accx.t[:], ALU.mult, ALU.add,
                            ubf.b + cw.b + accx.b, accx.b)
                        stt("dve", accx.t[:], ubf.t[:, 0:GW], cw.t[:, c, 0:1], accx.t[:], ALU.mult, ALU.add,
                            ubf.b + cw.b + accx.b, accx.b)
                        yield 0.9
                    ag, au = accg[fc % 2], accu[fc % 2]
                    act(ag.t[:], ag.t[:], AF.Silu, ag.b, ag.b)
                    tt("dve", aT.t[:, fc, :], ag.t[:], au.t[:], ALU.mult, ag.b + au.b, [aT.b[fc]])
                for tl in range(GT):
                    i = g * GT + tl
                    a = i % 2
                    for cc in range(2):
                        for fc in range(22):
                            mm(pdn.t[:, cc * 512:(cc + 1) * 512], aT.t[:, fc, tl * 128:(tl + 1) * 128],
                               wdn.t[:, fc, cc * 512:(cc + 1) * 512], fc == 0, fc == 21,
                               [aT.b[fc], wdn.b[fc]], [pdn.b[cc]])
                    smt = sm4[a]
                    act(junkc.t[:], pdn.t[:], AF.Square, pdn.b, junkc.b + smt.b, accum=smt.t[:, 4:5])
                    rstd_from_ss(smt.t[:, 4:5], smt.t[:, 5:6], smt.t[:, 6:7], 1.0 / D, smt.b, smt.b)
                    xrt, ot = xc[(g % 2) * GT + tl], outc[a]
                    stt("dve", ot.t[:], pdn.t[:], smt.t[:, 6:7], g4.t[:], ALU.mult, ALU.mult, pdn.b + smt.b + g4.b, ot.b)
                    tt("dve", ot.t[:], ot.t[:], xrt.t[:], ALU.add, ot.b + xrt.b, ot.b)
                    dma("sp", out_d[i * 128:(i + 1) * 128, :], ot.t[:], f"st_out{a}", ot.b, [])
                    yield 9.5

            driveC(None, PC_gen(0))
            for g in range(NG):
                driveC(MC_gen(g), PC_gen(g + 1) if g + 1 < NG else None)
            S.wait_all("sp", outc[0].b + outc[1].b)
        S.emit()
    return nc


def _consts():
    invf128 = 10000.0 ** (-(np.arange(64, dtype=np.float32) * 2.0 / 128)).astype(np.float32)
    invf64 = 10000.0 ** (-(np.arange(32, dtype=np.float32) * 2.0 / 64)).astype(np.float32)
    invf = np.concatenate([invf128, invf64]).astype(np.float32)
    c_invf = np.ascontiguousarray(np.broadcast_to(invf[None, :], (128, 96))).astype(np.float32)
    c_ident = np.eye(128, dtype=np.float32).astype(ml_dtypes.bfloat16)
    jj = np.arange(128)
    c_cmask = np.where(jj[None, :] <= jj[:, None], 0.0, NEG).astype(np.float32)
    gam = 1.0 - 2.0 ** (-5.0 - np.arange(4, dtype=np.float64))
    c = 128.0 ** -0.5
    decT = np.zeros((128, 4, 128), np.float64)
    for h in range(4):
        decT[:, h, :] = (c * gam[h] ** (-(jj[:, None] + 1.0))) * (jj[:, None] <= jj[None, :])
    zc = c * gam[None, :] ** (127.0 - jj[:, None])
    xi = gam[None, :] ** (jj[:, None] + 1.0)
    epsg = EPS / xi ** 2
    return dict(c_invf=c_invf, c_ident=c_ident, c_cmask=c_cmask, c_decT=decT.astype(np.float32),
                c_zc=zc.astype(np.float32), c_epsg=epsg.astype(np.float32))


def make_in_maps(inputs, NT=NTF, cores=8):
    T = NT * 128
    f = lambda a: np.ascontiguousarray(np.asarray(a, dtype=np.float32))
    cw = f(inputs["conv_w"])[0]
    cw_l = np.ascontiguousarray(cw.reshape(3, 44, 128).transpose(2, 1, 0))
    cb_l = np.ascontiguousarray(f(inputs["conv_b"])[0].reshape(44, 128).T)
    shared = dict(
        g_pre_mix=f(inputs["norm_pre_mix"]), w_in=f(inputs["w_in"])[0], w_br_attn=f(inputs["w_br_attn"])[0],
        w_br_ret=f(inputs["w_br_ret"])[0],
        ggnT=np.ascontiguousarray(f(inputs["ret_gn_gain"])[0].reshape(8, 128).T), w_out=f(inputs["w_out"])[0],
        g_post_mix=f(inputs["norm_post_mix"]), g_pre_ffn=f(inputs["norm_pre_ffn"]),
        w_ffn_up=f(inputs["w_ffn_up"])[0], conv_w=cw_l, conv_b=cb_l, w_ffn_down=f(inputs["w_ffn_down"])[0],
        g_post_ffn=f(inputs["norm_post_ffn"]),
    )
    shared.update(_consts())
    x = np.asarray(inputs["x"], dtype=np.float32)
    pos = np.asarray(inputs["positions"]).astype(np.int32)
    maps = []
    for b in range(cores):
        m = dict(shared)
        m["x"] = np.ascontiguousarray(x[b, :T])
        m["pos"] = np.ascontiguousarray(pos[b, :T].reshape(NT, 128).T)
        maps.append(m)
    return maps


_NC_CACHE = {}


def kernel(**inputs):
    if "full" not in _NC_CACHE:
        _NC_CACHE["full"] = build_program(NTF)
    nc = _NC_CACHE["full"]
    maps = make_in_maps(inputs, NTF, 8)
    res = run_bass_kernel_spmd(nc, maps, core_ids=list(range(8)))
    out = np.stack([np.asarray(r["out"], dtype=np.float32) for r in res.results], axis=0)
    return out
```

```python
import numpy as np
import ml_dtypes
from contextlib import ExitStack
import concourse.bass as bass
import concourse.mybir as mybir
from concourse.bass_utils import run_bass_kernel_spmd

F32 = mybir.dt.float32
BF16 = mybir.dt.bfloat16
I32 = mybir.dt.int32
ALU = mybir.AluOpType
AF = mybir.ActivationFunctionType
AX = mybir.AxisListType

D = 1024
T_FULL = 4096
NTF = 32
INW = 7240
DFF = 2816
EPS = 1e-6
TOPK = 256
NEG = -1.0e30
NBIS = 20
TWO_PI = float(2 * np.pi)


class Buf:
    __slots__ = ("wr", "rd")

    def __init__(self):
        self.wr = None
        self.rd = []


class Sched:
    ENGS = ("pe", "dve", "act", "pool", "sp")

    def __init__(self, nc, stack):
        self.nc = nc
        self.stack = stack
        self.lists = {e: [] for e in self.ENGS}
        self.sems = {}
        self.count = {}
        self.known = {e: {} for e in self.ENGS}
        for e in self.ENGS:
            self._newsem("E_" + e)

    def _newsem(self, key):
        s = self.stack.enter_context(self.nc.semaphore(key))
        self.sems[key] = s
        self.count[key] = 0

    def _deps(self, eng, reads, writes):
        deps = {}
        own = "E_" + eng

        def add(ev):
            if ev is None:
                return
            k, v = ev
            if eng == "pe" and k == own:
                return
            if deps.get(k, 0) < v:
                deps[k] = v
        for b in reads:
            add(b.wr)
        for b in writes:
            add(b.wr)
            for ev in b.rd:
                add(ev)
        waits = []
        kn = self.known[eng]
        for k, v in deps.items():
            if kn.get(k, 0) < v:
                kn[k] = v
                waits.append((k, v))
        return waits

    def _post(self, ev, reads, writes):
        for b in reads:
            b.rd.append(ev)
        for b in writes:
            b.wr = ev
            b.rd = []

    def op(self, eng, fn, reads=(), writes=()):
        waits = self._deps(eng, reads, writes)
        key = "E_" + eng
        self.count[key] += 1
        ev = (key, self.count[key])
        self.lists[eng].append((waits, fn, key, 1, self.count[key]))
        self._post(ev, reads, writes)

    def dma(self, eng, fn, semkey, reads=(), writes=()):
        if semkey not in self.sems:
            self._newsem(semkey)
        waits = self._deps(eng, reads, writes)
        self.count[semkey] += 16
        ev = (semkey, self.count[semkey])
        self.lists[eng].append((waits, fn, semkey, 16, None))
        self._post(ev, reads, writes)

    def wait_all(self, eng, bufs):
        waits = self._deps(eng, bufs, bufs)
        self.lists[eng].append((waits, None, None, 0, None))

    def emit(self):
        sems = self.sems
        lists = self.lists
        needed = {}
        for e in self.ENGS:
            for waits, fn, key, inc, seq in lists[e]:
                for k, v in waits:
                    if k.startswith("E_"):
                        needed.setdefault(k, set()).add(v)
        rank = {}
        for e in self.ENGS:
            key = "E_" + e
            need = needed.get(key, set())
            m = {}
            r = 0
            for waits, fn, k, inc, seq in lists[e]:
                if seq is not None and seq in need:
                    r += 1
                    m[seq] = r
            rank[key] = m

        def replay(e, items):
            for waits, fn, key, inc, seq in items:
                for k, v in waits:
                    if k.startswith("E_"):
                        v = rank[k][v]
                    e.wait_ge(sems[k], v)
                if fn is not None:
                    ins = fn(e)
                    if seq is None:
                        ins.then_inc(sems[key], inc)
                    elif seq in rank[key]:
                        ins.then_inc(sems[key], 1)

        with self.nc.Block() as block:
            @block.tensor
            def _(e):
                replay(e, lists["pe"])

            @block.vector
            def _(e):
                replay(e, lists["dve"])

            @block.scalar
            def _(e):
                replay(e, lists["act"])

            @block.gpsimd
            def _(e):
                replay(e, lists["pool"])

            @block.sync
            def _(e):
                replay(e, lists["sp"])
        self.stats = {k: len(v) for k, v in rank.items()}


class TB:
    def __init__(self, t, n=1):
        self.t = t
        self.b = [Buf() for _ in range(n)]


def build_program(NT=NTF, dbg=False, stop_after=None):
    nc = bass.Bass("TRN2", target_bir_lowering=False)
    T = NT * 128

    def din(name, shape, dt=F32):
        return nc.dram_tensor(name, list(shape), dt, kind="ExternalInput").ap()

    def dscr(name, shape, dt):
        return nc.dram_tensor(name, list(shape), dt, kind=("ExternalOutput" if dbg else "Internal")).ap()

    x_d = din("x", [T, D])
    pos_d = din("pos", [128, NT], I32)
    g1_d = din("g_pre_mix", [1, D])
    w_in_d = din("w_in", [D, INW])
    wba_d = din("w_br_attn", [512, D])
    wbr_d = din("w_br_ret", [D, D])
    ggnT_d = din("ggnT", [128, 8])
    wo_d = din("w_out", [D, D])
    g2_d = din("g_post_mix", [1, D])
    g3_d = din("g_pre_ffn", [1, D])
    wup_d = din("w_ffn_up", [D, 2 * DFF])
    cw_d = din("conv_w", [128, 44, 3])
    cb_d = din("conv_b", [128, 44])
    wdn_d = din("w_ffn_down", [DFF, D])
    g4_d = din("g_post_ffn", [1, D])
    invf_d = din("c_invf", [128, 96])
    ident_d = din("c_ident", [128, 128], BF16)
    cmask_d = din("c_cmask", [128, 128])
    decT_d = din("c_decT", [128, 4, 128])
    zc_d = din("c_zc", [128, 4])
    epsg_d = din("c_epsg", [128, 4])
    out_d = nc.dram_tensor("out", [T, D], F32, kind="ExternalOutput").ap()

    fm_d = dscr("s_fm", [NT, 128, 2048], BF16)
    kT_d = dscr("s_kT", [NT, 128, 512], BF16)
    ikT_d = dscr("s_ikT", [64, T], BF16)
    iw_d = dscr("s_iw", [T, 8], F32)
    v_d = dscr("s_v", [T, 520], BF16)
    tm_d = dscr("s_tm", [T, 4608], BF16)
    x1_d = dscr("s_x1", [T, D], F32)

    cdec = [float((1.0 - 2.0 ** (-5.0 - h)) ** 128) for h in range(4)]

    with ExitStack() as st:
        S = Sched(nc, st)

        def sb(name, shape, dt, n=1):
            return TB(st.enter_context(nc.sbuf_tensor(name, list(shape), dt)), n)

        def ps(name, shape, dt, n=1):
            return TB(st.enter_context(nc.psum_tensor(name, list(shape), dt)), n)

        def mm(out, lhsT, rhs, start, stop, reads, writes):
            S.op("pe", lambda e: e.matmul(out, lhsT=lhsT, rhs=rhs, start=start, stop=stop), reads, writes)

        def tr(out, in_, ident_ap, reads, writes):
            S.op("pe", lambda e: e.transpose(out=out, in_=in_, identity=ident_ap), reads, writes)

        def act(out, in_, func, reads, writes, bias=None, scale=None, accum=None):
            kw = {}
            if bias is not None:
                kw["bias"] = bias
            if scale is not None:
                kw["scale"] = scale
            if accum is not None:
                kw["accum_out"] = accum
            S.op("act", lambda e: e.activation(out=out, in_=in_, func=func, **kw), reads, writes)

        def tt(eng, out, in0, in1, op, reads, writes):
            S.op(eng, lambda e: e.tensor_tensor(out=out, in0=in0, in1=in1, op=op), reads, writes)

        def ts(eng, out, in0, s1, s2, op0, op1, reads, writes, accum=None):
            kw = {}
            if op1 is not None:
                kw["op1"] = op1
            if accum is not None:
                kw["accum_out"] = accum
            S.op(eng, lambda e: e.tensor_scalar(out=out, in0=in0, scalar1=s1, scalar2=s2, op0=op0, **kw), reads, writes)

        def stt(eng, out, in0, scalar, in1, op0, op1, reads, writes):
            S.op(eng, lambda e: e.scalar_tensor_tensor(out=out, in0=in0, scalar=scalar, in1=in1, op0=op0, op1=op1),
                 reads, writes)

        def cp(eng, out, in_, reads, writes):
            if eng == "act":
                S.op("act", lambda e: e.activation(out=out, in_=in_, func=AF.Copy), reads, writes)
            else:
                S.op(eng, lambda e: e.tensor_copy(out=out, in_=in_), reads, writes)

        def dma(eng, out, in_, key, reads, writes):
            S.dma(eng, lambda e: e.dma_start(out=out, in_=in_), key, reads, writes)

        chain = Buf()

        def cdma(out, in_, writes):
            S.dma("sp", lambda e: e.dma_start(out=out, in_=in_), "ld_misc", [], list(writes) + [chain])

        ident = sb("ident", [128, 128], BF16)
        epsc = sb("epsc", [128, 1], F32)
        cdma(ident.t[:], ident_d, ident.b)
        S.op("dve", lambda e: e.memset(epsc.t[:], EPS), [], epsc.b)

        def rstd_from_ss(ss_ap, ln_ap, out_ap, inv_n, reads, writes):
            act(ln_ap, ss_ap, AF.Ln, list(reads) + epsc.b, writes, bias=epsc.t[:, 0:1], scale=inv_n)
            act(out_ap, ln_ap, AF.Exp, writes, writes, scale=-0.5)

        with ExitStack() as sa:
            def sba(name, shape, dt, n=1):
                return TB(sa.enter_context(nc.sbuf_tensor(name, list(shape), dt)), n)

            def psa(name, shape, dt, n=1):
                return TB(sa.enter_context(nc.psum_tensor(name, list(shape), dt)), n)

            win = sba("win", [128, 8, INW], BF16, 8)
            gbc = sba("gbc", [128, D], F32)
            invf = sba("invf", [128, 96], F32)
            posi = sba("posi", [128, NT], I32)
            posf = sba("posf", [128, NT], F32)
            rt = [sba(f"rt{j}", [128, 3, 96], F32) for j in range(2)]
            ang = sba("ang", [128, 96], F32)
            angk = sba("angk", [128, 96], F32)
            angi = sba("angi", [128, 96], I32)
            zc = sba("zc", [128, 4], F32)
            xin = [sba(f"xin{j}", [128, D], F32) for j in range(2)]
            junkb = sba("junkb", [128, D], BF16)
            sm = [sba(f"smA{j}", [128, 8], F32) for j in range(2)]
            hb = sba("hb", [128, D], BF16)
            hT = [sba(f"hT{j}", [128, 8, 128], BF16) for j in range(2)]
            t1 = [sba(f"t1_{j}", [128, 512], F32) for j in range(2)]
            t2 = [sba(f"t2_{j}", [128, 512], F32) for j in range(2)]
            rob = [sba(f"rob{j}", [128, 512], BF16) for j in range(3)]
            tm = [sba(f"tmA{j}", [128, 4608], BF16, 9) for j in range(2)]
            fm = [sba(f"fmA{j}", [128, 2048], BF16, 4) for j in range(2)]
            kTs = [sba(f"kTs{j}", [128, 512], BF16) for j in range(2)]
            vs = [sba(f"vs{j}", [128, 8, 65], BF16) for j in range(2)]
            iks = [sba(f"iks{j}", [64, 128], BF16) for j in range(2)]
            iws = [sba(f"iws{j}", [128, 8], F32) for j in range(2)]
            pj = [psa(f"pj{j}", [128, 512], F32) for j in range(5)]
            ptr = [psa(f"ptrA{j}", [128, 1024], BF16) for j in range(2)]

            for kc in range(8):
                dma("pool", win.t[:, kc, :], w_in_d[kc * 128:(kc + 1) * 128, :], f"ld_win{kc}", [], [win.b[kc]])
            cdma(gbc.t[:], g1_d.broadcast_to([128, D]), gbc.b)
            cdma(invf.t[:], invf_d, invf.b)
            cdma(posi.t[:], pos_d, posi.b)
            cdma(zc.t[:], zc_d, zc.b)
            for j in range(2):
                S.op("pool", lambda e, j=j: e.memset(vs[j].t[:], 1.0), [], vs[j].b)

            cp("dve", posf.t[:], posi.t[:], posi.b, posf.b)

            def range_reduce(shift):
                ts("dve", angk.t[:], ang.t[:], shift, 1.0 / TWO_PI, ALU.add, ALU.mult, ang.b, angk.b)
                cp("dve", angi.t[:], angk.t[:], angk.b, angi.b)
                cp("dve", angk.t[:], angi.t[:], angi.b, angk.b)
                ts("dve", angk.t[:], angk.t[:], -TWO_PI, shift, ALU.mult, ALU.add, angk.b, angk.b)
                tt("dve", angk.t[:], angk.t[:], ang.t[:], ALU.add, angk.b + ang.b, angk.b)
                ts("dve", angk.t[:], angk.t[:], float(np.pi), -float(np.pi), ALU.min, ALU.max, angk.b, angk.b)

            def make_tables(i):
                r_ = rt[i % 2]
                ts("dve", ang.t[:], invf.t[:], posf.t[:, i:i + 1], None, ALU.mult, None, invf.b + posf.b, ang.b)
                range_reduce(float(np.pi / 2))
                act(r_.t[:, 0, :], angk.t[:], AF.Sin, angk.b, r_.b)
                range_reduce(0.0)
                act(r_.t[:, 2, :], angk.t[:], AF.Sin, angk.b, r_.b)
                ts("dve", r_.t[:, 1, :], r_.t[:, 2, :], -1.0, None, ALU.mult, None, r_.b, r_.b)

            def rotary(i, src, nh, half, toff, dst, reads, writes, k):
                a1, a2 = t1[k % 2], t2[k % 2]
                rti = rt[i % 2]
                w = nh * 2 * half
                s4 = src.rearrange("p (h t j) -> p h t j", h=nh, t=2, j=half)
                o1 = a1.t[:, 0:w].rearrange("p (h t j) -> p h t j", h=nh, t=2, j=half)
                o2 = a2.t[:, 0:w].rearrange("p (h t j) -> p h t j", h=nh, t=2, j=half)
                cosb = rti.t[:, 0, toff:toff + half].unsqueeze(1).unsqueeze(1).broadcast_to([128, nh, 2, half])
                nsin = rti.t[:, 1, toff:toff + half].unsqueeze(1).broadcast_to([128, nh, half])
                psin = rti.t[:, 2, toff:toff + half].unsqueeze(1).broadcast_to([128, nh, half])
                tt("dve", o1, s4, cosb, ALU.mult, list(reads) + rti.b, a1.b)
                tt("dve", o2[:, :, 0, :], s4[:, :, 1, :], nsin, ALU.mult, list(reads) + rti.b, a2.b)
                tt("dve", o2[:, :, 1, :], s4[:, :, 0, :], psin, ALU.mult, list(reads) + rti.b, a2.b)
                tt("dve", dst, a1.t[:, 0:w], a2.t[:, 0:w], ALU.add, a1.b + a2.b, writes)

            ncol = [512] * 4 + [72] + [512] * 10
            coff = [0]
            for c in ncol:
                coff.append(coff[-1] + c)
            rotc = {"k": 0}

            def drive(gy, gx):
                cy = cx = 0.0
                while gy is not None or gx is not None:
                    if gy is not None and (gx is None or cy <= cx):
                        try:
                            cy += next(gy)
                        except StopIteration:
                            gy = None
                    else:
                        try:
                            cx += next(gx)
                        except StopIteration:
                            gx = None

            def PA_gen(i):
                a = i % 2
                xt, smt, hTt = xin[a], sm[a], hT[a]
                dma("sp", xt.t[:], x_d[i * 128:(i + 1) * 128, :], f"ld_x{a}", [], xt.b)
                make_tables(i)
                act(junkb.t[:], xt.t[:], AF.Square, xt.b, junkb.b + smt.b, accum=smt.t[:, 0:1])
                rstd_from_ss(smt.t[:, 0:1], smt.t[:, 1:2], smt.t[:, 2:3], 1.0 / D, smt.b, smt.b)
                stt("dve", hb.t[:], xt.t[:], smt.t[:, 2:3], gbc.t[:], ALU.mult, ALU.mult,
                    xt.b + smt.b + gbc.b, hb.b)
                yield 12.0
                pt = ptr[0]
                for kc in range(8):
                    tr(pt.t[:, kc * 128:(kc + 1) * 128], hb.t[:, kc * 128:(kc + 1) * 128], ident.t[:],
                       hb.b + ident.b, pt.b)
                cp("act", hTt.t[:].rearrange("p a b -> p (a b)"), pt.t[:], pt.b, hTt.b)
                yield 1.0

            def MA_gen(i):
                a = i % 2
                hTt, tmt, fmt = hT[a], tm[a], fm[a]
                pend = []

                def flush(item):
                    c, r = item
                    pt = ptr[1]
                    if c == 4:
                        tr(pt.t[0:64, 0:128], r.t[:, 0:64], ident.t[:], r.b + ident.b, pt.b)
                        cp("act", iks[a].t[:], pt.t[0:64, 0:128], pt.b, iks[a].b)
                        dma("sp", ikT_d[:, i * 128:(i + 1) * 128], iks[a].t[:], f"st_ik{a}", iks[a].b, [])
                        return
                    for q in range(4):
                        tr(pt.t[:, q * 128:(q + 1) * 128], r.t[:, q * 128:(q + 1) * 128], ident.t[:],
                           r.b + ident.b, pt.b)
                    if c == 1:
                        cp("act", kTs[a].t[:], pt.t[:, 0:512], pt.b, kTs[a].b)
                        dma("sp", kT_d[i], kTs[a].t[:], f"st_kT{a}", kTs[a].b, [])
                    else:
                        slot = {0: 0, 3: 1, 5: 2, 6: 3}[c]
                        cp("act", fmt.t[:, slot * 512:(slot + 1) * 512], pt.t[:, 0:512], pt.b, [fmt.b[slot]])

                for c in range(15):
                    pb = pj[c % 5]
                    w = ncol[c]
                    for kc in range(8):
                        mm(pb.t[:, 0:w], hTt.t[:, kc, :], win.t[:, kc, coff[c]:coff[c] + w], kc == 0, kc == 7,
                           hTt.b + [win.b[kc]], pb.b)
                    if c in (0, 1, 3, 5, 6):
                        r = rob[rotc["k"] % 3]
                        if c in (5, 6):
                            rotary(i, pb.t[:, :], 4, 64, 0, r.t[:, 0:512], pb.b, r.b, rotc["k"])
                        else:
                            rotary(i, pb.t[:, :], 8, 32, 64, r.t[:, 0:512], pb.b, r.b, rotc["k"])
                        rotc["k"] += 1
                        if c == 6:
                            tt("dve", tmt.t[:, 0:512].rearrange("p (h d) -> p h d", h=4),
                               r.t[:].rearrange("p (h d) -> p h d", h=4),
                               zc.t[:].unsqueeze(2).broadcast_to([128, 4, 128]), ALU.mult,
                               r.b + zc.b, [tmt.b[0]])
                        pend.append((c, r))
                    elif c == 2:
                        cp("act", vs[a].t[:, :, 0:64], pb.t[:, :].rearrange("p (h d) -> p h d", h=8), pb.b, vs[a].b)
                        dma("sp", v_d[i * 128:(i + 1) * 128, :], vs[a].t[:].rearrange("p h d -> p (h d)"),
                            f"st_v{a}", vs[a].b, [])
                    elif c == 4:
                        r = rob[rotc["k"] % 3]
                        rotary(i, pb.t[:, 0:64], 1, 32, 64, r.t[:, 0:64], pb.b, r.b, rotc["k"])
                        rotc["k"] += 1
                        ts("dve", iws[a].t[:], pb.t[:, 64:72], float(8 ** -0.5 * 64 ** -0.5), None, ALU.mult, None,
                           pb.b, iws[a].b)
                        dma("sp", iw_d[i * 128:(i + 1) * 128, :], iws[a].t[:], f"st_iw{a}", iws[a].b, [])
                        pend.append((c, r))
                    elif c in (7, 8):
                        o = 512 + (c - 7) * 512
                        cp("act", tmt.t[:, o:o + 512], pb.t[:, :], pb.b, [tmt.b[1 + c - 7]])
                    elif c in (9, 10):
                        o = 1536 + (c - 9) * 512
                        act(tmt.t[:, o:o + 512], pb.t[:, :], AF.Silu, pb.b, [tmt.b[3 + c - 9]])
                    else:
                        o = 2560 + (c - 11) * 512
                        act(tmt.t[:, o:o + 512], pb.t[:, :], AF.Sigmoid, pb.b, [tmt.b[5 + c - 11]])
                    while pend and pend[0][0] <= c - 2:
                        flush(pend.pop(0))
                    yield 1.7
                while pend:
                    flush(pend.pop(0))
                dma("sp", fm_d[i], fmt.t[:], f"st_fm{a}", fmt.b, [])
                dma("sp", tm_d[i * 128:(i + 1) * 128, :], tmt.t[:], f"st_tm{a}", tmt.b, [])
                yield 0.5

            drive(None, PA_gen(0))
            for i in range(NT):
                drive(MA_gen(i), PA_gen(i + 1) if i + 1 < NT else None)
            fence_a = []
            for a in range(2):
                fence_a += tm[a].b + fm[a].b + kTs[a].b + vs[a].b + iks[a].b + iws[a].b
            allA = fence_a + win.b + gbc.b + invf.b + posi.b + posf.b + rt[0].b + rt[1].b + ang.b + angk.b + angi.b + zc.b + \
                xin[0].b + xin[1].b + junkb.b + sm[0].b + sm[1].b + hb.b + hT[0].b + hT[1].b + \
                t1[0].b + t1[1].b + t2[0].b + t2[1].b + rob[0].b + rob[1].b + rob[2].b + \
                [p.b[0] for p in pj] + [p.b[0] for p in ptr]
        for e in Sched.ENGS:
            S.wait_all(e, allA)

        if stop_after == "A":
            S.wait_all("sp", allA)
            S.emit()
            return nc

        with ExitStack() as sbk:
            def sbb(name, shape, dt, n=1):
                return TB(sbk.enter_context(nc.sbuf_tensor(name, list(shape), dt)), n)

            def psb(name, shape, dt, n=1):
                return TB(sbk.enter_context(nc.psum_tensor(name, list(shape), dt)), n)

            NGC = (NT + 7) // 8
            kT = sbb("kTc", [128, NT, 512], BF16, NGC)
            V = sbb("Vc", [128, NT, 520], BF16, NGC)
            ikT = sbb("ikTc", [128, T], BF16, NGC)
            wba = sbb("wba", [128, 4, D], BF16, 4)
            wbr = sbb("wbr", [128, 8, D], BF16, 8)
            wo = sbb("wo", [128, 8, D], BF16, 8)
            ggnT = sbb("ggnT_sb", [128, 8], F32)
            g2 = sbb("g2", [128, D], F32)
            cmask = sbb("cmask", [128, 128], F32)
            decT = sbb("decT", [128, 4, 128], F32)
            epsg = sbb("epsg", [128, 4], F32)
            bigI = sbb("bigI", [128, 128], BF16)
            fmq = [sbb(f"fmq{j}", [128, 2048], BF16) for j in range(2)]
            tmq = sbb("tmq0", [128, 4608], BF16)
            iwq = [sbb(f"iwq{j}", [128, 8], F32) for j in range(2)]
            Th = [sbb(f"Th{j}", [128, 512], BF16) for j in range(3)]
            PT = [sbb(f"PT{j}", [128, 512], BF16) for j in range(3)]
            Sc = sbb("Sc", [128, T], F32)
            maskb = sbb("maskb", [128, T], BF16)
            maskT = [sbb(f"maskT{j}", [128, NT, 128], BF16) for j in range(2)]
            bs = sbb("bs", [128, 8], F32)
            wk = sbb("wk", [128, NBIS + 1], F32)
            pw2 = sbb("pw2", [128, NBIS + 1], F32)
            rden = sbb("rden", [128, 8], F32)
            On = sbb("On", [128, 512], BF16)
            OnT = sbb("OnT", [128, 4, 128], BF16)
            attb = [sbb(f"attb{j}", [128, 128], BF16) for j in range(2)]
            Rf = sbb("Rf", [128, 4, 256], F32, 4)
            Rb = sbb("Rb", [128, 4, 256], BF16, 4)
            gst = sbb("gst", [128, 4, 6], F32)
            gmv = sbb("gmv", [128, 4, 2], F32)
            gsm = sbb("gsm", [128, 16], F32)
            fA = sbb("fA", [128, D], F32)
            fB = sbb("fB", [128, D], F32)
            ub = sbb("ub", [128, D], BF16)
            uT = sbb("uT", [128, 8, 128], BF16)
            sm2 = sbb("sm2", [128, 8], F32)
            pS = [psb(f"pS{j}", [128, 512], F32) for j in range(3)]
            ptX = psb("ptX", [128, 1024], BF16)
            pL = [psb(f"pL{j}", [128, 512], F32) for j in range(2)]
            pLb = [p.t.bitcast(BF16) for p in pL]
            pY = psb("pY", [128, 1024], F32, 2)

            for kc in range(4):
                dma("pool", wba.t[:, kc, :], wba_d[kc * 128:(kc + 1) * 128, :], f"ld_wba{kc}", [], [wba.b[kc]])
            for kc in range(8):
                dma("pool", wbr.t[:, kc, :], wbr_d[kc * 128:(kc + 1) * 128, :], f"ld_wbr{kc}", [], [wbr.b[kc]])
            for kc in range(8):
                dma("pool", wo.t[:, kc, :], wo_d[kc * 128:(kc + 1) * 128, :], f"ld_wo{kc}", [], [wo.b[kc]])
            cdma(ggnT.t[:], ggnT_d, ggnT.b)
            cdma(g2.t[:], g2_d.broadcast_to([128, D]), g2.b)
            cdma(cmask.t[:], cmask_d, cmask.b)
            cdma(decT.t[:], decT_d, decT.b)
            cdma(epsg.t[:], epsg_d, epsg.b)
            ts("dve", bigI.t[:], ident.t[:], 30000.0, None, ALU.mult, None, ident.b, bigI.b)
            for gc in range(NGC):
                t0, t1_ = gc * 8, min(NT, gc * 8 + 8)
                dma("sp", ikT.t[0:64, t0 * 128:t1_ * 128], ikT_d[:, t0 * 128:t1_ * 128], f"ld_ikA{gc}", fence_a,
                    [ikT.b[gc]])
                dma("sp", ikT.t[64:128, t0 * 128:t1_ * 128], ikT_d[:, t0 * 128:t1_ * 128], f"ld_ikB{gc}", fence_a,
                    [ikT.b[gc]])
                dma("sp", kT.t[:, t0:t1_, :], kT_d[t0:t1_].rearrange("t p c -> p t c"), f"ld_kT{gc}", fence_a,
                    [kT.b[gc]])
                dma("sp", V.t[:, t0:t1_, :], v_d[t0 * 128:t1_ * 128, :].rearrange("(t p) c -> p t c", p=128),
                    f"ld_V{gc}", fence_a, [V.b[gc]])
            for h in range(4):
                S.op("dve", lambda e, h=h: e.memset(Rf.t[:, h, :], 0.0), [], [Rf.b[h]])
                S.op("dve", lambda e, h=h: e.memset(Rb.t[:, h, :], 0.0), [], [Rb.b[h]])
            for k in range(NBIS + 1):
                kk = min(k, NBIS - 1)
                S.op("dve", lambda e, k=k, kk=kk: e.memset(pw2.t[:, k:k + 1], float(2.0 ** -(kk + 1))), [], pw2.b)

            cnt = {"s": 0, "l": 0, "th": 0, "pt": 0}

            def X_gen(i):
                a = i % 2
                fq, wq, mT_ = fmq[a], iwq[a], maskT[a]
                nkt = i + 1
                nk = nkt * 128
                dma("sp", fq.t[:], fm_d[i], f"ld_fmq{a}", fence_a, fq.b)
                dma("sp", wq.t[:], iw_d[i * 128:(i + 1) * 128, :], f"ld_iwq{a}", fence_a, wq.b)
                nch = (nkt + 3) // 4
                for c in range(nch):
                    k0 = c * 512
                    wc = min(512, nk - k0)
                    kts = list(range(c * 4, min(c * 4 + 4, nkt)))
                    for h in range(8):
                        hp, pr = h % 2, h // 2
                        pb = pS[cnt["s"] % 3]
                        cnt["s"] += 1
                        thb = Th[cnt["th"] % 3]
                        cnt["th"] += 1
                        mm(pb.t[:, 0:wc], fq.t[hp * 64:(hp + 1) * 64, 512 + pr * 128:512 + (pr + 1) * 128],
                           ikT.t[hp * 64:(hp + 1) * 64, k0:k0 + wc], True, True,
                           fq.b + [ikT.b[k // 8] for k in kts], pb.b)
                        act(thb.t[:, 0:wc], pb.t[:, 0:wc], AF.Relu, pb.b, thb.b)
                        if h == 0:
                            ts("dve", Sc.t[:, k0:k0 + wc], thb.t[:, 0:wc], wq.t[:, 0:1], None, ALU.mult, None,
                               thb.b + wq.b, Sc.b)
                        else:
                            stt("dve", Sc.t[:, k0:k0 + wc], thb.t[:, 0:wc], wq.t[:, h:h + 1], Sc.t[:, k0:k0 + wc],
                                ALU.mult, ALU.add, thb.b + wq.b + Sc.b, Sc.b)
                        yield 0.25
                    if kts[-1] == i:
                        d0 = k0 + wc - 128
                        tt("dve", Sc.t[:, d0:d0 + 128], Sc.t[:, d0:d0 + 128], cmask.t[:], ALU.add,
                           Sc.b + cmask.b, Sc.b)
                if i >= 2:
                    S.op("dve", lambda e: e.tensor_reduce(out=bs.t[:, 0:1], in_=Sc.t[:, 0:nk], axis=AX.X,
                                                          op=ALU.max), Sc.b, bs.b)
                    S.op("dve", lambda e: e.tensor_reduce(out=bs.t[:, 1:2], in_=Sc.t[:, 0:nk - 128], axis=AX.X,
                                                          op=ALU.min), Sc.b, bs.b)
                    tt("dve", bs.t[:, 2:3], bs.t[:, 0:1], bs.t[:, 1:2], ALU.subtract, bs.b, bs.b)
                    ts("dve", wk.t[:], pw2.t[:], bs.t[:, 2:3], None, ALU.mult, None, pw2.b + bs.b, wk.b)
                    tt("dve", bs.t[:, 3:4], bs.t[:, 1:2], wk.t[:, 0:1], ALU.add, bs.b + wk.b, bs.b)
                    yield 2 * nk / 960.0 + 1.0
                    for k in range(NBIS):
                        ts("dve", maskb.t[:, 0:nk], Sc.t[:, 0:nk], bs.t[:, 3:4], None, ALU.is_ge, ALU.add,
                           Sc.b + bs.b, maskb.b + bs.b, accum=bs.t[:, 4:5])
                        ts("dve", bs.t[:, 5:6], bs.t[:, 4:5], TOPK - 0.5, wk.t[:, k:k + 1], ALU.is_ge, ALU.mult,
                           bs.b + wk.b, bs.b)
                        stt("dve", bs.t[:, 3:4], bs.t[:, 5:6], wk.t[:, k + 1:k + 2], bs.t[:, 3:4],
                            ALU.subtract, ALU.add, bs.b + wk.b, bs.b)
                        yield nk / 960.0 + 0.8
                    ts("dve", maskb.t[:, 0:nk], Sc.t[:, 0:nk], bs.t[:, 3:4], -1.0, ALU.is_ge, ALU.add,
                       Sc.b + bs.b, maskb.b)
                else:
                    ts("dve", maskb.t[:, 0:nk], Sc.t[:, 0:nk], -1.0e29, -1.0, ALU.is_ge, ALU.add, Sc.b, maskb.b)
                yield nk / 1900.0 + 0.3
                for g in range((nkt + 7) // 8):
                    ktl = list(range(g * 8, min(g * 8 + 8, nkt)))
                    for j, kt in enumerate(ktl):
                        tr(ptX.t[:, j * 128:(j + 1) * 128], maskb.t[:, kt * 128:(kt + 1) * 128], ident.t[:],
                           maskb.b + ident.b, ptX.b)
                    n = len(ktl)
                    cp("act", mT_.t[:, g * 8:g * 8 + n, :].rearrange("p a b -> p (a b)"), ptX.t[:, 0:n * 128],
                       ptX.b, mT_.b)
                    yield 1.0

            def nextL():
                j = cnt["l"] % 2
                cnt["l"] += 1
                return pL[j], pLb[j]

            def Y_gen(i):
                a = i % 2
                fq, tq, mT_ = fmq[a], tmq, maskT[a]
                nkt = i + 1
                dma("sp", tq.t[:], tm_d[i * 128:(i + 1) * 128, :], "ld_tmq", fence_a, tq.b)
                ngrp = (nkt + 3) // 4
                groups = [(h, g) for h in range(8) for g in range(ngrp)]

                def qk(h, g):
                    hp, pr = h % 2, h // 2
                    ktl = list(range(g * 4, min(g * 4 + 4, nkt)))
                    n = len(ktl)
                    pb, _ = nextL()
                    ptt = PT[cnt["pt"] % 3]
                    cnt["pt"] += 1
                    mm(pb.t[:, 0:n * 128], bigI.t[:],
                       mT_.t[:, g * 4:g * 4 + n, :].rearrange("p a b -> p (a b)"), True, False,
                       bigI.b + mT_.b, pb.b)
                    for j, kt in enumerate(ktl):
                        mm(pb.t[:, j * 128:(j + 1) * 128],
                           kT.t[hp * 64:(hp + 1) * 64, kt, pr * 128:(pr + 1) * 128],
                           fq.t[hp * 64:(hp + 1) * 64, pr * 128:(pr + 1) * 128], False, j == n - 1,
                           [kT.b[kt // 8]] + fq.b, pb.b)
                    act(ptt.t[:, 0:n * 128], pb.t[:, 0:n * 128], AF.Exp, pb.b, ptt.b, scale=0.125)
                    return ktl, ptt

                def pv(h, ktl, ptt):
                    hb_ = pY.b[0] if h < 4 else pY.b[1]
                    ocol = (h % 4) * 65 + (0 if h < 4 else 512)
                    for j, kt in enumerate(ktl):
                        mm(pY.t[:, ocol:ocol + 65], ptt.t[:, j * 128:(j + 1) * 128],
                           V.t[:, kt, h * 65:(h + 1) * 65], kt == 0, kt == nkt - 1,
                           ptt.b + [V.b[kt // 8]], [hb_])

                nxt = qk(*groups[0])
                for k, (h, g) in enumerate(groups):
                    cur = nxt
                    if k + 1 < len(groups):
                        nxt = qk(*groups[k + 1])
                    pv(h, *cur)
                    yield 1.6
                for half in range(2):
                    o4 = pY.t[:, half * 512:half * 512 + 260].rearrange("p (h d) -> p h d", h=4)
                    S.op("dve", lambda e, o4=o4, half=half: e.reciprocal(
                        out=rden.t[:, half * 4:half * 4 + 4].unsqueeze(2), in_=o4[:, :, 64:65]),
                        [pY.b[half]], rden.b)
                    tt("dve", On.t[:, half * 256:(half + 1) * 256].rearrange("p (h d) -> p h d", h=4),
                       o4[:, :, 0:64], rden.t[:, half * 4:half * 4 + 4].unsqueeze(2).broadcast_to([128, 4, 64]),
                       ALU.mult, [pY.b[half]] + rden.b, On.b)
                pb, pbb = nextL()
                for q in range(4):
                    tr(pbb[:, q * 128:(q + 1) * 128], On.t[:, q * 128:(q + 1) * 128], ident.t[:], On.b + ident.b, pb.b)
                cp("act", OnT.t[:].rearrange("p a b -> p (a b)"), pbb[:, 0:512], pb.b, OnT.b)
                yield 1.5
                for cc in range(2):
                    for kc in range(4):
                        mm(pY.t[:, cc * 512:(cc + 1) * 512], OnT.t[:, kc, :], wba.t[:, kc, cc * 512:(cc + 1) * 512],
                           kc == 0, kc == 3, OnT.b + wba.b, [pY.b[cc]])
                tt("dve", fB.t[:], pY.t[:], tq.t[:, 2560:3584], ALU.mult, pY.b + tq.b, fB.b)
                yield 2.5
                for h in range(4):
                    pb, _ = nextL()
                    ab = attb[h % 2]
                    rqT = fq.t[:, 1024 + h * 128:1024 + (h + 1) * 128]
                    rkT = fq.t[:, 1536 + h * 128:1536 + (h + 1) * 128]
                    rv = tq.t[:, 512 + h * 256:512 + (h + 1) * 256]
                    rkz = tq.t[:, h * 128:(h + 1) * 128]
                    zb = pY.b[h // 2]
                    mm(pb.t[:, 0:128], rkT, rqT, True, True, fq.b, pb.b)
                    tt("dve", ab.t[:], pb.t[:, 0:128], decT.t[:, h, :], ALU.mult, pb.b + decT.b, ab.b)
                    pb2, _ = nextL()
                    mm(pb2.t[:, 0:256], rkz, rv, True, True, tq.b, pb2.b)
                    mm(pY.t[:, h * 256:(h + 1) * 256], rqT, Rb.t[:, h, :], True, False, fq.b + [Rb.b[h]], [zb])
                    mm(pY.t[:, h * 256:(h + 1) * 256], ab.t[:], rv, False, True, ab.b + tq.b, [zb])
                    stt("dve", Rf.t[:, h, :], Rf.t[:, h, :], cdec[h], pb2.t[:, 0:256], ALU.mult, ALU.add,
                        [Rf.b[h]] + pb2.b, [Rf.b[h]])
                    cp("act", Rb.t[:, h, :], Rf.t[:, h, :], [Rf.b[h]], [Rb.b[h]])
                    yield 1.5
                for h in range(4):
                    S.op("dve", lambda e, h=h: e.bn_stats(out=gst.t[:, h, :], in_=pY.t[:, h * 256:(h + 1) * 256]),
                         [pY.b[h // 2]], gst.b)
                    S.op("dve", lambda e, h=h: e.bn_aggr(out=gmv.t[:, h, :], in_=gst.t[:, h, :]), gst.b, gmv.b)
                tt("dve", gsm.t[:, 0:4].unsqueeze(2), gmv.t[:, :, 1:2], epsg.t[:].unsqueeze(2), ALU.add,
                   gmv.b + epsg.b, gsm.b)
                act(gsm.t[:, 4:8], gsm.t[:, 0:4], AF.Ln, gsm.b, gsm.b)
                act(gsm.t[:, 8:12], gsm.t[:, 4:8], AF.Exp, gsm.b, gsm.b, scale=-0.5)
                stt("dve", gsm.t[:, 12:16].unsqueeze(2), gmv.t[:, :, 0:1], -1.0, gsm.t[:, 8:12].unsqueeze(2),
                    ALU.mult, ALU.mult, gmv.b + gsm.b, gsm.b)
                yield 2.0
                for h in range(4):
                    act(fA.t[:, h * 256:(h + 1) * 256], pY.t[:, h * 256:(h + 1) * 256], AF.Identity,
                        [pY.b[h // 2]] + gsm.b, fA.b, bias=gsm.t[:, 12 + h:13 + h], scale=gsm.t[:, 8 + h:9 + h])
                tt("dve", ub.t[:], fA.t[:], tq.t[:, 1536:2560], ALU.mult, fA.b + tq.b, ub.b)
                pb, pbb = nextL()
                for kc in range(8):
                    tr(pbb[:, kc * 128:(kc + 1) * 128], ub.t[:, kc * 128:(kc + 1) * 128], ident.t[:], ub.b + ident.b, pb.b)
                for kc in range(8):
                    act(uT.t[:, kc, :], pbb[:, kc * 128:(kc + 1) * 128], AF.Copy, pb.b + ggnT.b, uT.b,
                        scale=ggnT.t[:, kc:kc + 1])
                yield 3.0
                for cc in range(2):
                    for kc in range(8):
                        mm(pY.t[:, cc * 512:(cc + 1) * 512], uT.t[:, kc, :], wbr.t[:, kc, cc * 512:(cc + 1) * 512],
                           kc == 0, kc == 7, uT.b + wbr.b, [pY.b[cc]])
                tt("dve", fA.t[:], pY.t[:], tq.t[:, 3584:4608], ALU.mult, pY.b + tq.b, fA.b)
                tt("dve", ub.t[:], fB.t[:], fA.t[:], ALU.add, fB.b + fA.b, ub.b)
                yield 4.0
                pb, pbb = nextL()
                for kc in range(8):
                    tr(pbb[:, kc * 128:(kc + 1) * 128], ub.t[:, kc * 128:(kc + 1) * 128], ident.t[:], ub.b + ident.b, pb.b)
                cp("act", uT.t[:].rearrange("p a b -> p (a b)"), pbb[:, :], pb.b, uT.b)
                for cc in range(2):
                    for kc in range(8):
                        mm(pY.t[:, cc * 512:(cc + 1) * 512], uT.t[:, kc, :], wo.t[:, kc, cc * 512:(cc + 1) * 512],
                           kc == 0, kc == 7, uT.b + wo.b, [pY.b[cc]])
                yield 4.0
                act(ub.t[:], pY.t[:], AF.Square, pY.b, ub.b + sm2.b, accum=sm2.t[:, 0:1])
                rstd_from_ss(sm2.t[:, 0:1], sm2.t[:, 1:2], sm2.t[:, 2:3], 1.0 / D, sm2.b, sm2.b)
                stt("dve", fB.t[:], pY.t[:], sm2.t[:, 2:3], g2.t[:], ALU.mult, ALU.mult, pY.b + sm2.b + g2.b, fB.b)
                dma("sp", x1_d[i * 128:(i + 1) * 128, :], fB.t[:], "st_x1", fB.b, [])
                yield 2.5

            def drive(gy, gx):
                cy = cx = 0.0
                while gy is not None or gx is not None:
                    if gy is not None and (gx is None or cy <= cx):
                        try:
                            cy += next(gy)
                        except StopIteration:
                            gy = None
                    else:
                        try:
                            cx += next(gx)
                        except StopIteration:
                            gx = None

            drive(None, X_gen(0))
            for i in range(NT):
                drive(Y_gen(i), X_gen(i + 1) if i + 1 < NT else None)

            fence_b = fB.b
            allB = fence_b + kT.b + V.b + ikT.b + wba.b + wbr.b + wo.b + ggnT.b + g2.b + cmask.b + decT.b + epsg.b \
                + bigI.b + tmq.b
            for lst in (fmq, iwq, Th, PT, attb, maskT, pS, pL):
                for x_ in lst:
                    allB += x_.b
            for x_ in (Sc, maskb, bs, wk, pw2, rden, On, OnT, Rf, Rb, gst, gmv, gsm, fA, fB, ub, uT,
                       sm2, ptX, pY):
                allB += x_.b
        for e in Sched.ENGS:
            S.wait_all(e, allB)

        if stop_after == "B":
            S.wait_all("sp", allB)
            S.emit()
            return nc

        GT = 2 if NT % 2 == 0 else 1
        NG = NT // GT
        GW = GT * 128
        with ExitStack() as sc:
            def sbc(name, shape, dt, n=1):
                return TB(sc.enter_context(nc.sbuf_tensor(name, list(shape), dt)), n)

            def psc_(name, shape, dt, n=1):
                return TB(sc.enter_context(nc.psum_tensor(name, list(shape), dt)), n)

            wup = sbc("wup", [128, 8, 2 * DFF], BF16, 8)
            wdn = sbc("wdn", [128, 22, D], BF16, 22)
            g3 = sbc("g3", [128, D], F32)
            g4 = sbc("g4", [128, D], F32)
            cw = sbc("cw", [128, 44, 3], F32)
            cb = sbc("cb", [128, 44], F32)
            hal = sbc("hal", [128, 44, 2], F32, 44)
            xc = [sbc(f"xc{j}", [128, D], F32) for j in range(2 * GT)]
            r1c = [sbc(f"r1c{j}", [128, D], F32) for j in range(2)]
            outc = [sbc(f"outc{j}", [128, D], F32) for j in range(2)]
            hbc = sbc("hbc", [128, D], BF16)
            junkc = sbc("junkc", [128, D], BF16)
            sm3 = [sbc(f"sm3_{j}", [128, 8], F32) for j in range(2)]
            sm4 = [sbc(f"sm4_{j}", [128, 8], F32) for j in range(2)]
            hTg2 = [sbc(f"hTg{j}", [128, 8, GW], BF16) for j in range(2)]
            ubuf = [sbc(f"ubuf{j}", [128, GW + 2], F32) for j in range(3)]
            accg = [sbc(f"accg{j}", [128, GW], F32) for j in range(2)]
            accu = [sbc(f"accu{j}", [128, GW], F32) for j in range(2)]
            sg = accg
            aT = sbc("aT", [128, 22, GW], BF16, 22)
            pup = [psc_(f"pup{j}", [128, 512], F32) for j in range(3)]
            ptc = psc_("ptc", [128, 1024], BF16)
            pdn = psc_("pdn", [128, 1024], F32, 2)

            for kc in range(8):
                dma("pool", wup.t[:, kc, :], wup_d[kc * 128:(kc + 1) * 128, :], f"ld_wup{kc}", [], [wup.b[kc]])
            for fc in range(22):
                dma("pool", wdn.t[:, fc, :], wdn_d[fc * 128:(fc + 1) * 128, :], f"ld_wdn{fc % 4}", [], [wdn.b[fc]])
            cdma(g3.t[:], g3_d.broadcast_to([128, D]), g3.b)
            cdma(g4.t[:], g4_d.broadcast_to([128, D]), g4.b)
            cdma(cw.t[:], cw_d, cw.b)
            cdma(cb.t[:], cb_d, cb.b)
            S.op("pool", lambda e: e.memset(hal.t[:], 0.0), [], hal.b)

            ukc = {"k": 0}

            def driveC(gy, gx):
                cy = cx = 0.0
                while gy is not None or gx is not None:
                    if gy is not None and (gx is None or cy <= cx):
                        try:
                            cy += next(gy)
                        except StopIteration:
                            gy = None
                    else:
                        try:
                            cx += next(gx)
                        except StopIteration:
                            gx = None

            def PC_gen(g):
                hTg = hTg2[g % 2]
                for tl in range(GT):
                    i = g * GT + tl
                    a = i % 2
                    xt, smt, r1t = xc[(g % 2) * GT + tl], sm3[a], r1c[a]
                    dma("sp", xt.t[:], x_d[i * 128:(i + 1) * 128, :], f"ld_xc{(g % 2) * GT + tl}", [], xt.b)
                    dma("sp", r1t.t[:], x1_d[i * 128:(i + 1) * 128, :], f"ld_r1c{a}", fence_b, r1t.b)
                    tt("dve", xt.t[:], xt.t[:], r1t.t[:], ALU.add, xt.b + r1t.b, xt.b)
                    act(hbc.t[:], xt.t[:], AF.Square, xt.b, hbc.b + smt.b, accum=smt.t[:, 0:1])
                    rstd_from_ss(smt.t[:, 0:1], smt.t[:, 1:2], smt.t[:, 2:3], 1.0 / D, smt.b, smt.b)
                    stt("dve", hbc.t[:], xt.t[:], smt.t[:, 2:3], g3.t[:], ALU.mult, ALU.mult, xt.b + smt.b + g3.b, hbc.b)
                    yield 14.0
                    for kc in range(8):
                        tr(ptc.t[:, kc * 128:(kc + 1) * 128], hbc.t[:, kc * 128:(kc + 1) * 128], ident.t[:],
                           hbc.b + ident.b, ptc.b)
                    cp("act", hTg.t[:, :, tl * 128:(tl + 1) * 128],
                       ptc.t[:].rearrange("p (a b) -> p a b", a=8), ptc.b, hTg.b)
                    yield 1.0

            def MC_gen(g):
                hTg = hTg2[g % 2]
                for fc in range(22):
                    for which, c in ((0, fc), (1, fc + 22)):
                        pb = pup[ukc["k"] % 3]
                        ubf = ubuf[ukc["k"] % 3]
                        ukc["k"] += 1
                        for kc in range(8):
                            mm(pb.t[:, 0:GW], wup.t[:, kc, c * 128:(c + 1) * 128], hTg.t[:, kc, :], kc == 0, kc == 7,
                               [wup.b[kc]] + hTg.b, pb.b)
                        accx = (accg if which == 0 else accu)[fc % 2]
                        cp("pool", ubf.t[:, 0:2], hal.t[:, c, :], [hal.b[c]], ubf.b)
                        cp("act", ubf.t[:, 2:GW + 2], pb.t[:, 0:GW], pb.b, ubf.b)
                        cp("pool", hal.t[:, c, :], ubf.t[:, GW:GW + 2], ubf.b, [hal.b[c]])
                        act(accx.t[:], pb.t[:, 0:GW], AF.Identity, pb.b + cw.b + cb.b, accx.b,
                            bias=cb.t[:, c:c + 1], scale=cw.t[:, c, 2:3])
                        stt("dve", accx.t[:], ubf.t[:, 1:GW + 1], cw.t[:, c, 1:2], accx.t[:], ALU.mult, ALU.add,
                            ubf.b + cw.b + accx.b, accx.b)
                        stt("dve", accx.t[:], ubf.t[:, 0:GW], cw.t[:, c, 0:1], accx.t[:], ALU.mult, ALU.add,
                            ubf.b + cw.b + accx.b, accx.b)
                        yield 0.9
                    ag, au = accg[fc % 2], accu[fc % 2]
                    act(ag.t[:], ag.t[:], AF.Silu, ag.b, ag.b)
                    tt("dve", aT.t[:, fc, :], ag.t[:], au.t[:], ALU.mult, ag.b + au.b, [aT.b[fc]])
                for tl in range(GT):
                    i = g * GT + tl
                    a = i % 2
                    for cc in range(2):
                        for fc in range(22):
                            mm(pdn.t[:, cc * 512:(cc + 1) * 512], aT.t[:, fc, tl * 128:(tl + 1) * 128],
                               wdn.t[:, fc, cc * 512:(cc + 1) * 512], fc == 0, fc == 21,
                               [aT.b[fc], wdn.b[fc]], [pdn.b[cc]])
                    smt = sm4[a]
                    act(junkc.t[:], pdn.t[:], AF.Square, pdn.b, junkc.b + smt.b, accum=smt.t[:, 4:5])
                    rstd_from_ss(smt.t[:, 4:5], smt.t[:, 5:6], smt.t[:, 6:7], 1.0 / D, smt.b, smt.b)
                    xrt, ot = xc[(g % 2) * GT + tl], outc[a]
                    stt("dve", ot.t[:], pdn.t[:], smt.t[:, 6:7], g4.t[:], ALU.mult, ALU.mult, pdn.b + smt.b + g4.b, ot.b)
                    tt("dve", ot.t[:], ot.t[:], xrt.t[:], ALU.add, ot.b + xrt.b, ot.b)
                    dma("sp", out_d[i * 128:(i + 1) * 128, :], ot.t[:], f"st_out{a}", ot.b, [])
                    yield 9.5

            driveC(None, PC_gen(0))
            for g in range(NG):
                driveC(MC_gen(g), PC_gen(g + 1) if g + 1 < NG else None)
            S.wait_all("sp", outc[0].b + outc[1].b)
        S.emit()
    return nc


def _consts():
    invf128 = 10000.0 ** (-(np.arange(64, dtype=np.float32) * 2.0 / 128)).astype(np.float32)
    invf64 = 10000.0 ** (-(np.arange(32, dtype=np.float32) * 2.0 / 64)).astype(np.float32)
    invf = np.concatenate([invf128, invf64]).astype(np.float32)
    c_invf = np.ascontiguousarray(np.broadcast_to(invf[None, :], (128, 96))).astype(np.float32)
    c_ident = np.eye(128, dtype=np.float32).astype(ml_dtypes.bfloat16)
    jj = np.arange(128)
    c_cmask = np.where(jj[None, :] <= jj[:, None], 0.0, NEG).astype(np.float32)
    gam = 1.0 - 2.0 ** (-5.0 - np.arange(4, dtype=np.float64))
    c = 128.0 ** -0.5
    decT = np.zeros((128, 4, 128), np.float64)
    for h in range(4):
        decT[:, h, :] = (c * gam[h] ** (-(jj[:, None] + 1.0))) * (jj[:, None] <= jj[None, :])
    zc = c * gam[None, :] ** (127.0 - jj[:, None])
    xi = gam[None, :] ** (jj[:, None] + 1.0)
    epsg = EPS / xi ** 2
    return dict(c_invf=c_invf, c_ident=c_ident, c_cmask=c_cmask, c_decT=decT.astype(np.float32),
                c_zc=zc.astype(np.float32), c_epsg=epsg.astype(np.float32))


def make_in_maps(inputs, NT=NTF, cores=8):
    T = NT * 128
    f = lambda a: np.ascontiguousarray(np.asarray(a, dtype=np.float32))
    cw = f(inputs["conv_w"])[0]
    cw_l = np.ascontiguousarray(cw.reshape(3, 44, 128).transpose(2, 1, 0))
    cb_l = np.ascontiguousarray(f(inputs["conv_b"])[0].reshape(44, 128).T)
    shared = dict(
        g_pre_mix=f(inputs["norm_pre_mix"]), w_in=f(inputs["w_in"])[0], w_br_attn=f(inputs["w_br_attn"])[0],
        w_br_ret=f(inputs["w_br_ret"])[0],
        ggnT=np.ascontiguousarray(f(inputs["ret_gn_gain"])[0].reshape(8, 128).T), w_out=f(inputs["w_out"])[0],
        g_post_mix=f(inputs["norm_post_mix"]), g_pre_ffn=f(inputs["norm_pre_ffn"]),
        w_ffn_up=f(inputs["w_ffn_up"])[0], conv_w=cw_l, conv_b=cb_l, w_ffn_down=f(inputs["w_ffn_down"])[0],
        g_post_ffn=f(inputs["norm_post_ffn"]),
    )
    shared.update(_consts())
    x = np.asarray(inputs["x"], dtype=np.float32)
    pos = np.asarray(inputs["positions"]).astype(np.int32)
    maps = []
    for b in range(cores):
        m = dict(shared)
        m["x"] = np.ascontiguousarray(x[b, :T])
        m["pos"] = np.ascontiguousarray(pos[b, :T].reshape(NT, 128).T)
        maps.append(m)
    return maps


_NC_CACHE = {}


def kernel(**inputs):
    if "full" not in _NC_CACHE:
        _NC_CACHE["full"] = build_program(NTF)
    nc = _NC_CACHE["full"]
    maps = make_in_maps(inputs, NTF, 8)
    res = run_bass_kernel_spmd(nc, maps, core_ids=list(range(8)))
    out = np.stack([np.asarray(r["out"], dtype=np.float32) for r in res.results], axis=0)
    return out
```
